# Optimizing a Trainium2 kernel written in Bass

```python
import jax
import jax.numpy as jnp
from jax import lax
import numpy as np

D_MODEL = 1024
BATCH = 2
SEQ = 16384
DEPTH = 4

GRID_W = 64
CTX_LEN = 256
HEAD_DIM = 64
NA_HEADS = 6
NA_DIM = NA_HEADS * HEAD_DIM
NA_ROWS = 8
NA_COLS = 16
NA_QBLOCK = 16
NA_KSPAN = 32
HG_HEADS = 6
HG_DK = 64
HG_DV = 64
HG_DIM = HG_HEADS * HG_DV
HG_CHUNK = 64
CONV_DIM = 256
CONV_WIDTH = 3
MIX_DIM = NA_DIM + HG_DIM + CONV_DIM
N_EXPERTS = 16
EXPERT_DIM = 512
EC_CAPACITY_FACTOR = 2
ROPE_BASE = 10000.0
NORM_EPS = 1e-6
N_MOD = 6

kernel_name = 'hybrid_na_hgrn2_shortconv_ecmoe_dit'


def _in_sizes():
    return [NA_DIM] * 3 + [HG_HEADS * HG_DK] * 3 + [HG_HEADS * HG_DV] * 2 + [CONV_DIM] * 3


def _split_points():
    return [int(s) for s in np.cumsum(_in_sizes())[:-1]]


def _rmsnorm(x, w):
    xf = x.astype(jnp.float32)
    y = xf * lax.rsqrt(jnp.mean(xf * xf, axis=-1, keepdims=True) + NORM_EPS)
    return (y * w.astype(jnp.float32)).astype(x.dtype)


def _adaln(cond, w, b):
    return jnp.matmul(jax.nn.silu(cond), w) + b


def _modulate(h, shift, scale):
    return h * (1 + scale) + shift


def _rope_tables(n, dtype):
    t = jnp.arange(n, dtype=jnp.int32)
    row = (t // GRID_W).astype(jnp.float32)
    col = (t % GRID_W).astype(jnp.float32)
    n_freq = HEAD_DIM // 4
    inv = ROPE_BASE ** (-jnp.arange(n_freq, dtype=jnp.float32) / n_freq)
    ar = row[:, None] * inv
    ac = col[:, None] * inv
    ang = jnp.concatenate([ar, ar, ac, ac], axis=-1)
    return jnp.cos(ang).astype(dtype)[:, None, :], jnp.sin(ang).astype(dtype)[:, None, :]


def _rot(x):
    x0, x1, x2, x3 = jnp.split(x, 4, axis=-1)
    return jnp.concatenate([-x1, x0, -x3, x2], axis=-1)


def _rope(x, cos, sin):
    return x * cos + _rot(x) * sin


def _na_col_tables():
    cols = np.arange(GRID_W)
    win_start = np.clip(cols - NA_COLS // 2, 0, GRID_W - NA_COLS)
    n_cb = GRID_W // NA_QBLOCK
    blk_start = np.clip(win_start[::NA_QBLOCK], 0, GRID_W - NA_KSPAN)
    key_cols = blk_start[:, None] + np.arange(NA_KSPAN)[None, :]
    q_cols = cols.reshape(n_cb, NA_QBLOCK)
    q_start = win_start.reshape(n_cb, NA_QBLOCK)
    kc = key_cols[:, None, :]
    mask = (kc >= q_start[..., None]) & (kc < q_start[..., None] + NA_COLS)
    rel = np.clip(kc - q_cols[..., None] + NA_COLS - 1, 0, 2 * NA_COLS - 2)
    return key_cols, rel, mask


def _na_latent(q, k, v, k_ctx, v_ctx, rpb):
    b, n, h, d = q.shape
    rows = n // GRID_W
    kr = min(NA_ROWS, rows)
    n_cb = GRID_W // NA_QBLOCK
    key_cols_np, rel_col_np, col_mask_np = _na_col_tables()
    key_cols = jnp.asarray(key_cols_np)
    rel_col = jnp.asarray(rel_col_np)
    n_loc = kr * NA_KSPAN
    win_mask = jnp.asarray(np.broadcast_to(col_mask_np[:, :, None, :], (n_cb, NA_QBLOCK, kr, NA_KSPAN)).reshape(n_cb, NA_QBLOCK, n_loc))
    kg = k.reshape(b, rows, GRID_W, h, d)
    vg = v.reshape(b, rows, GRID_W, h, d)
    q_rows = q.reshape(b, rows, n_cb, NA_QBLOCK, h, d).transpose(1, 0, 2, 3, 4, 5)
    scale = d ** -0.5

    def gather_blocks(g, rs):
        blk = lax.dynamic_slice_in_dim(g, rs, kr, axis=1)[:, :, key_cols]
        return blk.transpose(0, 2, 1, 3, 4, 5).reshape(b, n_cb, n_loc, h, d)

    def row_fn(xs):
        r, q_r = xs
        rs = jnp.clip(r - kr // 2, 0, rows - kr)
        k_loc = gather_blocks(kg, rs)
        v_loc = gather_blocks(vg, rs)
        rel_row = rs + jnp.arange(kr) - r + (NA_ROWS - 1)
        bias = rpb[:, rel_row[:, None, None, None], rel_col[None]]
        bias = bias.transpose(0, 2, 3, 1, 4).reshape(h, n_cb, NA_QBLOCK, n_loc)
        s_loc = jnp.einsum('bjqhd,bjkhd->bhjqk', q_r, k_loc).astype(jnp.float32) * scale + bias.astype(jnp.float32)
        s_loc = jnp.where(win_mask, s_loc, -jnp.inf)
        s_ctx = jnp.einsum('bjqhd,bchd->bhjqc', q_r, k_ctx).astype(jnp.float32) * scale
        p = jax.nn.softmax(jnp.concatenate([s_loc, s_ctx], axis=-1), axis=-1).astype(v.dtype)
        o_loc = jnp.einsum('bhjqk,bjkhd->bjqhd', p[..., :n_loc], v_loc)
        o_ctx = jnp.einsum('bhjqc,bchd->bjqhd', p[..., n_loc:], v_ctx)
        return o_loc + o_ctx

    out = lax.map(row_fn, (jnp.arange(rows, dtype=jnp.int32), q_rows))
    return out.transpose(1, 0, 2, 3, 4, 5).reshape(b, n, h, d)


def _ctx_attn(q, k, v):
    s = jnp.einsum('bqhd,bkhd->bhqk', q, k).astype(jnp.float32) * (q.shape[-1] ** -0.5)
    p = jax.nn.softmax(s, axis=-1).astype(v.dtype)
    return jnp.einsum('bhqk,bkhd->bqhd', p, v)


def _hgrn_lower_bounds(logits):
    p = jax.nn.softmax(logits.astype(jnp.float32), axis=1)
    return jnp.concatenate([jnp.zeros_like(p[:, :1]), jnp.cumsum(p[:, 1:], axis=1)], axis=1)


def _hgrn_gates(f_logit, lb):
    z = f_logit.astype(jnp.float32)
    log_f = jnp.logaddexp(jnp.log(lb), jnp.log1p(-lb) + jax.nn.log_sigmoid(z))
    k = (1 - lb) * jax.nn.sigmoid(-z)
    return log_f, k


def _heads(a, dh):
    return a.reshape(a.shape[0], a.shape[1], -1, dh).astype(jnp.float32)


def _gla_scan(q, k, log_f, v, s0):
    b, t, h, _ = q.shape
    nc = t // HG_CHUNK

    def to_chunks(a):
        return a.reshape(b, nc, HG_CHUNK, h, a.shape[-1]).transpose(1, 0, 3, 2, 4)

    causal = jnp.tril(jnp.ones((HG_CHUNK, HG_CHUNK), dtype=bool))

    def step(s, xs):
        qc, kc, gc, vc = xs
        a = jnp.cumsum(gc, axis=2)
        diff = a[:, :, :, None, :] - a[:, :, None, :, :]
        decay = jnp.exp(jnp.where(causal[:, :, None], diff, -jnp.inf))
        scores = jnp.sum(qc[:, :, :, None, :] * kc[:, :, None, :, :] * decay, axis=-1)
        o = jnp.einsum('bhts,bhsv->bhtv', scores, vc) + jnp.einsum('bhtk,bhkv->bhtv', qc * jnp.exp(a), s)
        a_last = a[:, :, -1:, :]
        s_new = jnp.exp(a_last[:, :, 0, :, None]) * s + jnp.einsum('bhsk,bhsv->bhkv', kc * jnp.exp(a_last - a), vc)
        return s_new, o

    s_fin, o = lax.scan(step, s0, (to_chunks(q), to_chunks(k), to_chunks(log_f), to_chunks(v)))
    o = o.transpose(1, 0, 3, 2, 4).reshape(b, t, h, v.shape[-1])
    return o, s_fin


def _hgrn2_mixer(lat, cxt, lb_fwd, lb_bwd, norm_w, need_ctx):
    q_l, ff_l, fb_l, i_l, g_l = lat
    q_c, ff_c, fb_c, i_c, g_c = cxt
    dtype = q_l.dtype
    b = q_l.shape[0]
    scale = HG_DK ** -0.5
    ql = _heads(q_l, HG_DK) * scale
    qc = _heads(q_c, HG_DK) * scale
    vl = _heads(i_l, HG_DV)
    vc = _heads(i_c, HG_DV)
    o_l = jnp.zeros_like(vl)
    o_c = jnp.zeros_like(vc)
    for f_lat, f_ctx, lb, rev in ((ff_l, ff_c, lb_fwd, False), (fb_l, fb_c, lb_bwd, True)):
        lb = lb.astype(jnp.float32).reshape(HG_HEADS, HG_DK)
        lf_l, k_l = _hgrn_gates(_heads(f_lat, HG_DK), lb)
        lf_c, k_c = _hgrn_gates(_heads(f_ctx, HG_DK), lb)
        lat_seq = (ql, k_l, lf_l, vl)
        ctx_seq = (qc, k_c, lf_c, vc)
        if rev:
            lat_seq = tuple(jnp.flip(a, axis=1) for a in lat_seq)
            ctx_seq = tuple(jnp.flip(a, axis=1) for a in ctx_seq)
        s0 = jnp.zeros((b, HG_HEADS, HG_DK, HG_DV), jnp.float32)
        oc, s_ctx = _gla_scan(*ctx_seq, s0)
        ol, _ = _gla_scan(*lat_seq, s_ctx)
        if rev:
            oc = jnp.flip(oc, axis=1)
            ol = jnp.flip(ol, axis=1)
        o_l = o_l + ol
        o_c = o_c + oc
    y_l = (_rmsnorm(o_l, norm_w) * jax.nn.silu(_heads(g_l, HG_DV))).reshape(g_l.shape).astype(dtype)
    if not need_ctx:
        return y_l, None
    y_c = (_rmsnorm(o_c, norm_w) * jax.nn.silu(_heads(g_c, HG_DV))).reshape(g_c.shape).astype(dtype)
    return y_l, y_c


def _short_gated_conv(b_gate, c_gate, u, w):
    z = c_gate * u
    t = z.shape[1]
    zp = jnp.pad(z, ((0, 0), (CONV_WIDTH // 2, CONV_WIDTH // 2), (0, 0)))
    y = sum(w[j] * zp[:, j:j + t] for j in range(CONV_WIDTH))
    return b_gate * y


def _token_mixers(h, hc, w_in_l, q_norm, k_norm, rpb, lb_fwd, lb_bwd, hg_norm_l, conv_w_l, rope_cos, rope_sin, need_ctx):
    b, n, _ = h.shape
    ctx_len = hc.shape[1]
    pts = _split_points()
    p = jnp.split(jnp.einsum('btd,dk->btk', h, w_in_l), pts, axis=-1)
    pc = jnp.split(jnp.einsum('btd,dk->btk', hc, w_in_l), pts, axis=-1)

    def na_heads(a):
        return a.reshape(a.shape[0], a.shape[1], NA_HEADS, HEAD_DIM)

    q = _rope(_rmsnorm(na_heads(p[0]), q_norm), rope_cos, rope_sin)
    k = _rope(_rmsnorm(na_heads(p[1]), k_norm), rope_cos, rope_sin)
    v = na_heads(p[2])
    qc = _rmsnorm(na_heads(pc[0]), q_norm)
    kc = _rmsnorm(na_heads(pc[1]), k_norm)
    vc = na_heads(pc[2])
    na = _na_latent(q, k, v, kc, vc, rpb).reshape(b, n, NA_DIM)
    hg, hg_c = _hgrn2_mixer(p[3:8], pc[3:8], lb_fwd, lb_bwd, hg_norm_l, need_ctx)
    cv = _short_gated_conv(p[8], p[9], p[10], conv_w_l)
    mix = jnp.concatenate([na, hg, cv], axis=-1)
    if not need_ctx:
        return mix, None
    na_c = _ctx_attn(qc, kc, vc).reshape(b, ctx_len, NA_DIM)
    cv_c = _short_gated_conv(pc[8], pc[9], pc[10], conv_w_l)
    return mix, jnp.concatenate([na_c, hg_c, cv_c], axis=-1)


def _expert_choice_ffn(h, w_router, w_gate, w_up, w_down):
    b, t, d = h.shape
    cap = EC_CAPACITY_FACTOR * t // N_EXPERTS
    aff = jax.nn.softmax(jnp.einsum('btd,de->bte', h, w_router).astype(jnp.float32), axis=-1)
    vals, idx = lax.top_k(aff.transpose(0, 2, 1), cap)
    xg = jax.vmap(lambda hb, ib: hb[ib])(h, idx)
    hid = jax.nn.silu(jnp.einsum('becd,edf->becf', xg, w_gate)) * jnp.einsum('becd,edf->becf', xg, w_up)
    out = jnp.einsum('becf,efd->becd', hid, w_down) * vals[..., None].astype(h.dtype)
    return jax.vmap(lambda ob, ib: jnp.zeros((t, d), h.dtype).at[ib.reshape(-1)].add(ob.reshape(-1, d)))(out, idx)


def setup_inputs(seed: int = 0) -> dict:
    key = jax.random.key(seed)
    ks = jax.random.split(key, 20)
    nrm = jax.random.normal
    d = D_MODEL
    in_cols = sum(_in_sizes())
    return {
        'x': nrm(ks[0], (BATCH, SEQ, d), jnp.float32),
        'c': nrm(ks[1], (BATCH, d), jnp.float32),
        'ctx': nrm(ks[2], (BATCH, CTX_LEN, d), jnp.float32),
        'c_ctx': nrm(ks[3], (d,), jnp.float32),
        'w_mod': nrm(ks[4], (DEPTH, d, N_MOD * d), jnp.float32) * (0.5 * d ** -0.5),
        'b_mod': 0.02 * nrm(ks[5], (DEPTH, N_MOD * d), jnp.float32),
        'norm1_w': 1.0 + 0.1 * nrm(ks[6], (DEPTH, d), jnp.float32),
        'w_in': nrm(ks[7], (DEPTH, d, in_cols), jnp.float32) * d ** -0.5,
        'na_q_norm': 1.0 + 0.1 * nrm(ks[8], (DEPTH, HEAD_DIM), jnp.float32),
        'na_k_norm': 1.0 + 0.1 * nrm(ks[9], (DEPTH, HEAD_DIM), jnp.float32),
        'na_rpb': 0.5 * nrm(ks[10], (DEPTH, NA_HEADS, 2 * NA_ROWS - 1, 2 * NA_COLS - 1), jnp.float32),
        'hg_lb_logits': nrm(ks[11], (2, DEPTH, HG_HEADS * HG_DK), jnp.float32),
        'hg_norm': 1.0 + 0.1 * nrm(ks[12], (DEPTH, HG_DV), jnp.float32),
        'conv_w': nrm(ks[13], (DEPTH, CONV_WIDTH, CONV_DIM), jnp.float32) * CONV_WIDTH ** -0.5,
        'w_out': nrm(ks[14], (DEPTH, MIX_DIM, d), jnp.float32) * MIX_DIM ** -0.5,
        'norm2_w': 1.0 + 0.1 * nrm(ks[15], (DEPTH, d), jnp.float32),
        'w_router': nrm(ks[16], (DEPTH, d, N_EXPERTS), jnp.float32) * d ** -0.5,
        'w_exp_gate': nrm(ks[17], (DEPTH, N_EXPERTS, d, EXPERT_DIM), jnp.float32) * d ** -0.5,
        'w_exp_up': nrm(ks[18], (DEPTH, N_EXPERTS, d, EXPERT_DIM), jnp.float32) * d ** -0.5,
        'w_exp_down': nrm(ks[19], (DEPTH, N_EXPERTS, EXPERT_DIM, d), jnp.float32) * EXPERT_DIM ** -0.5,
    }


def reference(x, c, ctx, c_ctx, w_mod, b_mod, norm1_w, w_in, na_q_norm, na_k_norm, na_rpb, hg_lb_logits, hg_norm, conv_w, w_out, norm2_w, w_router, w_exp_gate, w_exp_up, w_exp_down):
    n = x.shape[1]
    rope_cos, rope_sin = _rope_tables(n, x.dtype)
    lb = _hgrn_lower_bounds(hg_lb_logits)
    cx = ctx
    for l in range(DEPTH):
        need_ctx = l < DEPTH - 1
        m_lat = jnp.split(_adaln(c, w_mod[l], b_mod[l])[:, None, :], N_MOD, axis=-1)
        m_ctx = jnp.split(_adaln(c_ctx, w_mod[l], b_mod[l]), N_MOD, axis=-1)
        h = _modulate(_rmsnorm(x, norm1_w[l]), m_lat[0], m_lat[1])
        hc = _modulate(_rmsnorm(cx, norm1_w[l]), m_ctx[0], m_ctx[1])
        mix, mix_c = _token_mixers(h, hc, w_in[l], na_q_norm[l], na_k_norm[l], na_rpb[l], lb[0, l], lb[1, l], hg_norm[l], conv_w[l], rope_cos, rope_sin, need_ctx)
        x = x + m_lat[2] * jnp.einsum('btm,md->btd', mix, w_out[l])
        h2 = _modulate(_rmsnorm(x, norm2_w[l]), m_lat[3], m_lat[4])
        x = x + m_lat[5] * _expert_choice_ffn(h2, w_router[l], w_exp_gate[l], w_exp_up[l], w_exp_down[l])
        if need_ctx:
            cx = cx + m_ctx[2] * jnp.einsum('btm,md->btd', mix_c, w_out[l])
            hc2 = _modulate(_rmsnorm(cx, norm2_w[l]), m_ctx[3], m_ctx[4])
            cx = cx + m_ctx[5] * _expert_choice_ffn(hc2, w_router[l], w_exp_gate[l], w_exp_up[l], w_exp_down[l])
    return x
```

```python
import contextlib
import numpy as np
import concourse.bass as bass
import concourse.mybir as mybir
from concourse.bass_utils import run_bass_kernel_spmd

F32 = mybir.dt.float32
BF16 = mybir.dt.bfloat16
ALU = mybir.AluOpType
AF = mybir.ActivationFunctionType
AX = mybir.AxisListType

ENGS = ("sp", "act", "dve", "pool", "pe")
NSLOT = 8


class Buf:
    __slots__ = ("name", "w", "rs")

    def __init__(self, name=""):
        self.name = name
        self.w = None
        self.rs = []


class Ins:
    __slots__ = ("eng", "fn", "deps", "signal", "tick", "dma", "n", "k")


class Sched:
    def __init__(self, nc):
        self.nc = nc
        self.q = {e: [] for e in ENGS}
        self.ndma = {e: 0 for e in ENGS}
        self.stack = contextlib.ExitStack()
        self.finals = []
        self._nm = 0

    def sb(self, shape, dt=F32, name=None):
        self._nm += 1
        return self.stack.enter_context(self.nc.sbuf_tensor(name or f"sb{self._nm}", list(shape), dt))

    def ps(self, shape, dt=F32, name=None):
        self._nm += 1
        return self.stack.enter_context(self.nc.psum_tensor(name or f"ps{self._nm}", list(shape), dt))

    def dram(self, name, shape, dt=F32, kind="Internal"):
        return self.nc.dram_tensor(name, list(shape), dt, kind=kind)

    def op(self, eng, fn, r=(), w=(), dma=False):
        ins = Ins()
        ins.eng = eng
        ins.fn = fn
        ins.dma = dma
        ins.signal = False
        ins.tick = 0
        deps = {}
        for b in r:
            if b.w is not None:
                deps[id(b.w)] = b.w
        for b in w:
            if b.w is not None:
                deps[id(b.w)] = b.w
            for rd in b.rs:
                deps[id(rd)] = rd
        ins.deps = list(deps.values())
        for b in r:
            if not dma:
                b.rs = [x for x in b.rs if x.dma or x.eng != eng]
            b.rs.append(ins)
        for b in w:
            b.w = ins
            b.rs = []
        ins.k = len(self.q[eng])
        if dma:
            ins.n = self.ndma[eng]
            self.ndma[eng] += 1
        self.q[eng].append(ins)
        return ins

    def dma(self, eng, out, in_, r=(), w=(), final=False):
        ins = self.op(eng, lambda e: e.dma_start(out=out, in_=in_), r=r, w=w, dma=True)
        if final:
            self.finals.append(ins)
        return ins

    def emit(self):
        nc = self.nc
        for e in ENGS:
            for ins in self.q[e]:
                for d in ins.deps:
                    if d.dma:
                        continue
                    if d.eng == ins.eng:
                        if d.eng == "pe" and not ins.dma:
                            continue
                        if ins.dma or (ins.k - d.k) <= 8:
                            d.signal = True
                    else:
                        d.signal = True
        for e in ENGS:
            t = 0
            for ins in self.q[e]:
                if ins.signal and not ins.dma:
                    t += 1
                    ins.tick = t
        st = self.stack
        csem = {e: st.enter_context(nc.semaphore(f"c_{e}")) for e in ENGS}
        dsem = {e: [st.enter_context(nc.semaphore(f"d_{e}{i}")) for i in range(NSLOT)] for e in ("sp", "act", "pool")}
        engobj = {}
        finals = self.finals

        def run(e, eng):
            waited = {}

            def wait(sem, val):
                key = id(sem)
                if waited.get(key, 0) >= val:
                    return
                waited[key] = val
                eng.wait_ge(sem, val)

            for ins in self.q[e]:
                for d in ins.deps:
                    if d.dma:
                        wait(dsem[d.eng][d.n % NSLOT], 16 * (d.n // NSLOT + 1))
                    elif d.signal:
                        if d.eng == e and not ins.dma and (e == "pe" or (ins.k - d.k) > 8):
                            continue
                        wait(csem[d.eng], d.tick)
                if ins.dma:
                    if ins.n >= NSLOT:
                        wait(dsem[e][ins.n % NSLOT], 16 * (ins.n // NSLOT))
                    ins.fn(eng).then_inc(dsem[e][ins.n % NSLOT], 16)
                else:
                    bi = ins.fn(eng)
                    if ins.signal:
                        bi.then_inc(csem[e], 1)
            if e == "sp":
                for qe in ("sp", "act", "pool"):
                    n = self.ndma[qe]
                    for s in range(min(n, NSLOT)):
                        last = ((n - 1 - s) // NSLOT) * NSLOT + s
                        wait(dsem[qe][s], 16 * (last // NSLOT + 1))

        with nc.allow_non_contiguous_dma(reason="small strided scratch DMAs"), nc.Block() as block:
            @block.sync
            def _(eng):
                run("sp", eng)

            @block.scalar
            def _(eng):
                run("act", eng)

            @block.vector
            def _(eng):
                run("dve", eng)

            @block.gpsimd
            def _(eng):
                run("pool", eng)

            @block.tensor
            def _(eng):
                run("pe", eng)
        self.stack.close()


NLAT = 16384
NCTX = 256
NTOK = NLAT + NCTX
DM = 1024
NIN = 3840
NEXP = 16
EDIM = 512
TILES = [(i * 512, 512) for i in range(32)] + [(NLAT, 256)]
SB_BASE = 16512
SB_TOP = 229344


class Mem:
    def __init__(self, S):
        self.S = S
        self.off = SB_BASE
        self.n = 0

    def sb(self, shape, dt=F32, name=None):
        sz = int(np.prod(shape[1:])) * (2 if dt == BF16 else 4)
        sz = (sz + 31) // 32 * 32
        self.n += 1
        t = self.S.nc.alloc_sbuf_tensor_at(f"m{self.n}" + (("_" + name) if name else ""), list(shape), dt, offset=self.off)
        self.off += sz
        assert self.off <= SB_TOP, f"SBUF overflow {self.off}"
        return t

    def mark(self):
        return self.off

    def reset(self, m):
        self.off = m


class RR:
    def __init__(self, mem, shape, dt, n):
        self.t = [mem.sb(shape, dt) for _ in range(n)]
        self.b = [Buf() for _ in range(n)]
        self.i = 0

    def get(self):
        i = self.i
        self.i = (i + 1) % len(self.t)
        return self.t[i], self.b[i]


class PRR:
    def __init__(self, tiles):
        self.t = tiles
        self.b = [Buf() for _ in tiles]
        self.i = 0

    def get(self):
        i = self.i
        self.i = (i + 1) % len(self.t)
        return self.t[i], self.b[i]


def fence(S):
    lasts = []
    for e in ENGS:
        q = S.q[e]
        nd = 0
        seen_c = False
        for ins in reversed(q):
            if ins.dma:
                if nd < NSLOT:
                    lasts.append(ins)
                    nd += 1
            elif not seen_c:
                lasts.append(ins)
                seen_c = True
            if nd >= NSLOT and seen_c:
                break
    fb = Buf()
    for e in ENGS:
        ins = S.op(e, lambda eng: eng.nop(), r=(), w=())
        ins.deps = [d for d in lasts if d is not ins]


def build_layer(layer, rm_patterns, nlat=16384, debug=False, stop=None):
    NLAT = nlat
    NTOK = NLAT + NCTX
    NROWS = NLAT // 64
    TILES = [(i * 512, 512) for i in range(NLAT // 512)] + [(NLAT, 256)]
    nc = bass.Bass("TRN2", target_bir_lowering=False)
    S = Sched(nc)
    mem = Mem(S)
    EI = "ExternalInput"

    class _Stop(Exception):
        pass

    def chk(name):
        if stop == name:
            raise _Stop()


    def din(name, shape, dt=F32):
        return nc.dram_tensor(name, list(shape), dt, kind=EI)

    x_d = din("x", [NLAT, DM])
    cx_d = din("cx", [NCTX, DM])
    c2T_d = din("c2T", [128, 8, 2])
    wmod_d = din("wmod", [DM, 6 * DM])
    bmodT_d = din("bmodT", [128, 48])
    n1T_d = din("n1T", [128, 8])
    n2T_d = din("n2T", [128, 8])
    win_d = din("win", [DM, NIN])
    wout_d = din("wout", [DM, DM])
    wr_d = din("wr", [DM, NEXP])
    hvec_d = din("hvec", [128, 3])
    lbl_d = din("lbl", [128, 3, 2, 4])
    cw_d = din("cw", [128, 2, 3])
    tt_d = din("tt", [128, 6 * 22 * 64])
    rma_d = din("rma", [len(rm_patterns), 128, 128])
    rmb_d = din("rmb", [128, 512])
    cst_d = din("cst", [5, 128, 128])
    cos_d = din("cos", [128, NTOK])
    sin_d = din("sin", [128, NTOK])
    early = stop is not None and (stop.startswith("A") or stop == "pro")
    wg_d = None if early else din("wg", [NEXP, DM, EDIM])
    wu_d = None if early else din("wu", [NEXP, DM, EDIM])
    wd_d = None if early else din("wd", [NEXP, EDIM, DM])
    xo_d = nc.dram_tensor("xo", [NLAT, DM], F32, kind="ExternalOutput")
    cxo_d = nc.dram_tensor("cxo", [NCTX, DM], F32, kind="ExternalOutput")

    def xsrc(t0, n):
        return (x_d, t0) if t0 < NLAT else (cx_d, t0 - NLAT)

    def xdst(t0):
        return (xo_d, t0) if t0 < NLAT else (cxo_d, t0 - NLAT)

    def scr(name, shape, dt=F32):
        if debug and name in ("mix_s", "xm_s", "aff_s", "QT_s", "KT_s", "V_s", "oacc_s", "h2_s"):
            return nc.dram_tensor(name, list(shape), dt, kind="ExternalOutput")
        return nc.dram_tensor(name, list(shape), dt)

    QT_s = scr("QT_s", [3, 2, 128, NTOK], BF16)
    KT_s = scr("KT_s", [3, 128, NTOK], BF16)
    V_s = scr("V_s", [NTOK, 384], BF16)
    mix_s = scr("mix_s", [8, 128, NTOK], BF16)
    oacc_s = scr("oacc_s", [3, 128, NTOK])
    qhat_s = scr("qhat_s", [2, 3, 128, NTOK], BF16)
    U_s = scr("U_s", [2, 3, 128, NTOK // 64, 64])
    D_s = scr("D_s", [2, 3, 128, NTOK // 64])
    gs_s = scr("gs_s", [3, 128, NTOK])
    z_s = scr("z_s", [2, 128, NTOK + 4])
    cb_s = scr("cb_s", [2, 128, NTOK], BF16)
    xm_s = scr("xm_s", [NTOK, DM])
    h2_s = scr("h2_s", [8, 128, NTOK], BF16)
    aff_s = scr("aff_s", [NTOK, NEXP])
    wgb_s = scr("wgb_s", [NEXP, 128, 8, EDIM], BF16)
    wub_s = scr("wub_s", [NEXP, 128, 8, EDIM], BF16)
    wdb_s = scr("wdb_s", [NEXP, 128, 4, DM], BF16)
    dbuf = {}

    def DB(*key):
        if key not in dbuf:
            dbuf[key] = Buf()
        return dbuf[key]

    pfs = [S.ps([128, 512], F32) for _ in range(6)]
    pbs = [S.ps([128, 8, 128], BF16) for _ in range(2)]
    PF = PRR(pfs)
    PB = PRR(pbs)

    def mm(out, lhsT, rhs, st, sp_, r, w):
        return S.op("pe", lambda e: e.matmul(out, lhsT, rhs, start=st, stop=sp_), r=r, w=w)

    def tr(out, in_, idn, r, w):
        return S.op("pe", lambda e: e.transpose(out=out, in_=in_, identity=idn), r=r, w=w)

    def act(out, in_, func, r, w, **kw):
        return S.op("act", lambda e: e.activation(out=out, in_=in_, func=func, **kw), r=r, w=w)

    def tt(eng, out, in0, in1, op, r, w):
        return S.op(eng, lambda e: e.tensor_tensor(out=out, in0=in0, in1=in1, op=op), r=r, w=w)

    def ts(eng, out, in0, s1, s2, op0, op1, r, w):
        if s2 is None:
            return S.op(eng, lambda e: e.tensor_scalar(out=out, in0=in0, scalar1=s1, scalar2=None, op0=op0), r=r, w=w)
        return S.op(eng, lambda e: e.tensor_scalar(out=out, in0=in0, scalar1=s1, scalar2=s2, op0=op0, op1=op1), r=r, w=w)

    def stt(eng, out, in0, sc, in1, op0, op1, r, w):
        return S.op(eng, lambda e: e.scalar_tensor_tensor(out=out, in0=in0, scalar=sc, in1=in1, op0=op0, op1=op1), r=r, w=w)

    def cp(eng, out, in_, r, w):
        return S.op(eng, lambda e: e.tensor_copy(out=out, in_=in_), r=r, w=w)

    def recip(out, in_, r, w):
        return S.op("dve", lambda e: e.reciprocal(out=out, in_=in_), r=r, w=w)

    cstf = mem.sb([128, 5, 128]); b_cstf = Buf()
    S.dma("sp", cstf[:, :, :], cst_d[:, :, :].rearrange("c p n -> p c n"), w=[b_cstf])
    cstb = mem.sb([128, 5, 128], BF16); b_cst = Buf()
    cp("dve", cstb[:, :, :], cstf[:, :, :], [b_cstf], [b_cst])
    idb = cstb[:, 0, :]; blkb = cstb[:, 1, :]; rotb = cstb[:, 2, :]
    idf = cstf[:, 0, :]
    onesf = mem.sb([128, 128]); b_onesf = Buf()
    S.op("pool", lambda e: e.memset(onesf[:, :], 1.0), w=[b_onesf])
    onesb = mem.sb([128, 128], BF16); b_onesb = Buf()
    S.op("pool", lambda e: e.memset(onesb[:, :], 1.0), w=[b_onesb])

    modT = mem.sb([128, 48, 2]); b_mod = Buf()
    c2T = mem.sb([128, 8, 2]); b_c2 = Buf()
    S.dma("sp", c2T[:, :, :], c2T_d[:, :, :], w=[b_c2])
    scT = mem.sb([128, 8, 2]); b_sc = Buf()
    act(scT[:, :, :], c2T[:, :, :], AF.Silu, [b_c2], [b_sc])
    bmodT = mem.sb([128, 48]); b_bm = Buf()
    S.dma("sp", bmodT[:, :], bmodT_d[:, :], w=[b_bm])
    m0 = mem.mark()
    wmp = RR(mem, [128, 8, 1024], F32, 2)
    pm, bpm = PF.get()
    for m in range(6):
        wt, bw = wmp.get()
        for k in range(8):
            S.dma("sp" if k % 2 == 0 else "pool", wt[:, k, :], wmod_d[k * 128:(k + 1) * 128, m * 1024:(m + 1) * 1024], w=[bw])
        for oc in range(8):
            for k in range(8):
                mm(pm[:, (m * 8 + oc) * 2:(m * 8 + oc) * 2 + 2], wt[:, k, oc * 128:(oc + 1) * 128], scT[:, k, :], k == 0, k == 7, [bw, b_sc], [bpm])
    tt("dve", modT[:, :, :], pm[:, 0:96].rearrange("p (a r) -> p a r", r=2), bmodT[:, :].unsqueeze(2).to_broadcast([128, 48, 2]), ALU.add, [bpm, b_bm], [b_mod])
    mem.reset(m0)
    fence(S)
    n1T = mem.sb([128, 8]); n2T = mem.sb([128, 8]); b_n = Buf()
    S.dma("sp", n1T[:, :], n1T_d[:, :], w=[b_n])
    S.dma("sp", n2T[:, :], n2T_d[:, :], w=[b_n])
    A1 = mem.sb([128, 8, 2]); A2 = mem.sb([128, 8, 2]); b_A = Buf()
    for (A, nT, ms) in ((A1, n1T, 1), (A2, n2T, 4)):
        ts("dve", A[:, :, :], modT[:, ms * 8:(ms + 1) * 8, :], 1.0, None, ALU.add, None, [b_mod], [b_A])
        tt("dve", A[:, :, :], A[:, :, :], nT[:, :].unsqueeze(2).to_broadcast([128, 8, 2]), ALU.mult, [b_A, b_n], [b_A])

    def Bm(ms, k, r):
        return modT[:, ms * 8 + k, r:r + 1]

    G1 = mem.sb([128, 2, 1024]); G2 = mem.sb([128, 2, 1024]); b_G = Buf()
    dg = mem.sb([128, 128]); b_dg = Buf()
    for (G, ms) in ((G1, 2), (G2, 5)):
        for r in range(2):
            for half in range(2):
                pg, bpg = PF.get()
                for kk in range(4):
                    k = half * 4 + kk
                    ts("dve", dg[:, :], idf, modT[:, ms * 8 + k, r:r + 1], None, ALU.mult, None, [b_cstf, b_mod], [b_dg])
                    mm(pg[:, kk * 128:(kk + 1) * 128], onesf[:, :], dg[:, :], True, True, [b_onesf, b_dg], [bpg])
                cp("dve", G[:, r, half * 512:(half + 1) * 512], pg[:, :], [bpg], [b_G])
    hvec = mem.sb([128, 3]); b_hv = Buf()
    S.dma("sp", hvec[:, :], hvec_d[:, :], w=[b_hv])
    qw8 = mem.sb([128, 1])
    ts("dve", qw8[:, :], hvec[:, 0:1], 0.125, None, ALU.mult, None, [b_hv], [b_hv])
    lbl = mem.sb([128, 3, 2, 4]); b_lb = Buf()
    S.dma("sp", lbl[:, :, :, :], lbl_d[:, :, :, :], w=[b_lb])
    lmx = mem.sb([128, 3, 2]); lsum = mem.sb([128, 3, 2]); lbv = mem.sb([128, 3, 2]); omlb = mem.sb([128, 3, 2])
    S.op("dve", lambda e: e.tensor_reduce(out=lmx[:, :, :], in_=lbl[:, :, :, :], axis=AX.X, op=ALU.max), r=[b_lb], w=[b_lb])
    tt("dve", lbl[:, :, :, :], lbl[:, :, :, :], lmx[:, :, :].unsqueeze(3).to_broadcast([128, 3, 2, 4]), ALU.subtract, [b_lb], [b_lb])
    act(lbl[:, :, :, :], lbl[:, :, :, :], AF.Exp, [b_lb], [b_lb])
    S.op("dve", lambda e: e.tensor_reduce(out=lsum[:, :, :], in_=lbl[:, :, :, :], axis=AX.X, op=ALU.add), r=[b_lb], w=[b_lb])
    recip(lsum[:, :, :], lsum[:, :, :], [b_lb], [b_lb])
    S.op("dve", lambda e: e.memset(lbv[:, :, :], 0.0), r=[b_lb], w=[b_lb])
    for jl in range(1, layer + 1):
        tt("dve", lbv[:, :, :], lbv[:, :, :], lbl[:, :, :, jl], ALU.add, [b_lb], [b_lb])
    tt("dve", lbv[:, :, :], lbv[:, :, :], lsum[:, :, :], ALU.mult, [b_lb], [b_lb])
    ts("dve", omlb[:, :, :], lbv[:, :, :], -1.0, 1.0, ALU.mult, ALU.add, [b_lb], [b_lb])
    cw = mem.sb([128, 2, 3]); b_cw = Buf()
    S.dma("sp", cw[:, :, :], cw_d[:, :, :], w=[b_cw])
    PBASE = mem.mark()

    if stop == "pro":
        S.emit()
        return nc
    def norm_tile(src_d, row0, r, A, ms_shift, xt, bx, sm, hT, bhT, col0, fp32_out=None):
        junk, bj, ss, rt, rs_, xn, bxn, bsm = sm
        act(junk[:, :], xt[:, :], AF.Square, [bx], [bj, bsm], accum_out=ss[:, :])
        act(rt[:, :], ss[:, :], AF.Sqrt, [bsm], [bsm], scale=1.0 / DM, bias=1e-6)
        recip(rs_[:, :], rt[:, :], [bsm], [bsm])
        act(xn[:, :], xt[:, :], AF.Copy, [bx, bsm], [bxn], scale=rs_[:, 0:1])
        pt, bpt = PB.get()
        for k in range(8):
            tr(pt[:, k, :], xn[:, k * 128:(k + 1) * 128], idb, [bxn, b_cst], [bpt])
        for k in range(8):
            if k % 2 == 0:
                ts("dve", hT[:, k, col0:col0 + 128], pt[:, k, :], A[:, k, r:r + 1], Bm(ms_shift, k, r), ALU.mult, ALU.add, [bpt, b_A, b_mod], [bhT])
            else:
                act(hT[:, k, col0:col0 + 128], pt[:, k, :], AF.Identity, [bpt, b_A, b_mod], [bhT], scale=A[:, k, r:r + 1], bias=Bm(ms_shift, k, r))

    def norm_scratch(mem):
        junk = mem.sb([128, 1024], BF16)
        ss = mem.sb([128, 1]); rt = mem.sb([128, 1]); rs_ = mem.sb([128, 1])
        xn = mem.sb([128, 1024], BF16)
        return (junk, Buf(), ss, rt, rs_, xn, Buf(), Buf())

    Wb = mem.sb([128, 8, NIN], BF16); b_W = Buf()
    stg = RR(mem, [128, 1920], F32, 2)
    for k in range(8):
        for hf in range(2):
            st_, bst = stg.get()
            S.dma("sp" if hf == 0 else "pool", st_[:, :], win_d[k * 128:(k + 1) * 128, hf * 1920:(hf + 1) * 1920], w=[bst])
            cp("pool" if hf == 0 else "dve", Wb[:, k, hf * 1920:(hf + 1) * 1920], st_[:, :], [bst], [b_W])
    xtp = RR(mem, [128, 1024], F32, 2)
    nsm = norm_scratch(mem)
    hTp = RR(mem, [128, 8, 512], BF16, 2)
    cosp = RR(mem, [128, 512], F32, 2)
    sinp = RR(mem, [128, 512], F32, 2)
    zero_t = mem.sb([128, 512], BF16); b_zero = Buf()
    S.op("pool", lambda e: e.memset(zero_t[:, :], 0.0), w=[b_zero])
    zero_f = mem.sb([128, 4]); b_zf = Buf()
    S.op("pool", lambda e: e.memset(zero_f[:, :], 0.0), w=[b_zf])
    ones64 = mem.sb([128, 64]); b_o64 = Buf()
    S.op("pool", lambda e: e.memset(ones64[:, :], 1.0), w=[b_o64])
    for cc in range(2):
        for col in (0, NLAT + 1, NLAT + 2, NTOK + 3):
            S.dma("pool", z_s[cc, :, col:col + 1], zero_f[:, 0:1], r=[b_zf], w=[DB("z", cc, "pad", col)])

    def T512(dt=F32, name=None):
        return mem.sb([128, 512], dt, name), Buf()

    sq_t, b_sq = T512(BF16, name="sq_t")
    rt_t, b_rt = T512(name="rt_t")
    qn_t, b_qn = T512(BF16, name="qn_t")
    t1_t, b_t1 = T512(name="t1_t")
    t2_t, b_t2 = T512(name="t2_t")
    qo_t, b_qo = T512(BF16, name="qo_t")
    vt_p = RR(mem, [128, 384], BF16, 2)
    itm = mem.sb([128, 4, 384], BF16); b_itm = Buf()
    hq_t, b_hq = T512(name="hq_t")
    sg_t, b_sg = T512(name="sg_t")
    lf_t, b_lf = T512(name="lf_t")
    kk_t, b_kk = T512(name="kk_t")
    A_t, b_At = T512(name="A_t")
    a_t, b_a = T512(name="a_t")
    d_t, b_d = T512(name="d_t")
    e_t, b_e = T512(name="e_t")
    e2_t, b_e2 = T512(name="e2_t")
    qtl_t, b_qtl = T512(BF16, name="qtl_t")
    ktl_t, b_ktl = T512(BF16, name="ktl_t")
    qh_t, b_qh = T512(BF16, name="qh_t")
    kh_t, b_kh = T512(BF16, name="kh_t")
    khtm = mem.sb([128, 4, 128], BF16); b_khtm = Buf()
    Tt = mem.sb([128, 8]); rr_t = mem.sb([128, 16]); Dt = mem.sb([128, 8]); bn_t = mem.sb([128, 8]); b_T = Buf(); b_rr = Buf(); b_D = Buf(); b_bn = Buf()
    kz = [mem.sb([128, 512], BF16, "kzf"), mem.sb([128, 512], BF16, "kzb")]
    b_kz = [Buf(), Buf()]
    for i_ in range(2):
        S.op("pool", lambda e, t=kz[i_]: e.memset(t[:, :], 0.0), w=[b_kz[i_]])
    scm = mem.sb([128, 128], BF16); b_scm = Buf()
    Ut = mem.sb([128, 8, 64]); b_U = Buf()
    osb, b_osb = T512()
    gs_t, b_gs = T512()
    cs_t, b_cs = T512()
    cb_t, b_cb = T512(BF16)
    po_ps, b_po = [pfs[2], pfs[3]], [Buf(), Buf()]
    pu_ps, b_pu = [pfs[4], pfs[5]], [Buf(), Buf()]
    PF4 = PRR(pfs[0:2])

    def do_tile(t0, n):
        isc = t0 >= NLAT
        r = 1 if isc else 0
        nsub = n // 128
        nch = n // 64
        c0 = t0 // 64
        hT, bhT = hTp.get()
        src, so = xsrc(t0, n)
        for sub in range(nsub):
            xt, bx = xtp.get()
            S.dma("sp", xt[:, :], src[so + sub * 128: so + (sub + 1) * 128, :], w=[bx])
            norm_tile(src, so, r, A1, 0, xt, bx, nsm, hT, bhT, sub * 128)
        cs, bcs = cosp.get()
        sn, bsn = sinp.get()
        S.dma("pool", cs[:, 0:n], cos_d[:, t0:t0 + n], w=[bcs])
        S.dma("pool", sn[:, 0:n], sin_d[:, t0:t0 + n], w=[bsn])

        def proj(c0_):
            pf, bpf = PF4.get()
            for k in range(8):
                mm(pf[:, 0:n], Wb[:, k, c0_:c0_ + 128], hT[:, k, 0:n], k == 0, k == 7, [b_W, bhT], [bpf])
            return pf, bpf

        chk("A1")
        for which in range(2):
            for pr in range(3):
                pf, bpf = proj(which * 384 + pr * 128)
                act(sq_t[:, 0:n], pf[:, 0:n], AF.Square, [bpf], [b_sq])
                pss, bpss = PF4.get()
                mm(pss[:, 0:n], blkb, sq_t[:, 0:n], True, True, [b_cst, b_sq], [bpss])
                act(rt_t[:, 0:n], pss[:, 0:n], AF.Sqrt, [bpss], [b_rt], scale=1.0 / 64, bias=1e-6)
                recip(rt_t[:, 0:n], rt_t[:, 0:n], [b_rt], [b_rt])
                wv = qw8[:, 0:1] if which == 0 else hvec[:, 1:2]
                stt("dve", qn_t[:, 0:n], pf[:, 0:n], wv, rt_t[:, 0:n], ALU.mult, ALU.mult, [bpf, b_rt, b_hv], [b_qn])
                prot, bprot = PF4.get()
                mm(prot[:, 0:n], rotb, qn_t[:, 0:n], True, True, [b_cst, b_qn], [bprot])
                tt("pool", t1_t[:, 0:n], qn_t[:, 0:n], cs[:, 0:n], ALU.mult, [b_qn, bcs], [b_t1])
                tt("dve", t2_t[:, 0:n], prot[:, 0:n], sn[:, 0:n], ALU.mult, [bprot, bsn], [b_t2])
                tt("pool", qo_t[:, 0:n], t1_t[:, 0:n], t2_t[:, 0:n], ALU.add, [b_t1, b_t2], [b_qo])
                if which == 0:
                    for hh in range(2):
                        oh = 1 - hh
                        S.dma("sp", QT_s[pr, hh, hh * 64:(hh + 1) * 64, t0:t0 + n], qo_t[hh * 64:(hh + 1) * 64, 0:n], r=[b_qo], w=[DB("Q", pr, hh, t0)])
                        S.dma("pool", QT_s[pr, hh, oh * 64:(oh + 1) * 64, t0:t0 + n], zero_t[oh * 64:(oh + 1) * 64, 0:n], r=[b_zero], w=[DB("Qz", pr, hh, t0)])
                else:
                    S.dma("sp", KT_s[pr, :, t0:t0 + n], qo_t[:, 0:n], r=[b_qo], w=[DB("K", pr, t0)])
        chk("A2")
        for sub in range(nsub):
            pv, bpv = PF4.get()
            for k in range(8):
                mm(pv[:, 0:384], hT[:, k, sub * 128:(sub + 1) * 128], Wb[:, k, 768:1152], k == 0, k == 7, [bhT, b_W], [bpv])
            vt, bvt = vt_p.get()
            act(vt[:, :], pv[:, 0:384], AF.Copy, [bpv], [bvt])
            S.dma("sp", V_s[t0 + sub * 128:t0 + (sub + 1) * 128, :], vt[:, :], r=[bvt], w=[DB("V", t0, sub)])
            pi, bpi = PF4.get()
            for k in range(8):
                mm(pi[:, 0:384], hT[:, k, sub * 128:(sub + 1) * 128], Wb[:, k, 2304:2688], k == 0, k == 7, [bhT, b_W], [bpi])
            cp("dve", itm[:, sub, :], pi[:, 0:384], [bpi], [b_itm])
        chk("A3")
        for hp in range(3):
            pf, bpf = proj(1152 + hp * 128)
            act(hq_t[:, 0:n], pf[:, 0:n], AF.Copy, [bpf], [b_hq], scale=0.125)
            pf, bpf = proj(2688 + hp * 128)
            act(gs_t[:, 0:n], pf[:, 0:n], AF.Silu, [bpf], [b_gs])
            S.dma("pool", gs_s[hp, :, t0:t0 + n], gs_t[:, 0:n], r=[b_gs], w=[DB("gs", hp, t0)])
            for dr in range(2):
                pf, bpf = proj(1536 + dr * 384 + hp * 128)
                act(sg_t[:, 0:n], pf[:, 0:n], AF.Sigmoid, [bpf], [b_sg])
                ts("dve", sg_t[:, 0:n], sg_t[:, 0:n], omlb[:, hp, dr:dr + 1], lbv[:, hp, dr:dr + 1], ALU.mult, ALU.add, [b_sg, b_lb], [b_sg])
                act(lf_t[:, 0:n], sg_t[:, 0:n], AF.Ln, [b_sg], [b_lf])
                ts("pool", kk_t[:, 0:n], sg_t[:, 0:n], -1.0, 1.0, ALU.mult, ALU.add, [b_sg], [b_kk])
                for c in range(nch):
                    S.op("dve", lambda e, c=c: e.tensor_tensor_scan(out=A_t[:, c * 64:(c + 1) * 64], data0=ones64[:, :], data1=lf_t[:, c * 64:(c + 1) * 64], initial=0.0, op0=ALU.mult, op1=ALU.add), r=[b_lf, b_o64], w=[b_At])
                A3 = A_t[:, 0:n].rearrange("p (c s) -> p c s", s=64)
                cp("dve", Tt[:, 0:nch], A3[:, :, 63], [b_At], [b_T])
                Tb = Tt[:, 0:nch].unsqueeze(2).to_broadcast([128, nch, 64])
                a3 = a_t[:, 0:n].rearrange("p (c s) -> p c s", s=64)
                if dr == 0:
                    cp("pool", a_t[:, 0:n], A_t[:, 0:n], [b_At], [b_a])
                else:
                    tt("pool", a_t[:, 0:n], lf_t[:, 0:n], A_t[:, 0:n], ALU.subtract, [b_lf, b_At], [b_a])
                    tt("pool", a3, a3, Tb, ALU.add, [b_a, b_T], [b_a])
                d3 = d_t[:, 0:n].rearrange("p (c s) -> p c s", s=64)
                cp("dve", bn_t[:, 0:nch], a3[:, :, 31 if dr == 0 else 32], [b_a], [b_bn])
                tt("pool", d3, a3, bn_t[:, 0:nch].unsqueeze(2).to_broadcast([128, nch, 64]), ALU.subtract, [b_a, b_bn], [b_d])
                act(e_t[:, 0:n], d_t[:, 0:n], AF.Exp, [b_d], [b_e])
                tt("dve", qtl_t[:, 0:n], hq_t[:, 0:n], e_t[:, 0:n], ALU.mult, [b_hq, b_e], [b_qtl])
                act(e2_t[:, 0:n], d_t[:, 0:n], AF.Exp, [b_d], [b_e2], scale=-1.0)
                tt("pool", ktl_t[:, 0:n], kk_t[:, 0:n], e2_t[:, 0:n], ALU.mult, [b_kk, b_e2], [b_ktl])
                hsl = slice(0, 32) if dr == 0 else slice(32, 64)
                cp("pool", kz[dr][:, 0:n].rearrange("p (c s) -> p c s", s=64)[:, :, hsl], ktl_t[:, 0:n].rearrange("p (c s) -> p c s", s=64)[:, :, hsl], [b_ktl], [b_kz[dr]])
                act(e_t[:, 0:n], a_t[:, 0:n], AF.Exp, [b_a], [b_e])
                tt("dve", qh_t[:, 0:n], hq_t[:, 0:n], e_t[:, 0:n], ALU.mult, [b_hq, b_e], [b_qh])
                S.dma("sp", qhat_s[dr, hp, :, t0:t0 + n], qh_t[:, 0:n], r=[b_qh], w=[DB("qh", dr, hp, t0)])
                tt("pool", d3, Tb, a3, ALU.subtract, [b_a, b_T], [b_d])
                act(e2_t[:, 0:n], d_t[:, 0:n], AF.Exp, [b_d], [b_e2])
                tt("pool", kh_t[:, 0:n], kk_t[:, 0:n], e2_t[:, 0:n], ALU.mult, [b_kk, b_e2], [b_kh])
                act(Dt[:, 0:nch], Tt[:, 0:nch], AF.Exp, [b_T], [b_D])
                S.dma("pool", D_s[dr, hp, :, c0:c0 + nch], Dt[:, 0:nch], r=[b_D], w=[DB("D", dr, hp, t0)])
                chk("A4")
                pt, bpt = PB.get()
                for sub in range(nsub):
                    tr(pt[:, sub, :], kh_t[:, sub * 128:(sub + 1) * 128], idb, [b_kh, b_cst], [bpt])
                act(khtm[:, 0:nsub, :], pt[:, 0:nsub, :], AF.Copy, [bpt], [b_khtm])
                mk = cstb[:, 3 + dr, :]
                for sub in range(nsub):
                    pscs = [PF4.get(), PF4.get()]
                    for c_ in range(2):
                        tb = sub * 128 + c_ * 64
                        rows = slice(c_ * 64, (c_ + 1) * 64)
                        for hh in range(2):
                            hs = slice(hh * 64, (hh + 1) * 64)
                            psc, bpsc = pscs[hh]
                            tfull, tz = (1, 0) if dr == 0 else (0, 1)
                            mm(psc[rows, tfull * 32:(tfull + 1) * 32], ktl_t[hs, tb:tb + 64], qtl_t[hs, tb + tfull * 32:tb + (tfull + 1) * 32], True, True, [b_ktl, b_qtl], [bpsc])
                            mm(psc[rows, tz * 32:(tz + 1) * 32], kz[dr][hs, tb:tb + 64], qtl_t[hs, tb + tz * 32:tb + (tz + 1) * 32], True, True, [b_kz[dr], b_qtl], [bpsc])
                    for hh in range(2):
                        psc, bpsc = pscs[hh]
                        tt("dve", scm[:, hh * 64:(hh + 1) * 64], psc[:, 0:64], mk[:, 0:64], ALU.mult, [bpsc, b_cst], [b_scm])
                    for c_ in range(2):
                        ps_ = slice(c_ * 64, (c_ + 1) * 64)
                        for hh in range(2):
                            hs = slice(hh * 64, (hh + 1) * 64)
                            vcol = slice((hp * 2 + hh) * 64, (hp * 2 + hh + 1) * 64)
                            mm(pu_ps[c_][hs, sub * 64:(sub + 1) * 64], khtm[ps_, sub, hs], itm[ps_, sub, vcol], True, True, [b_khtm, b_itm], [b_pu[c_]])
                            mm(po_ps[c_][hs, sub * 64:(sub + 1) * 64], itm[ps_, sub, vcol], scm[ps_, hs], True, True, [b_itm, b_scm], [b_po[c_]])
                for c_ in range(2):
                    cp("dve", Ut[:, 0:nch, :].rearrange("p (s c) d -> p s c d", c=2)[:, :, c_, :], pu_ps[c_][:, 0:nsub * 64].rearrange("p (s d) -> p s d", d=64), [b_pu[c_]], [b_U])
                S.dma("sp", U_s[dr, hp, :, c0:c0 + nch, :], Ut[:, 0:nch, :], r=[b_U], w=[DB("U", dr, hp, t0)])
                for c_ in range(2):
                    ov = osb[:, 0:n].rearrange("p (s c t) -> p s c t", c=2, t=64)[:, :, c_, :]
                    pv_ = po_ps[c_][:, 0:nsub * 64].rearrange("p (s t) -> p s t", t=64)
                    if dr == 0:
                        act(ov, pv_, AF.Copy, [b_po[c_]], [b_osb])
                    else:
                        tt("dve", ov, ov, pv_, ALU.add, [b_osb, b_po[c_]], [b_osb])
            S.dma("sp", oacc_s[hp, :, t0:t0 + n], osb[:, 0:n], r=[b_osb], w=[DB("oacc", hp, t0)])
        chk("A5")
        zoff = t0 + 3 if isc else t0 + 1
        for cc in range(2):
            pf, bpf = proj(3328 + cc * 128)
            act(cs_t[:, 0:n], pf[:, 0:n], AF.Copy, [bpf], [b_cs])
            pf, bpf = proj(3584 + cc * 128)
            tt("dve", cs_t[:, 0:n], cs_t[:, 0:n], pf[:, 0:n], ALU.mult, [b_cs, bpf], [b_cs])
            S.dma("pool", z_s[cc, :, zoff:zoff + n], cs_t[:, 0:n], r=[b_cs], w=[DB("z", cc, t0)])
            pf, bpf = proj(3072 + cc * 128)
            act(cb_t[:, 0:n], pf[:, 0:n], AF.Copy, [bpf], [b_cb])
            S.dma("pool", cb_s[cc, :, t0:t0 + n], cb_t[:, 0:n], r=[b_cb], w=[DB("cb", cc, t0)])
    try:
        for (t0_, n_) in (TILES[:1] if (stop or "").startswith("A") and stop != "A" else TILES):
            do_tile(t0_, n_)
    except _Stop:
        S.emit()
        return nc
    fence(S)
    mem.reset(PBASE)

    if stop == "A":
        S.emit()
        return nc
    NP_ = len(rm_patterns)
    ttb = mem.sb([128, 6 * 22 * 64], BF16); b_tt = Buf()
    stg2 = RR(mem, [128, 2112], F32, 2)
    for i4 in range(4):
        st_, bst = stg2.get()
        S.dma("sp", st_[:, :], tt_d[:, i4 * 2112:(i4 + 1) * 2112], w=[bst])
        cp("dve", ttb[:, i4 * 2112:(i4 + 1) * 2112], st_[:, :], [bst], [b_tt])
    rmaf = mem.sb([128, NP_, 128]); rmab = mem.sb([128, NP_, 128], BF16); rmbf = mem.sb([128, 512]); rmbb = mem.sb([128, 512], BF16); b_rm = Buf()
    S.dma("sp", rmaf[:, :, :], rma_d[:, :, :].rearrange("n k m -> k n m"), w=[b_rm])
    S.dma("sp", rmbf[:, :], rmb_d[:, :], w=[b_rm])
    cp("dve", rmab[:, :, :], rmaf[:, :, :], [b_rm], [b_rm])
    cp("dve", rmbb[:, :], rmbf[:, :], [b_rm], [b_rm])
    kcx = mem.sb([128, 3, 256], BF16); vcx = mem.sb([128, 2, 384], BF16); b_cxkv = Buf()
    S.dma("sp", kcx[:, :, :], KT_s[:, :, NLAT:NTOK].rearrange("c p t -> p c t"), r=[DB("K", pr, NLAT) for pr in range(3)], w=[b_cxkv])
    S.dma("sp", vcx[:, :, :], V_s[NLAT:NTOK, :].rearrange("(s t) c -> t s c", t=128), r=[DB("V", NLAT, s_) for s_ in range(2)], w=[b_cxkv])
    qp = RR(mem, [128, 6, 512], BF16, 2)
    kp = RR(mem, [128, 3, 1024], BF16, 2)
    vp = RR(mem, [128, 8, 384], BF16, 2)
    ptp = RR(mem, [128, 512], BF16, 3)
    rin_t, b_rin = T512()
    ob_p = RR(mem, [128, 512], BF16, 2)
    PS2 = PRR(pfs[0:2])
    pos = PRR(pfs[2:4])
    pds = PRR(pfs[4:6])

    def rowpat(g, p):
        out = []
        for a in range(2):
            for i in range(8):
                kr = 8 * g - 4 + 2 * p + a
                rq = 8 * g + i
                rs0 = min(max(rq - 4, 0), NROWS - 8)
                out.append(rs0 <= kr < rs0 + 8)
        return tuple(out)

    for (t0, n) in TILES:
        isc = t0 >= NLAT
        g = t0 // 512
        qt, bq = qp.get()
        S.dma("sp", qt[:, :, 0:n], QT_s[:, :, :, t0:t0 + n].rearrange("c h p t -> p (c h) t"),
              r=[DB(nm, pr, hh, t0) for nm in ("Q", "Qz") for pr in range(3) for hh in range(2)], w=[bq])
        chunks = []
        if not isc:
            ps_valid = [p for p in range(8) if 0 <= 8 * g - 4 + 2 * p < NROWS]
            p_lo, p_hi = ps_valid[0], ps_valid[-1]
            k0 = (8 * g - 4) * 64
            kt, bk = kp.get()
            vt_, bv = vp.get()
            tl, th = k0 + p_lo * 128, k0 + (p_hi + 1) * 128
            tiles_touched = sorted(set([(tl // 512) * 512, ((th - 1) // 512) * 512, (((tl + th) // 2) // 512) * 512]))
            S.dma("pool", kt[:, :, p_lo * 128:(p_hi + 1) * 128], KT_s[:, :, tl:th].rearrange("c p t -> p c t"),
                  r=[DB("K", pr, tt0) for pr in range(3) for tt0 in tiles_touched], w=[bk])
            S.dma("pool", vt_[:, p_lo:p_hi + 1, :], V_s[tl:th, :].rearrange("(s t) c -> t s c", t=128),
                  r=[DB("V", tt0, s_) for tt0 in tiles_touched for s_ in range(4)], w=[bv])
            chunks = [("loc", p) for p in ps_valid]
        chunks += [("ctx", 0), ("ctx", 1)]
        for pr in range(3):
            po_, bpo = pos.get()
            pd_, bpd = pds.get()
            for hh in range(2):
                h = pr * 2 + hh
                hs = slice(hh * 64, (hh + 1) * 64)
                for ci, (kind, p) in enumerate(chunks):
                    st_ps, bst_ps = PS2.get()
                    first, last = ci == 0, ci == len(chunks) - 1
                    if kind == "loc":
                        mm(st_ps[:, 0:n], kt[:, pr, p * 128:(p + 1) * 128], qt[:, h, 0:n], True, False, [bk, bq], [bst_ps])
                        u0 = 14 - 2 * p
                        mm(st_ps[:, 0:n], idb, ttb[:, (h * 22 + u0) * 64:(h * 22 + u0 + 8) * 64], False, False, [b_cst, b_tt], [bst_ps])
                        pat = rm_patterns.index(rowpat(g, p))
                        mm(st_ps[:, 0:n], rmab[:, pat, :], rmbb[:, :], False, True, [b_rm], [bst_ps])
                        vl = vt_[:, p, h * 64:(h + 1) * 64]
                        rv = [bv]
                    else:
                        mm(st_ps[:, 0:n], kcx[:, pr, p * 128:(p + 1) * 128], qt[:, h, 0:n], True, True, [b_cxkv, bq], [bst_ps])
                        vl = vcx[:, p, h * 64:(h + 1) * 64]
                        rv = [b_cxkv]
                    pT, bpT = ptp.get()
                    act(pT[:, 0:n], st_ps[:, 0:n], AF.Exp, [bst_ps], [bpT])
                    mm(po_[hs, 0:n], vl, pT[:, 0:n], first, last, rv + [bpT], [bpo])
                    mm(pd_[hs, 0:n], onesb[:, 0:64], pT[:, 0:n], first, last, [b_onesb, bpT], [bpd])
            recip(rin_t[:, 0:n], pd_[:, 0:n], [bpd], [b_rin])
            ob, bob = ob_p.get()
            tt("dve", ob[:, 0:n], po_[:, 0:n], rin_t[:, 0:n], ALU.mult, [bpo, b_rin], [bob])
            S.dma("sp", mix_s[pr, :, t0:t0 + n], ob[:, 0:n], r=[bob], w=[DB("mix", pr, t0)])
    fence(S)
    mem.reset(PBASE)

    if stop == "B":
        S.emit()
        return nc
    S32 = [[mem.sb([128, 64]) for _ in range(3)] for _ in range(2)]
    Sbf = [[mem.sb([128, 64], BF16) for _ in range(3)] for _ in range(2)]
    bS = [[Buf() for _ in range(3)] for _ in range(2)]
    bSb = [[Buf() for _ in range(3)] for _ in range(2)]
    qh_p = RR(mem, [128, 3, 512], BF16, 2)
    U_p = RR(mem, [128, 3, 8, 64], F32, 2)
    D_p = RR(mem, [128, 3, 8], F32, 2)
    oa_p = RR(mem, [128, 3, 512], F32, 2)
    gsl_p = RR(mem, [128, 3, 512], F32, 2)
    sq2, b_sq2 = T512(BF16)
    rt2, b_rt2 = T512()
    y2, b_y2 = T512()
    yb_p = RR(mem, [128, 512], BF16, 2)
    pin = [pfs[0], pfs[1]]
    bpin = [Buf(), Buf()]
    PSX = PRR(pfs[2:6])
    for dr in range(2):
        for hp in range(3):
            S.op("pool", lambda e, t=S32[dr][hp]: e.memset(t[:, :], 0.0), w=[bS[dr][hp]])
            S.op("pool", lambda e, t=Sbf[dr][hp]: e.memset(t[:, :], 0.0), w=[bSb[dr][hp]])
        order = [TILES[-1]] + (TILES[:-1] if dr == 0 else TILES[:-1][::-1])
        for (t0, n) in order:
            nch = n // 64
            c0 = t0 // 64
            qh, bqh = qh_p.get()
            Uu, bUu = U_p.get()
            Dd, bDd = D_p.get()
            oa, boa = oa_p.get()
            S.dma("sp", qh[:, :, 0:n], qhat_s[dr, :, :, t0:t0 + n].rearrange("c p t -> p c t"), r=[DB("qh", dr, hp, t0) for hp in range(3)], w=[bqh])
            S.dma("pool", Uu[:, :, 0:nch, :], U_s[dr, :, :, c0:c0 + nch, :].rearrange("c p a b -> p c a b"), r=[DB("U", dr, hp, t0) for hp in range(3)], w=[bUu])
            S.dma("pool", Dd[:, :, 0:nch], D_s[dr, :, :, c0:c0 + nch].rearrange("c p a -> p c a"), r=[DB("D", dr, hp, t0) for hp in range(3)], w=[bDd])
            S.dma("sp", oa[:, :, 0:n], oacc_s[:, :, t0:t0 + n].rearrange("c p t -> p c t"), r=[DB("oacc", hp, t0) for hp in range(3)], w=[boa])
            if dr == 1:
                gl, bgl = gsl_p.get()
                S.dma("sp", gl[:, :, 0:n], gs_s[:, :, t0:t0 + n].rearrange("c p t -> p c t"), r=[DB("gs", hp, t0) for hp in range(3)], w=[bgl])
            corder = list(range(nch)) if dr == 0 else list(range(nch))[::-1]
            for hp in range(3):
                for c in corder:
                    for hh in range(2):
                        hs = slice(hh * 64, (hh + 1) * 64)
                        mm(pin[hh][hs, c * 64:(c + 1) * 64], Sbf[dr][hp][hs, :], qh[hs, hp, c * 64:(c + 1) * 64], True, True, [bSb[dr][hp], bqh], [bpin[hh]])
                    stt("dve", S32[dr][hp][:, :], S32[dr][hp][:, :], Dd[:, hp, c:c + 1], Uu[:, hp, c, :], ALU.mult, ALU.add, [bS[dr][hp], bDd, bUu], [bS[dr][hp]])
                    cp("pool", Sbf[dr][hp][:, :], S32[dr][hp][:, :], [bS[dr][hp]], [bSb[dr][hp]])
                for hh in range(2):
                    hs = slice(hh * 64, (hh + 1) * 64)
                    tt("dve", oa[hs, hp, 0:n], oa[hs, hp, 0:n], pin[hh][hs, 0:n], ALU.add, [boa, bpin[hh]], [boa])
            if dr == 0:
                S.dma("sp", oacc_s[:, :, t0:t0 + n].rearrange("c p t -> p c t"), oa[:, :, 0:n], r=[boa], w=[DB("oacc", hp, t0) for hp in range(3)])
            else:
                for hp in range(3):
                    act(sq2[:, 0:n], oa[:, hp, 0:n], AF.Square, [boa], [b_sq2])
                    pss, bpss = PSX.get()
                    mm(pss[:, 0:n], blkb, sq2[:, 0:n], True, True, [b_cst, b_sq2], [bpss])
                    act(rt2[:, 0:n], pss[:, 0:n], AF.Sqrt, [bpss], [b_rt2], scale=1.0 / 64, bias=1e-6)
                    recip(rt2[:, 0:n], rt2[:, 0:n], [b_rt2], [b_rt2])
                    stt("dve", y2[:, 0:n], oa[:, hp, 0:n], hvec[:, 2:3], rt2[:, 0:n], ALU.mult, ALU.mult, [boa, b_rt2, b_hv], [b_y2])
                    yb, byb = yb_p.get()
                    tt("pool", yb[:, 0:n], y2[:, 0:n], gl[:, hp, 0:n], ALU.mult, [b_y2, bgl], [byb])
                    S.dma("sp", mix_s[3 + hp, :, t0:t0 + n], yb[:, 0:n], r=[byb], w=[DB("mix", 3 + hp, t0)])
    fence(S)
    mem.reset(PBASE)

    if stop == "C":
        S.emit()
        return nc
    zw_p = RR(mem, [128, 514], F32, 2)
    cbl_p = RR(mem, [128, 512], BF16, 2)
    y3, b_y3 = T512()
    yc_p = RR(mem, [128, 512], BF16, 2)
    for (t0, n) in TILES:
        isc = t0 >= NLAT
        zoff = t0 + 3 if isc else t0 + 1
        for cc in range(2):
            zw, bzw = zw_p.get()
            cbl, bcbl = cbl_p.get()
            rz = [DB("z", cc, tt0) for tt0 in (t0 - 512, t0, t0 + 512) if 0 <= tt0 < NLAT or tt0 == t0] + [DB("z", cc, "pad", col) for col in (0, NLAT + 1, NLAT + 2, NTOK + 3)]
            S.dma("sp", zw[:, 0:n + 2], z_s[cc, :, zoff - 1:zoff + n + 1], r=rz, w=[bzw])
            S.dma("pool", cbl[:, 0:n], cb_s[cc, :, t0:t0 + n], r=[DB("cb", cc, t0)], w=[bcbl])
            ts("dve", y3[:, 0:n], zw[:, 0:n], cw[:, cc, 0:1], None, ALU.mult, None, [bzw, b_cw], [b_y3])
            stt("dve", y3[:, 0:n], zw[:, 1:n + 1], cw[:, cc, 1:2], y3[:, 0:n], ALU.mult, ALU.add, [bzw, b_cw, b_y3], [b_y3])
            stt("dve", y3[:, 0:n], zw[:, 2:n + 2], cw[:, cc, 2:3], y3[:, 0:n], ALU.mult, ALU.add, [bzw, b_cw, b_y3], [b_y3])
            yc, byc = yc_p.get()
            tt("pool", yc[:, 0:n], y3[:, 0:n], cbl[:, 0:n], ALU.mult, [b_y3, bcbl], [byc])
            S.dma("sp", mix_s[6 + cc, :, t0:t0 + n], yc[:, 0:n], r=[byc], w=[DB("mix", 6 + cc, t0)])
    fence(S)
    mem.reset(PBASE)

    if stop == "D":
        S.emit()
        return nc
    Wob = mem.sb([128, 8, 1024], BF16); b_Wo = Buf()
    stg3 = RR(mem, [128, 1024], F32, 2)
    for k in range(8):
        st_, bst = stg3.get()
        S.dma("sp", st_[:, :], wout_d[k * 128:(k + 1) * 128, :], w=[bst])
        cp("pool", Wob[:, k, :], st_[:, :], [bst], [b_Wo])
    wrf = mem.sb([128, 8, 16]); wrb = mem.sb([128, 8, 16], BF16); b_wr = Buf()
    S.dma("sp", wrf[:, :, :], wr_d[:, :].rearrange("(k p) e -> p k e", p=128), w=[b_wr])
    cp("dve", wrb[:, :, :], wrf[:, :, :], [b_wr], [b_wr])
    mx_p = RR(mem, [128, 8, 128], BF16, 2)
    xt_p = RR(mem, [128, 1024], F32, 2)
    xm_p = RR(mem, [128, 1024], F32, 2)
    nsm2 = norm_scratch(mem)
    h2_p = RR(mem, [128, 8, 128], BF16, 2)
    lmax = mem.sb([128, 1]); lsm = mem.sb([128, 1]); ex_t = mem.sb([128, 16]); b_sm = Buf()
    af_p = RR(mem, [128, 16], F32, 2)
    PE6 = PRR(pfs[0:6])
    for (t0, n) in TILES:
        isc = t0 >= NLAT
        r = 1 if isc else 0
        src, so = xsrc(t0, n)
        for sub in range(n // 128):
            ta = t0 + sub * 128
            mx, bmx = mx_p.get()
            S.dma("sp", mx[:, :, :], mix_s[:, :, ta:ta + 128].rearrange("k p t -> p k t"), r=[DB("mix", k, t0) for k in range(8)], w=[bmx])
            xt, bx = xt_p.get()
            S.dma("pool", xt[:, :], src[so + sub * 128:so + (sub + 1) * 128, :], w=[bx])
            xm, bxm = xm_p.get()
            for half in range(2):
                pso, bpso = PE6.get()
                for k in range(8):
                    mm(pso[:, :], mx[:, k, :], Wob[:, k, half * 512:(half + 1) * 512], k == 0, k == 7, [bmx, b_Wo], [bpso])
                tt("dve", xm[:, half * 512:(half + 1) * 512], pso[:, :], G1[:, r, half * 512:(half + 1) * 512], ALU.mult, [bpso, b_G], [bxm])
            tt("pool", xm[:, :], xm[:, :], xt[:, :], ALU.add, [bxm, bx], [bxm])
            S.dma("sp", xm_s[ta:ta + 128, :], xm[:, :], r=[bxm], w=[DB("xm", ta)])
            h2, bh2 = h2_p.get()
            norm_tile(None, 0, r, A2, 3, xm, bxm, nsm2, h2, bh2, 0)
            S.dma("pool", h2_s[:, :, ta:ta + 128].rearrange("k p t -> p k t"), h2[:, :, :], r=[bh2], w=[DB("h2", ta)])
            pl, bpl = PE6.get()
            for k in range(8):
                mm(pl[:, 0:16], h2[:, k, :], wrb[:, k, :], k == 0, k == 7, [bh2, b_wr], [bpl])
            S.op("dve", lambda e, pl=pl: e.tensor_reduce(out=lmax[:, :], in_=pl[:, 0:16], axis=AX.X, op=ALU.max), r=[bpl], w=[b_sm])
            ts("dve", lmax[:, :], lmax[:, :], -1.0, None, ALU.mult, None, [b_sm], [b_sm])
            act(ex_t[:, :], pl[:, 0:16], AF.Exp, [bpl, b_sm], [b_sm], bias=lmax[:, 0:1], accum_out=lsm[:, :])
            recip(lsm[:, :], lsm[:, :], [b_sm], [b_sm])
            af, baf = af_p.get()
            ts("dve", af[:, :], ex_t[:, :], lsm[:, 0:1], None, ALU.mult, None, [b_sm], [baf])
            S.dma("pool", aff_s[ta:ta + 128, :], af[:, :], r=[baf], w=[DB("aff", ta)])
    fence(S)
    mem.reset(PBASE)

    if stop == "E":
        S.emit()
        return nc
    mF = mem.mark()
    stg4 = RR(mem, [128, 8, 512], F32, 2)
    cst4 = RR(mem, [128, 8, 512], BF16, 2)
    for ex in range(NEXP):
        for (wsrc, wdst, nm) in ((wg_d, wgb_s, "wg"), (wu_d, wub_s, "wu")):
            st_, bst = stg4.get()
            S.dma("sp", st_[:, :, :], wsrc[ex, :, :].rearrange("(k p) f -> p k f", p=128), w=[bst])
            cb_, bcb_ = cst4.get()
            cp("pool" if nm == "wg" else "dve", cb_[:, :, :], st_[:, :, :], [bst], [bcb_])
            S.dma("pool", wdst[ex, :, :, :], cb_[:, :, :], r=[bcb_], w=[DB(nm, ex)])
        st_, bst = stg4.get()
        S.dma("sp", st_[:, 0:4, :].rearrange("p k (a f) -> p k a f", a=1), wd_d[ex, :, :].rearrange("(k p) (a f) -> p k a f", p=128, a=1)[:, :, :, 0:512], w=[bst])
        S.dma("sp", st_[:, 4:8, :].rearrange("p k (a f) -> p k a f", a=1), wd_d[ex, :, :].rearrange("(k p) (a f) -> p k a f", p=128, a=2)[:, :, 1:2, :], w=[bst])
        cb_, bcb_ = cst4.get()
        cp("pool", cb_[:, :, :], st_[:, :, :], [bst], [bcb_])
        S.dma("pool", wdb_s[ex, :, :, 0:512], cb_[:, 0:4, :], r=[bcb_], w=[DB("wd0", ex)])
        S.dma("pool", wdb_s[ex, :, :, 512:1024], cb_[:, 4:8, :], r=[bcb_], w=[DB("wd1", ex)])
    fence(S)
    mem.reset(mF)
    thr = mem.sb([128, 2, 16]); b_thr = Buf()
    mB = mem.mark()
    for (r, tok0, ntok, cap) in ((0, 0, NLAT, 2 * NLAT // NEXP), (1, NLAT, NCTX, 2 * NCTX // NEXP)):
        per = ntok // 128
        afa = mem.sb([128, per, 16]); b_afa = Buf()
        S.dma("sp", afa[:, :, :], aff_s[tok0:tok0 + ntok, :].rearrange("(p t) e -> p t e", p=128), r=[DB("aff", ta) for ta in range(tok0, tok0 + ntok, 128)], w=[b_afa])
        afv = afa[:, :, :].rearrange("p t e -> p e t")
        cmp_ = mem.sb([128, 16, per]); b_cmp = Buf()
        lo = mem.sb([128, 16]); hi = mem.sb([128, 16]); mid = mem.sb([128, 16]); cnt = mem.sb([128, 16]); prd = mem.sb([128, 16]); dlt = mem.sb([128, 16])
        b_b = Buf()
        S.op("dve", lambda e, lo=lo: e.memset(lo[:, :], 0.0), w=[b_b])
        S.op("dve", lambda e, hi=hi: e.memset(hi[:, :], 1.0), w=[b_b])
        for it in range(30):
            tt("dve", mid[:, :], lo[:, :], hi[:, :], ALU.add, [b_b], [b_b])
            ts("dve", mid[:, :], mid[:, :], 0.5, None, ALU.mult, None, [b_b], [b_b])
            tt("dve", cmp_[:, :, :], afv, mid[:, :].unsqueeze(2).to_broadcast([128, 16, per]), ALU.is_ge, [b_afa, b_b], [b_cmp])
            S.op("dve", lambda e, cnt=cnt, cmp_=cmp_: e.tensor_reduce(out=cnt[:, :], in_=cmp_[:, :, :], axis=AX.X, op=ALU.add), r=[b_cmp], w=[b_b])
            pc, bpc = PE6.get()
            mm(pc[:, 0:16], onesf[:, :], cnt[:, :], True, True, [b_onesf, b_b], [bpc])
            ts("dve", prd[:, :], pc[:, 0:16], float(cap) - 0.5, None, ALU.is_ge, None, [bpc], [b_b])
            tt("dve", dlt[:, :], mid[:, :], lo[:, :], ALU.subtract, [b_b], [b_b])
            tt("dve", dlt[:, :], dlt[:, :], prd[:, :], ALU.mult, [b_b], [b_b])
            tt("dve", lo[:, :], lo[:, :], dlt[:, :], ALU.add, [b_b], [b_b])
            tt("dve", dlt[:, :], hi[:, :], mid[:, :], ALU.subtract, [b_b], [b_b])
            tt("dve", dlt[:, :], dlt[:, :], prd[:, :], ALU.mult, [b_b], [b_b])
            tt("dve", hi[:, :], mid[:, :], dlt[:, :], ALU.add, [b_b], [b_b])
        cp("dve", thr[:, r, :], lo[:, :], [b_b], [b_thr])
        fence(S)
        mem.reset(mB)
    h2l_p = RR(mem, [128, 8, 1024], BF16, 2)
    acc = mem.sb([128, 8, 1024]); b_acc = Buf()
    sgate = mem.sb([128, 8, 16]); afo = mem.sb([128, 8, 16]); b_sg2 = Buf()
    wgl_p = RR(mem, [128, 8, 512], BF16, 2)
    wul_p = RR(mem, [128, 8, 512], BF16, 2)
    wdl_p = RR(mem, [128, 4, 1024], BF16, 2)
    hid_p = RR(mem, [128, 4, 512], BF16, 2)
    sl_t, b_sl = T512()
    xo_p = RR(mem, [128, 1024], F32, 2)
    STILES = [(i * 1024, 1024) for i in range(NLAT // 1024)] + [(NLAT, 256)]
    for (t0, n) in STILES:
        isc = t0 >= NLAT
        r = 1 if isc else 0
        nsub = n // 128
        h2l, bh2l = h2l_p.get()
        S.dma("sp", h2l[:, :, 0:n], h2_s[:, :, t0:t0 + n].rearrange("k p t -> p k t"), r=[DB("h2", ta) for ta in range(t0, t0 + n, 128)], w=[bh2l])
        S.dma("pool", afo[:, 0:nsub, :], aff_s[t0:t0 + n, :].rearrange("(s t) e -> t s e", t=128), r=[DB("aff", ta) for ta in range(t0, t0 + n, 128)], w=[b_sg2])
        tt("dve", sgate[:, 0:nsub, :], afo[:, 0:nsub, :], thr[:, r, :].unsqueeze(1).to_broadcast([128, nsub, 16]), ALU.is_ge, [b_sg2, b_thr], [b_sg2])
        tt("dve", sgate[:, 0:nsub, :], sgate[:, 0:nsub, :], afo[:, 0:nsub, :], ALU.mult, [b_sg2], [b_sg2])
        S.op("pool", lambda e: e.memset(acc[:, :, :], 0.0), w=[b_acc])
        for ex in range(NEXP):
            wgl, bwg = wgl_p.get()
            wul, bwu = wul_p.get()
            wdl, bwd = wdl_p.get()
            S.dma("sp", wgl[:, :, :], wgb_s[ex, :, :, :], r=[DB("wg", ex)], w=[bwg])
            S.dma("pool", wul[:, :, :], wub_s[ex, :, :, :], r=[DB("wu", ex)], w=[bwu])
            S.dma("sp", wdl[:, :, :], wdb_s[ex, :, :, :], r=[DB("wd0", ex), DB("wd1", ex)], w=[bwd])
            for tt0 in range(0, n, 512):
                nn = min(512, n - tt0)
                hid, bhid = hid_p.get()
                for fc in range(4):
                    pg_, bpg_ = PE6.get()
                    for k in range(8):
                        mm(pg_[:, 0:nn], wgl[:, k, fc * 128:(fc + 1) * 128], h2l[:, k, tt0:tt0 + nn], k == 0, k == 7, [bwg, bh2l], [bpg_])
                    pu2, bpu2 = PE6.get()
                    for k in range(8):
                        mm(pu2[:, 0:nn], wul[:, k, fc * 128:(fc + 1) * 128], h2l[:, k, tt0:tt0 + nn], k == 0, k == 7, [bwu, bh2l], [bpu2])
                    act(sl_t[:, 0:nn], pg_[:, 0:nn], AF.Silu, [bpg_], [b_sl])
                    tt("dve", hid[:, fc, 0:nn], sl_t[:, 0:nn], pu2[:, 0:nn], ALU.mult, [b_sl, bpu2], [bhid])
                for sub in range(nn // 128):
                    sa = (tt0 // 128) + sub
                    for half in range(2):
                        pdn, bpdn = PE6.get()
                        for fc in range(4):
                            mm(pdn[:, :], hid[:, fc, sub * 128:(sub + 1) * 128], wdl[:, fc, half * 512:(half + 1) * 512], fc == 0, fc == 3, [bhid, bwd], [bpdn])
                        stt("dve", acc[:, sa, half * 512:(half + 1) * 512], pdn[:, :], sgate[:, sa, ex:ex + 1], acc[:, sa, half * 512:(half + 1) * 512], ALU.mult, ALU.add, [bpdn, b_sg2, b_acc], [b_acc])
        dst, do = xdst(t0)
        for sub in range(nsub):
            ta = t0 + sub * 128
            xo, bxo = xo_p.get()
            S.dma("sp", xo[:, :], xm_s[ta:ta + 128, :], r=[DB("xm", ta)], w=[bxo])
            tt("pool", acc[:, sub, :], acc[:, sub, :], G2[:, r, :], ALU.mult, [b_acc, b_G], [b_acc])
            tt("pool", xo[:, :], xo[:, :], acc[:, sub, :], ALU.add, [bxo, b_acc], [bxo])
            S.dma("sp", dst[do + sub * 128:do + (sub + 1) * 128, :], xo[:, :], r=[bxo], final=True)
    S.emit()
    return nc


def _rm_patterns(nlat=16384):
    NROWS = nlat // 64
    pats = []
    for g in range(nlat // 512):
        for p in range(8):
            if not (0 <= 8 * g - 4 + 2 * p < NROWS):
                continue
            t = []
            for a in range(2):
                for i in range(8):
                    kr = 8 * g - 4 + 2 * p + a
                    rq = 8 * g + i
                    rs0 = min(max(rq - 4, 0), NROWS - 8)
                    t.append(rs0 <= kr < rs0 + 8)
            t = tuple(t)
            if t not in pats:
                pats.append(t)
    return pats


def _consts(pats, nlat=16384):
    NLAT = nlat
    NTOK = NLAT + NCTX
    NEG = -30000.0
    idn = np.eye(128, dtype=np.float32)
    blk = np.zeros((128, 128), np.float32)
    blk[:64, :64] = 1
    blk[64:, 64:] = 1
    rot = np.zeros((128, 128), np.float32)
    for hb in (0, 64):
        for m in range(64):
            q = m // 16
            if q in (0, 2):
                rot[hb + m + 16, hb + m] = -1.0
            else:
                rot[hb + m - 16, hb + m] = 1.0
    s = np.arange(64)
    mf = (s[:, None] <= s[None, :]).astype(np.float32)
    mb = (s[:, None] >= s[None, :]).astype(np.float32)
    cst = np.stack([idn, blk, rot, np.tile(mf, (2, 2)), np.tile(mb, (2, 2))]).astype(np.float32)
    rma = np.zeros((len(pats), 128, 128), np.float32)
    for pi, pt in enumerate(pats):
        for a in range(2):
            for i in range(8):
                if not pt[a * 8 + i]:
                    rma[pi, a * 8 + i, a * 64:(a + 1) * 64] = NEG
    rmb = np.zeros((128, 8, 64), np.float32)
    for a in range(2):
        for i in range(8):
            rmb[a * 8 + i, i, :] = 1.0
    rmb = rmb.reshape(128, 512)
    t = np.arange(NLAT)
    row = (t // 64).astype(np.float32)
    col = (t % 64).astype(np.float32)
    inv = (np.float32(10000.0) ** (-np.arange(16, dtype=np.float32) / np.float32(16))).astype(np.float32)
    ar = row[:, None] * inv
    ac = col[:, None] * inv
    ang = np.concatenate([ar, ar, ac, ac], axis=-1)
    cos = np.ones((128, NTOK), np.float32)
    sin = np.zeros((128, NTOK), np.float32)
    cos[:64, :NLAT] = np.cos(ang).T
    cos[64:, :NLAT] = np.cos(ang).T
    sin[:64, :NLAT] = np.sin(ang).T
    sin[64:, :NLAT] = np.sin(ang).T
    return cst, rma, rmb, cos, sin


def _tt_table(rpb):
    NEG = -30000.0
    tt = np.zeros((128, 6, 22, 64), np.float32)
    qc = np.arange(64)
    ws = np.clip(qc - 8, 0, 48)
    kc = np.arange(64)
    inwin = (kc[:, None] >= ws[None, :]) & (kc[:, None] < ws[None, :] + 16)
    rel = np.clip(kc[:, None] - qc[None, :] + 15, 0, 30)
    for a in range(2):
        for u in range(22):
            dr = a + 10 - u
            if abs(dr) <= 7:
                vals = rpb[:, dr + 7, :][:, rel]
                blk = np.where(inwin[None], vals, np.float32(NEG))
            else:
                blk = np.zeros((6, 64, 64), np.float32)
            tt[a * 64:(a + 1) * 64, :, u, :] = blk.transpose(1, 0, 2)
    return np.ascontiguousarray(tt.reshape(128, 6 * 22 * 64))


_NC_CACHE = {}


def _f(a):
    return np.ascontiguousarray(np.asarray(a, dtype=np.float32))


def _fm(v):
    return np.ascontiguousarray(_f(v).reshape(-1, 128).T)


def _common(l, W, consts):
    cst, rma, rmb, cos, sin = consts
    lbl = np.ascontiguousarray(_f(W["hg_lb_logits"]).reshape(2, 4, 3, 128).transpose(3, 2, 0, 1))
    return {
        "wmod": _f(W["w_mod"][l]), "bmodT": _fm(W["b_mod"][l]), "n1T": _fm(W["norm1_w"][l]), "n2T": _fm(W["norm2_w"][l]),
        "win": _f(W["w_in"][l]), "wout": _f(W["w_out"][l]), "wr": _f(W["w_router"][l]),
        "hvec": np.ascontiguousarray(np.stack([np.tile(_f(W["na_q_norm"][l]), 2), np.tile(_f(W["na_k_norm"][l]), 2),
                                               np.tile(_f(W["hg_norm"][l]), 2)], axis=1)),
        "lbl": lbl, "cw": np.ascontiguousarray(_f(W["conv_w"][l]).reshape(3, 2, 128).transpose(2, 1, 0)),
        "tt": _tt_table(_f(W["na_rpb"][l])), "rma": rma, "rmb": rmb, "cst": cst, "cos": cos, "sin": sin,
        "wg": _f(W["w_exp_gate"][l]), "wu": _f(W["w_exp_up"][l]), "wd": _f(W["w_exp_down"][l]),
    }


def run_layer(nc, common, xs, cxs, c, c_ctx, ncores=2):
    names = set(a.memorylocations[0].name for a in nc.m.functions[0].allocations
                if isinstance(a, mybir.MemoryLocationSet) and a.kind == "ExternalInput")
    in_maps = []
    for core in range(ncores):
        b = core % 2
        c2 = np.stack([c[b], c_ctx], axis=0)
        c2T = np.ascontiguousarray(c2.reshape(2, 8, 128).transpose(2, 1, 0))
        d = dict(common)
        d.update({"x": xs[b], "cx": cxs[b], "c2T": c2T})
        in_maps.append({k: v for k, v in d.items() if k in names})
    return run_bass_kernel_spmd(nc, in_maps, core_ids=list(range(ncores)))


def kernel(x, c, ctx, c_ctx, w_mod, b_mod, norm1_w, w_in, na_q_norm, na_k_norm, na_rpb, hg_lb_logits, hg_norm, conv_w,
           w_out, norm2_w, w_router, w_exp_gate, w_exp_up, w_exp_down):
    W = dict(w_mod=w_mod, b_mod=b_mod, norm1_w=norm1_w, w_in=w_in, na_q_norm=na_q_norm, na_k_norm=na_k_norm, na_rpb=na_rpb,
             hg_lb_logits=hg_lb_logits, hg_norm=hg_norm, conv_w=conv_w, w_out=w_out, norm2_w=norm2_w, w_router=w_router,
             w_exp_gate=w_exp_gate, w_exp_up=w_exp_up, w_exp_down=w_exp_down)
    x = _f(x); ctx = _f(ctx); c = _f(c); c_ctx = _f(c_ctx)
    nlat = x.shape[1]
    pats = _rm_patterns(nlat)
    consts = _consts(pats, nlat)
    xs = [x[0], x[1]]
    cxs = [ctx[0], ctx[1]]
    for l in range(4):
        key = (l, nlat)
        if key not in _NC_CACHE:
            _NC_CACHE[key] = build_layer(l, pats, nlat)
        res = run_layer(_NC_CACHE[key], _common(l, W, consts), xs, cxs, c, c_ctx)
        xs = [np.asarray(res.results[b]["xo"], dtype=np.float32) for b in range(2)]
        cxs = [np.asarray(res.results[b]["cxo"], dtype=np.float32) for b in range(2)]
    return np.stack(xs, axis=0).astype(np.float32)
```

```python
import contextlib
import numpy as np
import concourse.bass as bass
import concourse.mybir as mybir
from concourse.bass_utils import run_bass_kernel_spmd

F32 = mybir.dt.float32
BF16 = mybir.dt.bfloat16
ALU = mybir.AluOpType
AF = mybir.ActivationFunctionType
AX = mybir.AxisListType

ENGS = ("sp", "act", "dve", "pool", "pe")
NSLOT = 8


class Buf:
    __slots__ = ("name", "w", "rs")

    def __init__(self, name=""):
        self.name = name
        self.w = None
        self.rs = []


class Ins:
    __slots__ = ("eng", "fn", "deps", "signal", "tick", "dma", "n", "k")


class Sched:
    def __init__(self, nc):
        self.nc = nc
        self.q = {e: [] for e in ENGS}
        self.ndma = {e: 0 for e in ENGS}
        self.stack = contextlib.ExitStack()
        self.finals = []
        self._nm = 0

    def sb(self, shape, dt=F32, name=None):
        self._nm += 1
        return self.stack.enter_context(self.nc.sbuf_tensor(name or f"sb{self._nm}", list(shape), dt))

    def ps(self, shape, dt=F32, name=None):
        self._nm += 1
        return self.stack.enter_context(self.nc.psum_tensor(name or f"ps{self._nm}", list(shape), dt))

    def dram(self, name, shape, dt=F32, kind="Internal"):
        return self.nc.dram_tensor(name, list(shape), dt, kind=kind)

    def op(self, eng, fn, r=(), w=(), dma=False):
        ins = Ins()
        ins.eng = eng
        ins.fn = fn
        ins.dma = dma
        ins.signal = False
        ins.tick = 0
        deps = {}
        for b in r:
            if b.w is not None:
                deps[id(b.w)] = b.w
        for b in w:
            if b.w is not None:
                deps[id(b.w)] = b.w
            for rd in b.rs:
                deps[id(rd)] = rd
        ins.deps = list(deps.values())
        for b in r:
            if not dma:
                b.rs = [x for x in b.rs if x.dma or x.eng != eng]
            b.rs.append(ins)
        for b in w:
            b.w = ins
            b.rs = []
        ins.k = len(self.q[eng])
        if dma:
            ins.n = self.ndma[eng]
            self.ndma[eng] += 1
        self.q[eng].append(ins)
        return ins

    def dma(self, eng, out, in_, r=(), w=(), final=False):
        ins = self.op(eng, lambda e: e.dma_start(out=out, in_=in_), r=r, w=w, dma=True)
        if final:
            self.finals.append(ins)
        return ins

    def emit(self):
        nc = self.nc
        for e in ENGS:
            for ins in self.q[e]:
                for d in ins.deps:
                    if d.dma:
                        continue
                    if d.eng == ins.eng:
                        if d.eng == "pe" and not ins.dma:
                            continue
                        if ins.dma or (ins.k - d.k) <= 8:
                            d.signal = True
                    else:
                        d.signal = True
        for e in ENGS:
            t = 0
            for ins in self.q[e]:
                if ins.signal and not ins.dma:
                    t += 1
                    ins.tick = t
        st = self.stack
        csem = {e: st.enter_context(nc.semaphore(f"c_{e}")) for e in ENGS}
        dsem = {e: [st.enter_context(nc.semaphore(f"d_{e}{i}")) for i in range(NSLOT)] for e in ("sp", "act", "pool")}
        engobj = {}
        finals = self.finals

        def run(e, eng):
            waited = {}

            def wait(sem, val):
                key = id(sem)
                if waited.get(key, 0) >= val:
                    return
                waited[key] = val
                eng.wait_ge(sem, val)

            for ins in self.q[e]:
                for d in ins.deps:
                    if d.dma:
                        wait(dsem[d.eng][d.n % NSLOT], 16 * (d.n // NSLOT + 1))
                    elif d.signal:
                        if d.eng == e and not ins.dma and (e == "pe" or (ins.k - d.k) > 8):
                            continue
                        wait(csem[d.eng], d.tick)
                if ins.dma:
                    if ins.n >= NSLOT:
                        wait(dsem[e][ins.n % NSLOT], 16 * (ins.n // NSLOT))
                    ins.fn(eng).then_inc(dsem[e][ins.n % NSLOT], 16)
                else:
                    bi = ins.fn(eng)
                    if ins.signal:
                        bi.then_inc(csem[e], 1)
            if e == "sp":
                for qe in ("sp", "act", "pool"):
                    n = self.ndma[qe]
                    for s in range(min(n, NSLOT)):
                        last = ((n - 1 - s) // NSLOT) * NSLOT + s
                        wait(dsem[qe][s], 16 * (last // NSLOT + 1))

        with nc.allow_non_contiguous_dma(reason="small strided scratch DMAs"), nc.Block() as block:
            @block.sync
            def _(eng):
                run("sp", eng)

            @block.scalar
            def _(eng):
                run("act", eng)

            @block.vector
            def _(eng):
                run("dve", eng)

            @block.gpsimd
            def _(eng):
                run("pool", eng)

            @block.tensor
            def _(eng):
                run("pe", eng)
        self.stack.close()


NLAT = 16384
NCTX = 256
NTOK = NLAT + NCTX
DM = 1024
NIN = 3840
NEXP = 16
EDIM = 512
TILES = [(i * 512, 512) for i in range(32)] + [(NLAT, 256)]
SB_BASE = 16512
SB_TOP = 229344


class Mem:
    def __init__(self, S):
        self.S = S
        self.off = SB_BASE
        self.n = 0

    def sb(self, shape, dt=F32, name=None):
        sz = int(np.prod(shape[1:])) * (2 if dt == BF16 else 4)
        sz = (sz + 31) // 32 * 32
        self.n += 1
        t = self.S.nc.alloc_sbuf_tensor_at(f"m{self.n}" + (("_" + name) if name else ""), list(shape), dt, offset=self.off)
        self.off += sz
        assert self.off <= SB_TOP, f"SBUF overflow {self.off}"
        return t

    def mark(self):
        return self.off

    def reset(self, m):
        self.off = m


class RR:
    def __init__(self, mem, shape, dt, n):
        self.t = [mem.sb(shape, dt) for _ in range(n)]
        self.b = [Buf() for _ in range(n)]
        self.i = 0

    def get(self):
        i = self.i
        self.i = (i + 1) % len(self.t)
        return self.t[i], self.b[i]


class PRR:
    def __init__(self, tiles):
        self.t = tiles
        self.b = [Buf() for _ in tiles]
        self.i = 0

    def get(self):
        i = self.i
        self.i = (i + 1) % len(self.t)
        return self.t[i], self.b[i]


def fence(S):
    lasts = []
    for e in ENGS:
        q = S.q[e]
        nd = 0
        seen_c = False
        for ins in reversed(q):
            if ins.dma:
                if nd < NSLOT:
                    lasts.append(ins)
                    nd += 1
            elif not seen_c:
                lasts.append(ins)
                seen_c = True
            if nd >= NSLOT and seen_c:
                break
    fb = Buf()
    for e in ENGS:
        ins = S.op(e, lambda eng: eng.nop(), r=(), w=())
        ins.deps = [d for d in lasts if d is not ins]


def build_model(layers, rm_patterns, nlat=16384, debug=False, stop=None):
    NL_ = len(layers)
    NLAT = nlat
    NTOK = NLAT + NCTX
    NROWS = NLAT // 64
    TILES = [(i * 512, 512) for i in range(NLAT // 512)] + [(NLAT, 256)]
    nc = bass.Bass("TRN2", target_bir_lowering=False)
    S = Sched(nc)
    mem = Mem(S)
    EI = "ExternalInput"

    class _Stop(Exception):
        pass

    def chk(name):
        if stop == name:
            raise _Stop()


    def din(name, shape, dt=F32):
        return nc.dram_tensor(name, list(shape), dt, kind=EI)

    x_in_d = din("x", [NLAT, DM])
    cx_in_d = din("cx", [NCTX, DM])
    c2T_d = din("c2T", [128, 8, 2])
    wmod_a = din("wmod", [NL_, DM, 6 * DM])
    bmodT_a = din("bmodT", [NL_, 128, 48])
    n1T_a = din("n1T", [NL_, 128, 8])
    n2T_a = din("n2T", [NL_, 128, 8])
    win_a = din("win", [NL_, DM, NIN])
    wout_a = din("wout", [NL_, DM, DM])
    wr_a = din("wr", [NL_, DM, NEXP])
    hvec_a = din("hvec", [NL_, 128, 3])
    lbl_d = din("lbl", [128, 3, 2, 4])
    cw_a = din("cw", [NL_, 128, 2, 3])
    tt_a = din("tt", [NL_, 128, 6 * 22 * 64])
    rma_d = din("rma", [len(rm_patterns), 128, 128])
    rmb_d = din("rmb", [128, 512])
    cst_d = din("cst", [5, 128, 128])
    cos_d = din("cos", [128, NTOK])
    sin_d = din("sin", [128, NTOK])
    early = stop is not None and (stop.startswith("A") or stop == "pro")
    wg_a = None if early else din("wg", [NL_, NEXP, DM, EDIM])
    wu_a = None if early else din("wu", [NL_, NEXP, DM, EDIM])
    wd_a = None if early else din("wd", [NL_, NEXP, EDIM, DM])
    xo_out_d = nc.dram_tensor("xo", [NLAT, DM], F32, kind="ExternalOutput")
    cxo_out_d = nc.dram_tensor("cxo", [NCTX, DM], F32, kind="ExternalOutput")
    xbuf = [nc.dram_tensor(f"xbuf{i}", [NLAT, DM], F32) for i in range(2)]
    cxbuf = [nc.dram_tensor(f"cxbuf{i}", [NCTX, DM], F32) for i in range(2)]

    def xsrc(t0, n):
        return (x_d, t0) if t0 < NLAT else (cx_d, t0 - NLAT)

    def xdst(t0):
        return (xo_d, t0) if t0 < NLAT else (cxo_d, t0 - NLAT)

    def scr(name, shape, dt=F32):
        if debug and name in ("mix_s", "xm_s", "aff_s", "QT_s", "KT_s", "V_s", "oacc_s", "h2_s"):
            return nc.dram_tensor(name, list(shape), dt, kind="ExternalOutput")
        return nc.dram_tensor(name, list(shape), dt)

    QT_s = scr("QT_s", [3, 2, 128, NTOK], BF16)
    KT_s = scr("KT_s", [3, 128, NTOK], BF16)
    V_s = scr("V_s", [NTOK, 384], BF16)
    mix_s = scr("mix_s", [8, 128, NTOK], BF16)
    oacc_s = scr("oacc_s", [3, 128, NTOK])
    qhat_s = scr("qhat_s", [2, 3, 128, NTOK], BF16)
    U_s = scr("U_s", [2, 3, 128, NTOK // 64, 64])
    D_s = scr("D_s", [2, 3, 128, NTOK // 64])
    gs_s = scr("gs_s", [3, 128, NTOK])
    z_s = scr("z_s", [2, 128, NTOK + 4])
    cb_s = scr("cb_s", [2, 128, NTOK], BF16)
    xm_s = scr("xm_s", [NTOK, DM])
    h2_s = scr("h2_s", [8, 128, NTOK], BF16)
    aff_s = scr("aff_s", [NTOK, NEXP])
    wgb_s = scr("wgb_s", [NEXP, 128, 8, EDIM], BF16)
    wub_s = scr("wub_s", [NEXP, 128, 8, EDIM], BF16)
    wdb_s = scr("wdb_s", [NEXP, 128, 4, DM], BF16)
    dbuf = {}

    def DB(*key):
        if key not in dbuf:
            dbuf[key] = Buf()
        return dbuf[key]

    pfs = [S.ps([128, 512], F32) for _ in range(6)]
    pbs = [S.ps([128, 8, 128], BF16) for _ in range(2)]
    PF = PRR(pfs)
    PB = PRR(pbs)

    def mm(out, lhsT, rhs, st, sp_, r, w):
        return S.op("pe", lambda e: e.matmul(out, lhsT, rhs, start=st, stop=sp_), r=r, w=w)

    def tr(out, in_, idn, r, w):
        return S.op("pe", lambda e: e.transpose(out=out, in_=in_, identity=idn), r=r, w=w)

    def act(out, in_, func, r, w, **kw):
        return S.op("act", lambda e: e.activation(out=out, in_=in_, func=func, **kw), r=r, w=w)

    def tt(eng, out, in0, in1, op, r, w):
        return S.op(eng, lambda e: e.tensor_tensor(out=out, in0=in0, in1=in1, op=op), r=r, w=w)

    def ts(eng, out, in0, s1, s2, op0, op1, r, w):
        if s2 is None:
            return S.op(eng, lambda e: e.tensor_scalar(out=out, in0=in0, scalar1=s1, scalar2=None, op0=op0), r=r, w=w)
        return S.op(eng, lambda e: e.tensor_scalar(out=out, in0=in0, scalar1=s1, scalar2=s2, op0=op0, op1=op1), r=r, w=w)

    def stt(eng, out, in0, sc, in1, op0, op1, r, w):
        return S.op(eng, lambda e: e.scalar_tensor_tensor(out=out, in0=in0, scalar=sc, in1=in1, op0=op0, op1=op1), r=r, w=w)

    def cp(eng, out, in_, r, w):
        return S.op(eng, lambda e: e.tensor_copy(out=out, in_=in_), r=r, w=w)

    def recip(out, in_, r, w):
        return S.op("dve", lambda e: e.reciprocal(out=out, in_=in_), r=r, w=w)

    cstf = mem.sb([128, 5, 128]); b_cstf = Buf()
    S.dma("sp", cstf[:, :, :], cst_d[:, :, :].rearrange("c p n -> p c n"), w=[b_cstf])
    cstb = mem.sb([128, 5, 128], BF16); b_cst = Buf()
    cp("dve", cstb[:, :, :], cstf[:, :, :], [b_cstf], [b_cst])
    idb = cstb[:, 0, :]; blkb = cstb[:, 1, :]; rotb = cstb[:, 2, :]
    idf = cstf[:, 0, :]
    onesf = mem.sb([128, 128]); b_onesf = Buf()
    S.op("pool", lambda e: e.memset(onesf[:, :], 1.0), w=[b_onesf])
    onesb = mem.sb([128, 128], BF16); b_onesb = Buf()
    S.op("pool", lambda e: e.memset(onesb[:, :], 1.0), w=[b_onesb])

    def layer_body(layer, x_d, cx_d, xo_d, cxo_d, wmod_d, bmodT_d, n1T_d, n2T_d, win_d, wout_d, wr_d, hvec_d, cw_d, tt_d, wg_d, wu_d, wd_d):
        def xsrc(t0, n):
            return (x_d, t0) if t0 < NLAT else (cx_d, t0 - NLAT)

        def xdst(t0):
            return (xo_d, t0) if t0 < NLAT else (cxo_d, t0 - NLAT)

        modT = mem.sb([128, 48, 2]); b_mod = Buf()
        c2T = mem.sb([128, 8, 2]); b_c2 = Buf()
        S.dma("sp", c2T[:, :, :], c2T_d[:, :, :], w=[b_c2])
        scT = mem.sb([128, 8, 2]); b_sc = Buf()
        act(scT[:, :, :], c2T[:, :, :], AF.Silu, [b_c2], [b_sc])
        bmodT = mem.sb([128, 48]); b_bm = Buf()
        S.dma("sp", bmodT[:, :], bmodT_d[:, :], w=[b_bm])
        m0 = mem.mark()
        wmp = RR(mem, [128, 8, 1024], F32, 2)
        pm, bpm = PF.get()
        for m in range(6):
            wt, bw = wmp.get()
            for k in range(8):
                S.dma("sp" if k % 2 == 0 else "pool", wt[:, k, :], wmod_d[k * 128:(k + 1) * 128, m * 1024:(m + 1) * 1024], w=[bw])
            for oc in range(8):
                for k in range(8):
                    mm(pm[:, (m * 8 + oc) * 2:(m * 8 + oc) * 2 + 2], wt[:, k, oc * 128:(oc + 1) * 128], scT[:, k, :], k == 0, k == 7, [bw, b_sc], [bpm])
        tt("dve", modT[:, :, :], pm[:, 0:96].rearrange("p (a r) -> p a r", r=2), bmodT[:, :].unsqueeze(2).to_broadcast([128, 48, 2]), ALU.add, [bpm, b_bm], [b_mod])
        mem.reset(m0)
        fence(S)
        n1T = mem.sb([128, 8]); n2T = mem.sb([128, 8]); b_n = Buf()
        S.dma("sp", n1T[:, :], n1T_d[:, :], w=[b_n])
        S.dma("sp", n2T[:, :], n2T_d[:, :], w=[b_n])
        A1 = mem.sb([128, 8, 2]); A2 = mem.sb([128, 8, 2]); b_A = Buf()
        for (A, nT, ms) in ((A1, n1T, 1), (A2, n2T, 4)):
            ts("dve", A[:, :, :], modT[:, ms * 8:(ms + 1) * 8, :], 1.0, None, ALU.add, None, [b_mod], [b_A])
            tt("dve", A[:, :, :], A[:, :, :], nT[:, :].unsqueeze(2).to_broadcast([128, 8, 2]), ALU.mult, [b_A, b_n], [b_A])

        def Bm(ms, k, r):
            return modT[:, ms * 8 + k, r:r + 1]

        G1 = mem.sb([128, 2, 1024]); G2 = mem.sb([128, 2, 1024]); b_G = Buf()
        dg = mem.sb([128, 128]); b_dg = Buf()
        for (G, ms) in ((G1, 2), (G2, 5)):
            for r in range(2):
                for half in range(2):
                    pg, bpg = PF.get()
                    for kk in range(4):
                        k = half * 4 + kk
                        ts("dve", dg[:, :], idf, modT[:, ms * 8 + k, r:r + 1], None, ALU.mult, None, [b_cstf, b_mod], [b_dg])
                        mm(pg[:, kk * 128:(kk + 1) * 128], onesf[:, :], dg[:, :], True, True, [b_onesf, b_dg], [bpg])
                    cp("dve", G[:, r, half * 512:(half + 1) * 512], pg[:, :], [bpg], [b_G])
        hvec = mem.sb([128, 3]); b_hv = Buf()
        S.dma("sp", hvec[:, :], hvec_d[:, :], w=[b_hv])
        qw8 = mem.sb([128, 1])
        ts("dve", qw8[:, :], hvec[:, 0:1], 0.125, None, ALU.mult, None, [b_hv], [b_hv])
        lbl = mem.sb([128, 3, 2, 4]); b_lb = Buf()
        S.dma("sp", lbl[:, :, :, :], lbl_d[:, :, :, :], w=[b_lb])
        lmx = mem.sb([128, 3, 2]); lsum = mem.sb([128, 3, 2]); lbv = mem.sb([128, 3, 2]); omlb = mem.sb([128, 3, 2])
        S.op("dve", lambda e: e.tensor_reduce(out=lmx[:, :, :], in_=lbl[:, :, :, :], axis=AX.X, op=ALU.max), r=[b_lb], w=[b_lb])
        tt("dve", lbl[:, :, :, :], lbl[:, :, :, :], lmx[:, :, :].unsqueeze(3).to_broadcast([128, 3, 2, 4]), ALU.subtract, [b_lb], [b_lb])
        act(lbl[:, :, :, :], lbl[:, :, :, :], AF.Exp, [b_lb], [b_lb])
        S.op("dve", lambda e: e.tensor_reduce(out=lsum[:, :, :], in_=lbl[:, :, :, :], axis=AX.X, op=ALU.add), r=[b_lb], w=[b_lb])
        recip(lsum[:, :, :], lsum[:, :, :], [b_lb], [b_lb])
        S.op("dve", lambda e: e.memset(lbv[:, :, :], 0.0), r=[b_lb], w=[b_lb])
        for jl in range(1, layer + 1):
            tt("dve", lbv[:, :, :], lbv[:, :, :], lbl[:, :, :, jl], ALU.add, [b_lb], [b_lb])
        tt("dve", lbv[:, :, :], lbv[:, :, :], lsum[:, :, :], ALU.mult, [b_lb], [b_lb])
        ts("dve", omlb[:, :, :], lbv[:, :, :], -1.0, 1.0, ALU.mult, ALU.add, [b_lb], [b_lb])
        cw = mem.sb([128, 2, 3]); b_cw = Buf()
        S.dma("sp", cw[:, :, :], cw_d[:, :, :], w=[b_cw])
        PBASE = mem.mark()

        if stop == "pro":
            raise _Stop()
        def norm_tile(src_d, row0, r, A, ms_shift, xt, bx, sm, hT, bhT, col0, fp32_out=None):
            junk, bj, ss, rt, rs_, xn, bxn, bsm = sm
            act(junk[:, :], xt[:, :], AF.Square, [bx], [bj, bsm], accum_out=ss[:, :])
            act(rt[:, :], ss[:, :], AF.Sqrt, [bsm], [bsm], scale=1.0 / DM, bias=1e-6)
            recip(rs_[:, :], rt[:, :], [bsm], [bsm])
            act(xn[:, :], xt[:, :], AF.Copy, [bx, bsm], [bxn], scale=rs_[:, 0:1])
            pt, bpt = PB.get()
            for k in range(8):
                tr(pt[:, k, :], xn[:, k * 128:(k + 1) * 128], idb, [bxn, b_cst], [bpt])
            for k in range(8):
                if k % 2 == 0:
                    ts("dve", hT[:, k, col0:col0 + 128], pt[:, k, :], A[:, k, r:r + 1], Bm(ms_shift, k, r), ALU.mult, ALU.add, [bpt, b_A, b_mod], [bhT])
                else:
                    act(hT[:, k, col0:col0 + 128], pt[:, k, :], AF.Identity, [bpt, b_A, b_mod], [bhT], scale=A[:, k, r:r + 1], bias=Bm(ms_shift, k, r))

        def norm_scratch(mem):
            junk = mem.sb([128, 1024], BF16)
            ss = mem.sb([128, 1]); rt = mem.sb([128, 1]); rs_ = mem.sb([128, 1])
            xn = mem.sb([128, 1024], BF16)
            return (junk, Buf(), ss, rt, rs_, xn, Buf(), Buf())

        Wb = mem.sb([128, 8, NIN], BF16); b_W = Buf()
        stg = RR(mem, [128, 1920], F32, 2)
        for k in range(8):
            for hf in range(2):
                st_, bst = stg.get()
                S.dma("sp" if hf == 0 else "pool", st_[:, :], win_d[k * 128:(k + 1) * 128, hf * 1920:(hf + 1) * 1920], w=[bst])
                cp("pool" if hf == 0 else "dve", Wb[:, k, hf * 1920:(hf + 1) * 1920], st_[:, :], [bst], [b_W])
        xtp = RR(mem, [128, 1024], F32, 2)
        nsm = norm_scratch(mem)
        hTp = RR(mem, [128, 8, 512], BF16, 2)
        cosp = RR(mem, [128, 512], F32, 2)
        sinp = RR(mem, [128, 512], F32, 2)
        zero_t = mem.sb([128, 512], BF16); b_zero = Buf()
        S.op("pool", lambda e: e.memset(zero_t[:, :], 0.0), w=[b_zero])
        zero_f = mem.sb([128, 4]); b_zf = Buf()
        S.op("pool", lambda e: e.memset(zero_f[:, :], 0.0), w=[b_zf])
        ones64 = mem.sb([128, 64]); b_o64 = Buf()
        S.op("pool", lambda e: e.memset(ones64[:, :], 1.0), w=[b_o64])
        for cc in range(2):
            for col in (0, NLAT + 1, NLAT + 2, NTOK + 3):
                S.dma("pool", z_s[cc, :, col:col + 1], zero_f[:, 0:1], r=[b_zf], w=[DB("z", cc, "pad", col)])

        def T512(dt=F32, name=None):
            return mem.sb([128, 512], dt, name), Buf()

        sq_t, b_sq = T512(BF16, name="sq_t")
        rt_t, b_rt = T512(name="rt_t")
        qn_t, b_qn = T512(BF16, name="qn_t")
        t1_t, b_t1 = T512(name="t1_t")
        t2_t, b_t2 = T512(name="t2_t")
        qo_t, b_qo = T512(BF16, name="qo_t")
        vt_p = RR(mem, [128, 384], BF16, 2)
        itm = mem.sb([128, 4, 384], BF16); b_itm = Buf()
        hq_t, b_hq = T512(name="hq_t")
        sg_t, b_sg = T512(name="sg_t")
        lf_t, b_lf = T512(name="lf_t")
        kk_t, b_kk = T512(name="kk_t")
        A_t, b_At = T512(name="A_t")
        a_t, b_a = T512(name="a_t")
        d_t, b_d = T512(name="d_t")
        e_t, b_e = T512(name="e_t")
        e2_t, b_e2 = T512(name="e2_t")
        qtl_t, b_qtl = T512(BF16, name="qtl_t")
        ktl_t, b_ktl = T512(BF16, name="ktl_t")
        qh_t, b_qh = T512(BF16, name="qh_t")
        kh_t, b_kh = T512(BF16, name="kh_t")
        khtm = mem.sb([128, 4, 128], BF16); b_khtm = Buf()
        Tt = mem.sb([128, 8]); rr_t = mem.sb([128, 16]); Dt = mem.sb([128, 8]); bn_t = mem.sb([128, 8]); b_T = Buf(); b_rr = Buf(); b_D = Buf(); b_bn = Buf()
        kz = [mem.sb([128, 512], BF16, "kzf"), mem.sb([128, 512], BF16, "kzb")]
        b_kz = [Buf(), Buf()]
        for i_ in range(2):
            S.op("pool", lambda e, t=kz[i_]: e.memset(t[:, :], 0.0), w=[b_kz[i_]])
        scm = mem.sb([128, 128], BF16); b_scm = Buf()
        Ut = mem.sb([128, 8, 64]); b_U = Buf()
        osb, b_osb = T512()
        gs_t, b_gs = T512()
        cs_t, b_cs = T512()
        cb_t, b_cb = T512(BF16)
        po_ps, b_po = [pfs[2], pfs[3]], [Buf(), Buf()]
        pu_ps, b_pu = [pfs[4], pfs[5]], [Buf(), Buf()]
        PF4 = PRR(pfs[0:2])

        def do_tile(t0, n):
            isc = t0 >= NLAT
            r = 1 if isc else 0
            nsub = n // 128
            nch = n // 64
            c0 = t0 // 64
            hT, bhT = hTp.get()
            src, so = xsrc(t0, n)
            for sub in range(nsub):
                xt, bx = xtp.get()
                S.dma("sp", xt[:, :], src[so + sub * 128: so + (sub + 1) * 128, :], w=[bx])
                norm_tile(src, so, r, A1, 0, xt, bx, nsm, hT, bhT, sub * 128)
            cs, bcs = cosp.get()
            sn, bsn = sinp.get()
            S.dma("pool", cs[:, 0:n], cos_d[:, t0:t0 + n], w=[bcs])
            S.dma("pool", sn[:, 0:n], sin_d[:, t0:t0 + n], w=[bsn])

            def proj(c0_):
                pf, bpf = PF4.get()
                for k in range(8):
                    mm(pf[:, 0:n], Wb[:, k, c0_:c0_ + 128], hT[:, k, 0:n], k == 0, k == 7, [b_W, bhT], [bpf])
                return pf, bpf

            chk("A1")
            for which in range(2):
                for pr in range(3):
                    pf, bpf = proj(which * 384 + pr * 128)
                    act(sq_t[:, 0:n], pf[:, 0:n], AF.Square, [bpf], [b_sq])
                    pss, bpss = PF4.get()
                    mm(pss[:, 0:n], blkb, sq_t[:, 0:n], True, True, [b_cst, b_sq], [bpss])
                    act(rt_t[:, 0:n], pss[:, 0:n], AF.Sqrt, [bpss], [b_rt], scale=1.0 / 64, bias=1e-6)
                    recip(rt_t[:, 0:n], rt_t[:, 0:n], [b_rt], [b_rt])
                    wv = qw8[:, 0:1] if which == 0 else hvec[:, 1:2]
                    stt("dve", qn_t[:, 0:n], pf[:, 0:n], wv, rt_t[:, 0:n], ALU.mult, ALU.mult, [bpf, b_rt, b_hv], [b_qn])
                    prot, bprot = PF4.get()
                    mm(prot[:, 0:n], rotb, qn_t[:, 0:n], True, True, [b_cst, b_qn], [bprot])
                    tt("pool", t1_t[:, 0:n], qn_t[:, 0:n], cs[:, 0:n], ALU.mult, [b_qn, bcs], [b_t1])
                    tt("dve", t2_t[:, 0:n], prot[:, 0:n], sn[:, 0:n], ALU.mult, [bprot, bsn], [b_t2])
                    tt("pool", qo_t[:, 0:n], t1_t[:, 0:n], t2_t[:, 0:n], ALU.add, [b_t1, b_t2], [b_qo])
                    if which == 0:
                        for hh in range(2):
                            oh = 1 - hh
                            S.dma("sp", QT_s[pr, hh, hh * 64:(hh + 1) * 64, t0:t0 + n], qo_t[hh * 64:(hh + 1) * 64, 0:n], r=[b_qo], w=[DB("Q", pr, hh, t0)])
                            S.dma("pool", QT_s[pr, hh, oh * 64:(oh + 1) * 64, t0:t0 + n], zero_t[oh * 64:(oh + 1) * 64, 0:n], r=[b_zero], w=[DB("Qz", pr, hh, t0)])
                    else:
                        S.dma("sp", KT_s[pr, :, t0:t0 + n], qo_t[:, 0:n], r=[b_qo], w=[DB("K", pr, t0)])
            chk("A2")
            for sub in range(nsub):
                pv, bpv = PF4.get()
                for k in range(8):
                    mm(pv[:, 0:384], hT[:, k, sub * 128:(sub + 1) * 128], Wb[:, k, 768:1152], k == 0, k == 7, [bhT, b_W], [bpv])
                vt, bvt = vt_p.get()
                act(vt[:, :], pv[:, 0:384], AF.Copy, [bpv], [bvt])
                S.dma("sp", V_s[t0 + sub * 128:t0 + (sub + 1) * 128, :], vt[:, :], r=[bvt], w=[DB("V", t0, sub)])
                pi, bpi = PF4.get()
                for k in range(8):
                    mm(pi[:, 0:384], hT[:, k, sub * 128:(sub + 1) * 128], Wb[:, k, 2304:2688], k == 0, k == 7, [bhT, b_W], [bpi])
                cp("dve", itm[:, sub, :], pi[:, 0:384], [bpi], [b_itm])
            chk("A3")
            for hp in range(3):
                pf, bpf = proj(1152 + hp * 128)
                act(hq_t[:, 0:n], pf[:, 0:n], AF.Copy, [bpf], [b_hq], scale=0.125)
                pf, bpf = proj(2688 + hp * 128)
                act(gs_t[:, 0:n], pf[:, 0:n], AF.Silu, [bpf], [b_gs])
                S.dma("pool", gs_s[hp, :, t0:t0 + n], gs_t[:, 0:n], r=[b_gs], w=[DB("gs", hp, t0)])
                for dr in range(2):
                    pf, bpf = proj(1536 + dr * 384 + hp * 128)
                    act(sg_t[:, 0:n], pf[:, 0:n], AF.Sigmoid, [bpf], [b_sg])
                    ts("dve", sg_t[:, 0:n], sg_t[:, 0:n], omlb[:, hp, dr:dr + 1], lbv[:, hp, dr:dr + 1], ALU.mult, ALU.add, [b_sg, b_lb], [b_sg])
                    act(lf_t[:, 0:n], sg_t[:, 0:n], AF.Ln, [b_sg], [b_lf])
                    ts("pool", kk_t[:, 0:n], sg_t[:, 0:n], -1.0, 1.0, ALU.mult, ALU.add, [b_sg], [b_kk])
                    for c in range(nch):
                        S.op("dve", lambda e, c=c: e.tensor_tensor_scan(out=A_t[:, c * 64:(c + 1) * 64], data0=ones64[:, :], data1=lf_t[:, c * 64:(c + 1) * 64], initial=0.0, op0=ALU.mult, op1=ALU.add), r=[b_lf, b_o64], w=[b_At])
                    A3 = A_t[:, 0:n].rearrange("p (c s) -> p c s", s=64)
                    cp("dve", Tt[:, 0:nch], A3[:, :, 63], [b_At], [b_T])
                    Tb = Tt[:, 0:nch].unsqueeze(2).to_broadcast([128, nch, 64])
                    a3 = a_t[:, 0:n].rearrange("p (c s) -> p c s", s=64)
                    if dr == 0:
                        cp("pool", a_t[:, 0:n], A_t[:, 0:n], [b_At], [b_a])
                    else:
                        tt("pool", a_t[:, 0:n], lf_t[:, 0:n], A_t[:, 0:n], ALU.subtract, [b_lf, b_At], [b_a])
                        tt("pool", a3, a3, Tb, ALU.add, [b_a, b_T], [b_a])
                    d3 = d_t[:, 0:n].rearrange("p (c s) -> p c s", s=64)
                    cp("dve", bn_t[:, 0:nch], a3[:, :, 31 if dr == 0 else 32], [b_a], [b_bn])
                    tt("pool", d3, a3, bn_t[:, 0:nch].unsqueeze(2).to_broadcast([128, nch, 64]), ALU.subtract, [b_a, b_bn], [b_d])
                    act(e_t[:, 0:n], d_t[:, 0:n], AF.Exp, [b_d], [b_e])
                    tt("dve", qtl_t[:, 0:n], hq_t[:, 0:n], e_t[:, 0:n], ALU.mult, [b_hq, b_e], [b_qtl])
                    act(e2_t[:, 0:n], d_t[:, 0:n], AF.Exp, [b_d], [b_e2], scale=-1.0)
                    tt("pool", ktl_t[:, 0:n], kk_t[:, 0:n], e2_t[:, 0:n], ALU.mult, [b_kk, b_e2], [b_ktl])
                    hsl = slice(0, 32) if dr == 0 else slice(32, 64)
                    cp("pool", kz[dr][:, 0:n].rearrange("p (c s) -> p c s", s=64)[:, :, hsl], ktl_t[:, 0:n].rearrange("p (c s) -> p c s", s=64)[:, :, hsl], [b_ktl], [b_kz[dr]])
                    act(e_t[:, 0:n], a_t[:, 0:n], AF.Exp, [b_a], [b_e])
                    tt("dve", qh_t[:, 0:n], hq_t[:, 0:n], e_t[:, 0:n], ALU.mult, [b_hq, b_e], [b_qh])
                    S.dma("sp", qhat_s[dr, hp, :, t0:t0 + n], qh_t[:, 0:n], r=[b_qh], w=[DB("qh", dr, hp, t0)])
                    tt("pool", d3, Tb, a3, ALU.subtract, [b_a, b_T], [b_d])
                    act(e2_t[:, 0:n], d_t[:, 0:n], AF.Exp, [b_d], [b_e2])
                    tt("pool", kh_t[:, 0:n], kk_t[:, 0:n], e2_t[:, 0:n], ALU.mult, [b_kk, b_e2], [b_kh])
                    act(Dt[:, 0:nch], Tt[:, 0:nch], AF.Exp, [b_T], [b_D])
                    S.dma("pool", D_s[dr, hp, :, c0:c0 + nch], Dt[:, 0:nch], r=[b_D], w=[DB("D", dr, hp, t0)])
                    chk("A4")
                    pt, bpt = PB.get()
                    for sub in range(nsub):
                        tr(pt[:, sub, :], kh_t[:, sub * 128:(sub + 1) * 128], idb, [b_kh, b_cst], [bpt])
                    act(khtm[:, 0:nsub, :], pt[:, 0:nsub, :], AF.Copy, [bpt], [b_khtm])
                    mk = cstb[:, 3 + dr, :]
                    for sub in range(nsub):
                        pscs = [PF4.get(), PF4.get()]
                        for c_ in range(2):
                            tb = sub * 128 + c_ * 64
                            rows = slice(c_ * 64, (c_ + 1) * 64)
                            for hh in range(2):
                                hs = slice(hh * 64, (hh + 1) * 64)
                                psc, bpsc = pscs[hh]
                                tfull, tz = (1, 0) if dr == 0 else (0, 1)
                                mm(psc[rows, tfull * 32:(tfull + 1) * 32], ktl_t[hs, tb:tb + 64], qtl_t[hs, tb + tfull * 32:tb + (tfull + 1) * 32], True, True, [b_ktl, b_qtl], [bpsc])
                                mm(psc[rows, tz * 32:(tz + 1) * 32], kz[dr][hs, tb:tb + 64], qtl_t[hs, tb + tz * 32:tb + (tz + 1) * 32], True, True, [b_kz[dr], b_qtl], [bpsc])
                        for hh in range(2):
                            psc, bpsc = pscs[hh]
                            tt("dve", scm[:, hh * 64:(hh + 1) * 64], psc[:, 0:64], mk[:, 0:64], ALU.mult, [bpsc, b_cst], [b_scm])
                        for c_ in range(2):
                            ps_ = slice(c_ * 64, (c_ + 1) * 64)
                            for hh in range(2):
                                hs = slice(hh * 64, (hh + 1) * 64)
                                vcol = slice((hp * 2 + hh) * 64, (hp * 2 + hh + 1) * 64)
                                mm(pu_ps[c_][hs, sub * 64:(sub + 1) * 64], khtm[ps_, sub, hs], itm[ps_, sub, vcol], True, True, [b_khtm, b_itm], [b_pu[c_]])
                                mm(po_ps[c_][hs, sub * 64:(sub + 1) * 64], itm[ps_, sub, vcol], scm[ps_, hs], True, True, [b_itm, b_scm], [b_po[c_]])
                    for c_ in range(2):
                        cp("dve", Ut[:, 0:nch, :].rearrange("p (s c) d -> p s c d", c=2)[:, :, c_, :], pu_ps[c_][:, 0:nsub * 64].rearrange("p (s d) -> p s d", d=64), [b_pu[c_]], [b_U])
                    S.dma("sp", U_s[dr, hp, :, c0:c0 + nch, :], Ut[:, 0:nch, :], r=[b_U], w=[DB("U", dr, hp, t0)])
                    for c_ in range(2):
                        ov = osb[:, 0:n].rearrange("p (s c t) -> p s c t", c=2, t=64)[:, :, c_, :]
                        pv_ = po_ps[c_][:, 0:nsub * 64].rearrange("p (s t) -> p s t", t=64)
                        if dr == 0:
                            act(ov, pv_, AF.Copy, [b_po[c_]], [b_osb])
                        else:
                            tt("dve", ov, ov, pv_, ALU.add, [b_osb, b_po[c_]], [b_osb])
                S.dma("sp", oacc_s[hp, :, t0:t0 + n], osb[:, 0:n], r=[b_osb], w=[DB("oacc", hp, t0)])
            chk("A5")
            zoff = t0 + 3 if isc else t0 + 1
            for cc in range(2):
                pf, bpf = proj(3328 + cc * 128)
                act(cs_t[:, 0:n], pf[:, 0:n], AF.Copy, [bpf], [b_cs])
                pf, bpf = proj(3584 + cc * 128)
                tt("dve", cs_t[:, 0:n], cs_t[:, 0:n], pf[:, 0:n], ALU.mult, [b_cs, bpf], [b_cs])
                S.dma("pool", z_s[cc, :, zoff:zoff + n], cs_t[:, 0:n], r=[b_cs], w=[DB("z", cc, t0)])
                pf, bpf = proj(3072 + cc * 128)
                act(cb_t[:, 0:n], pf[:, 0:n], AF.Copy, [bpf], [b_cb])
                S.dma("pool", cb_s[cc, :, t0:t0 + n], cb_t[:, 0:n], r=[b_cb], w=[DB("cb", cc, t0)])
        try:
            for (t0_, n_) in (TILES[:1] if (stop or "").startswith("A") and stop != "A" else TILES):
                do_tile(t0_, n_)
        except _Stop:
            raise _Stop()
        fence(S)
        mem.reset(PBASE)

        if stop == "A":
            raise _Stop()
        NP_ = len(rm_patterns)
        ttb = mem.sb([128, 6 * 22 * 64], BF16); b_tt = Buf()
        stg2 = RR(mem, [128, 2112], F32, 2)
        for i4 in range(4):
            st_, bst = stg2.get()
            S.dma("sp", st_[:, :], tt_d[:, i4 * 2112:(i4 + 1) * 2112], w=[bst])
            cp("dve", ttb[:, i4 * 2112:(i4 + 1) * 2112], st_[:, :], [bst], [b_tt])
        rmaf = mem.sb([128, NP_, 128]); rmab = mem.sb([128, NP_, 128], BF16); rmbf = mem.sb([128, 512]); rmbb = mem.sb([128, 512], BF16); b_rm = Buf()
        S.dma("sp", rmaf[:, :, :], rma_d[:, :, :].rearrange("n k m -> k n m"), w=[b_rm])
        S.dma("sp", rmbf[:, :], rmb_d[:, :], w=[b_rm])
        cp("dve", rmab[:, :, :], rmaf[:, :, :], [b_rm], [b_rm])
        cp("dve", rmbb[:, :], rmbf[:, :], [b_rm], [b_rm])
        kcx = mem.sb([128, 3, 256], BF16); vcx = mem.sb([128, 2, 384], BF16); b_cxkv = Buf()
        S.dma("sp", kcx[:, :, :], KT_s[:, :, NLAT:NTOK].rearrange("c p t -> p c t"), r=[DB("K", pr, NLAT) for pr in range(3)], w=[b_cxkv])
        S.dma("sp", vcx[:, :, :], V_s[NLAT:NTOK, :].rearrange("(s t) c -> t s c", t=128), r=[DB("V", NLAT, s_) for s_ in range(2)], w=[b_cxkv])
        qp = RR(mem, [128, 6, 512], BF16, 2)
        kp = RR(mem, [128, 3, 1024], BF16, 2)
        vp = RR(mem, [128, 8, 384], BF16, 2)
        ptp = RR(mem, [128, 512], BF16, 3)
        rin_t, b_rin = T512()
        ob_p = RR(mem, [128, 512], BF16, 2)
        PS2 = PRR(pfs[0:2])
        pos = PRR(pfs[2:4])
        pds = PRR(pfs[4:6])

        def rowpat(g, p):
            out = []
            for a in range(2):
                for i in range(8):
                    kr = 8 * g - 4 + 2 * p + a
                    rq = 8 * g + i
                    rs0 = min(max(rq - 4, 0), NROWS - 8)
                    out.append(rs0 <= kr < rs0 + 8)
            return tuple(out)

        for (t0, n) in TILES:
            isc = t0 >= NLAT
            g = t0 // 512
            qt, bq = qp.get()
            S.dma("sp", qt[:, :, 0:n], QT_s[:, :, :, t0:t0 + n].rearrange("c h p t -> p (c h) t"),
                  r=[DB(nm, pr, hh, t0) for nm in ("Q", "Qz") for pr in range(3) for hh in range(2)], w=[bq])
            chunks = []
            if not isc:
                ps_valid = [p for p in range(8) if 0 <= 8 * g - 4 + 2 * p < NROWS]
                p_lo, p_hi = ps_valid[0], ps_valid[-1]
                k0 = (8 * g - 4) * 64
                kt, bk = kp.get()
                vt_, bv = vp.get()
                tl, th = k0 + p_lo * 128, k0 + (p_hi + 1) * 128
                tiles_touched = sorted(set([(tl // 512) * 512, ((th - 1) // 512) * 512, (((tl + th) // 2) // 512) * 512]))
                S.dma("pool", kt[:, :, p_lo * 128:(p_hi + 1) * 128], KT_s[:, :, tl:th].rearrange("c p t -> p c t"),
                      r=[DB("K", pr, tt0) for pr in range(3) for tt0 in tiles_touched], w=[bk])
                S.dma("pool", vt_[:, p_lo:p_hi + 1, :], V_s[tl:th, :].rearrange("(s t) c -> t s c", t=128),
                      r=[DB("V", tt0, s_) for tt0 in tiles_touched for s_ in range(4)], w=[bv])
                chunks = [("loc", p) for p in ps_valid]
            chunks += [("ctx", 0), ("ctx", 1)]
            for pr in range(3):
                po_, bpo = pos.get()
                pd_, bpd = pds.get()
                for hh in range(2):
                    h = pr * 2 + hh
                    hs = slice(hh * 64, (hh + 1) * 64)
                    for ci, (kind, p) in enumerate(chunks):
                        st_ps, bst_ps = PS2.get()
                        first, last = ci == 0, ci == len(chunks) - 1
                        if kind == "loc":
                            mm(st_ps[:, 0:n], kt[:, pr, p * 128:(p + 1) * 128], qt[:, h, 0:n], True, False, [bk, bq], [bst_ps])
                            u0 = 14 - 2 * p
                            mm(st_ps[:, 0:n], idb, ttb[:, (h * 22 + u0) * 64:(h * 22 + u0 + 8) * 64], False, False, [b_cst, b_tt], [bst_ps])
                            pat = rm_patterns.index(rowpat(g, p))
                            mm(st_ps[:, 0:n], rmab[:, pat, :], rmbb[:, :], False, True, [b_rm], [bst_ps])
                            vl = vt_[:, p, h * 64:(h + 1) * 64]
                            rv = [bv]
                        else:
                            mm(st_ps[:, 0:n], kcx[:, pr, p * 128:(p + 1) * 128], qt[:, h, 0:n], True, True, [b_cxkv, bq], [bst_ps])
                            vl = vcx[:, p, h * 64:(h + 1) * 64]
                            rv = [b_cxkv]
                        pT, bpT = ptp.get()
                        act(pT[:, 0:n], st_ps[:, 0:n], AF.Exp, [bst_ps], [bpT])
                        mm(po_[hs, 0:n], vl, pT[:, 0:n], first, last, rv + [bpT], [bpo])
                        mm(pd_[hs, 0:n], onesb[:, 0:64], pT[:, 0:n], first, last, [b_onesb, bpT], [bpd])
                recip(rin_t[:, 0:n], pd_[:, 0:n], [bpd], [b_rin])
                ob, bob = ob_p.get()
                tt("dve", ob[:, 0:n], po_[:, 0:n], rin_t[:, 0:n], ALU.mult, [bpo, b_rin], [bob])
                S.dma("sp", mix_s[pr, :, t0:t0 + n], ob[:, 0:n], r=[bob], w=[DB("mix", pr, t0)])
        fence(S)
        mem.reset(PBASE)

        if stop == "B":
            raise _Stop()
        S32 = [[mem.sb([128, 64]) for _ in range(3)] for _ in range(2)]
        Sbf = [[mem.sb([128, 64], BF16) for _ in range(3)] for _ in range(2)]
        bS = [[Buf() for _ in range(3)] for _ in range(2)]
        bSb = [[Buf() for _ in range(3)] for _ in range(2)]
        qh_p = RR(mem, [128, 3, 512], BF16, 2)
        U_p = RR(mem, [128, 3, 8, 64], F32, 2)
        D_p = RR(mem, [128, 3, 8], F32, 2)
        oa_p = RR(mem, [128, 3, 512], F32, 2)
        gsl_p = RR(mem, [128, 3, 512], F32, 2)
        sq2, b_sq2 = T512(BF16)
        rt2, b_rt2 = T512()
        y2, b_y2 = T512()
        yb_p = RR(mem, [128, 512], BF16, 2)
        pin = [pfs[0], pfs[1]]
        bpin = [Buf(), Buf()]
        PSX = PRR(pfs[2:6])
        for dr in range(2):
            for hp in range(3):
                S.op("pool", lambda e, t=S32[dr][hp]: e.memset(t[:, :], 0.0), w=[bS[dr][hp]])
                S.op("pool", lambda e, t=Sbf[dr][hp]: e.memset(t[:, :], 0.0), w=[bSb[dr][hp]])
            order = [TILES[-1]] + (TILES[:-1] if dr == 0 else TILES[:-1][::-1])
            for (t0, n) in order:
                nch = n // 64
                c0 = t0 // 64
                qh, bqh = qh_p.get()
                Uu, bUu = U_p.get()
                Dd, bDd = D_p.get()
                oa, boa = oa_p.get()
                S.dma("sp", qh[:, :, 0:n], qhat_s[dr, :, :, t0:t0 + n].rearrange("c p t -> p c t"), r=[DB("qh", dr, hp, t0) for hp in range(3)], w=[bqh])
                S.dma("pool", Uu[:, :, 0:nch, :], U_s[dr, :, :, c0:c0 + nch, :].rearrange("c p a b -> p c a b"), r=[DB("U", dr, hp, t0) for hp in range(3)], w=[bUu])
                S.dma("pool", Dd[:, :, 0:nch], D_s[dr, :, :, c0:c0 + nch].rearrange("c p a -> p c a"), r=[DB("D", dr, hp, t0) for hp in range(3)], w=[bDd])
                S.dma("sp", oa[:, :, 0:n], oacc_s[:, :, t0:t0 + n].rearrange("c p t -> p c t"), r=[DB("oacc", hp, t0) for hp in range(3)], w=[boa])
                if dr == 1:
                    gl, bgl = gsl_p.get()
                    S.dma("sp", gl[:, :, 0:n], gs_s[:, :, t0:t0 + n].rearrange("c p t -> p c t"), r=[DB("gs", hp, t0) for hp in range(3)], w=[bgl])
                corder = list(range(nch)) if dr == 0 else list(range(nch))[::-1]
                for hp in range(3):
                    for c in corder:
                        for hh in range(2):
                            hs = slice(hh * 64, (hh + 1) * 64)
                            mm(pin[hh][hs, c * 64:(c + 1) * 64], Sbf[dr][hp][hs, :], qh[hs, hp, c * 64:(c + 1) * 64], True, True, [bSb[dr][hp], bqh], [bpin[hh]])
                        stt("dve", S32[dr][hp][:, :], S32[dr][hp][:, :], Dd[:, hp, c:c + 1], Uu[:, hp, c, :], ALU.mult, ALU.add, [bS[dr][hp], bDd, bUu], [bS[dr][hp]])
                        cp("pool", Sbf[dr][hp][:, :], S32[dr][hp][:, :], [bS[dr][hp]], [bSb[dr][hp]])
                    for hh in range(2):
                        hs = slice(hh * 64, (hh + 1) * 64)
                        tt("dve", oa[hs, hp, 0:n], oa[hs, hp, 0:n], pin[hh][hs, 0:n], ALU.add, [boa, bpin[hh]], [boa])
                if dr == 0:
                    S.dma("sp", oacc_s[:, :, t0:t0 + n].rearrange("c p t -> p c t"), oa[:, :, 0:n], r=[boa], w=[DB("oacc", hp, t0) for hp in range(3)])
                else:
                    for hp in range(3):
                        act(sq2[:, 0:n], oa[:, hp, 0:n], AF.Square, [boa], [b_sq2])
                        pss, bpss = PSX.get()
                        mm(pss[:, 0:n], blkb, sq2[:, 0:n], True, True, [b_cst, b_sq2], [bpss])
                        act(rt2[:, 0:n], pss[:, 0:n], AF.Sqrt, [bpss], [b_rt2], scale=1.0 / 64, bias=1e-6)
                        recip(rt2[:, 0:n], rt2[:, 0:n], [b_rt2], [b_rt2])
                        stt("dve", y2[:, 0:n], oa[:, hp, 0:n], hvec[:, 2:3], rt2[:, 0:n], ALU.mult, ALU.mult, [boa, b_rt2, b_hv], [b_y2])
                        yb, byb = yb_p.get()
                        tt("pool", yb[:, 0:n], y2[:, 0:n], gl[:, hp, 0:n], ALU.mult, [b_y2, bgl], [byb])
                        S.dma("sp", mix_s[3 + hp, :, t0:t0 + n], yb[:, 0:n], r=[byb], w=[DB("mix", 3 + hp, t0)])
        fence(S)
        mem.reset(PBASE)

        if stop == "C":
            raise _Stop()
        zw_p = RR(mem, [128, 514], F32, 2)
        cbl_p = RR(mem, [128, 512], BF16, 2)
        y3, b_y3 = T512()
        yc_p = RR(mem, [128, 512], BF16, 2)
        for (t0, n) in TILES:
            isc = t0 >= NLAT
            zoff = t0 + 3 if isc else t0 + 1
            for cc in range(2):
                zw, bzw = zw_p.get()
                cbl, bcbl = cbl_p.get()
                rz = [DB("z", cc, tt0) for tt0 in (t0 - 512, t0, t0 + 512) if 0 <= tt0 < NLAT or tt0 == t0] + [DB("z", cc, "pad", col) for col in (0, NLAT + 1, NLAT + 2, NTOK + 3)]
                S.dma("sp", zw[:, 0:n + 2], z_s[cc, :, zoff - 1:zoff + n + 1], r=rz, w=[bzw])
                S.dma("pool", cbl[:, 0:n], cb_s[cc, :, t0:t0 + n], r=[DB("cb", cc, t0)], w=[bcbl])
                ts("dve", y3[:, 0:n], zw[:, 0:n], cw[:, cc, 0:1], None, ALU.mult, None, [bzw, b_cw], [b_y3])
                stt("dve", y3[:, 0:n], zw[:, 1:n + 1], cw[:, cc, 1:2], y3[:, 0:n], ALU.mult, ALU.add, [bzw, b_cw, b_y3], [b_y3])
                stt("dve", y3[:, 0:n], zw[:, 2:n + 2], cw[:, cc, 2:3], y3[:, 0:n], ALU.mult, ALU.add, [bzw, b_cw, b_y3], [b_y3])
                yc, byc = yc_p.get()
                tt("pool", yc[:, 0:n], y3[:, 0:n], cbl[:, 0:n], ALU.mult, [b_y3, bcbl], [byc])
                S.dma("sp", mix_s[6 + cc, :, t0:t0 + n], yc[:, 0:n], r=[byc], w=[DB("mix", 6 + cc, t0)])
        fence(S)
        mem.reset(PBASE)

        if stop == "D":
            raise _Stop()
        Wob = mem.sb([128, 8, 1024], BF16); b_Wo = Buf()
        stg3 = RR(mem, [128, 1024], F32, 2)
        for k in range(8):
            st_, bst = stg3.get()
            S.dma("sp", st_[:, :], wout_d[k * 128:(k + 1) * 128, :], w=[bst])
            cp("pool", Wob[:, k, :], st_[:, :], [bst], [b_Wo])
        wrf = mem.sb([128, 8, 16]); wrb = mem.sb([128, 8, 16], BF16); b_wr = Buf()
        S.dma("sp", wrf[:, :, :], wr_d[:, :].rearrange("(k p) e -> p k e", p=128), w=[b_wr])
        cp("dve", wrb[:, :, :], wrf[:, :, :], [b_wr], [b_wr])
        mx_p = RR(mem, [128, 8, 128], BF16, 2)
        xt_p = RR(mem, [128, 1024], F32, 2)
        xm_p = RR(mem, [128, 1024], F32, 2)
        nsm2 = norm_scratch(mem)
        h2_p = RR(mem, [128, 8, 128], BF16, 2)
        lmax = mem.sb([128, 1]); lsm = mem.sb([128, 1]); ex_t = mem.sb([128, 16]); b_sm = Buf()
        af_p = RR(mem, [128, 16], F32, 2)
        PE6 = PRR(pfs[0:6])
        for (t0, n) in TILES:
            isc = t0 >= NLAT
            r = 1 if isc else 0
            src, so = xsrc(t0, n)
            for sub in range(n // 128):
                ta = t0 + sub * 128
                mx, bmx = mx_p.get()
                S.dma("sp", mx[:, :, :], mix_s[:, :, ta:ta + 128].rearrange("k p t -> p k t"), r=[DB("mix", k, t0) for k in range(8)], w=[bmx])
                xt, bx = xt_p.get()
                S.dma("pool", xt[:, :], src[so + sub * 128:so + (sub + 1) * 128, :], w=[bx])
                xm, bxm = xm_p.get()
                for half in range(2):
                    pso, bpso = PE6.get()
                    for k in range(8):
                        mm(pso[:, :], mx[:, k, :], Wob[:, k, half * 512:(half + 1) * 512], k == 0, k == 7, [bmx, b_Wo], [bpso])
                    tt("dve", xm[:, half * 512:(half + 1) * 512], pso[:, :], G1[:, r, half * 512:(half + 1) * 512], ALU.mult, [bpso, b_G], [bxm])
                tt("pool", xm[:, :], xm[:, :], xt[:, :], ALU.add, [bxm, bx], [bxm])
                S.dma("sp", xm_s[ta:ta + 128, :], xm[:, :], r=[bxm], w=[DB("xm", ta)])
                h2, bh2 = h2_p.get()
                norm_tile(None, 0, r, A2, 3, xm, bxm, nsm2, h2, bh2, 0)
                S.dma("pool", h2_s[:, :, ta:ta + 128].rearrange("k p t -> p k t"), h2[:, :, :], r=[bh2], w=[DB("h2", ta)])
                pl, bpl = PE6.get()
                for k in range(8):
                    mm(pl[:, 0:16], h2[:, k, :], wrb[:, k, :], k == 0, k == 7, [bh2, b_wr], [bpl])
                S.op("dve", lambda e, pl=pl: e.tensor_reduce(out=lmax[:, :], in_=pl[:, 0:16], axis=AX.X, op=ALU.max), r=[bpl], w=[b_sm])
                ts("dve", lmax[:, :], lmax[:, :], -1.0, None, ALU.mult, None, [b_sm], [b_sm])
                act(ex_t[:, :], pl[:, 0:16], AF.Exp, [bpl, b_sm], [b_sm], bias=lmax[:, 0:1], accum_out=lsm[:, :])
                recip(lsm[:, :], lsm[:, :], [b_sm], [b_sm])
                af, baf = af_p.get()
                ts("dve", af[:, :], ex_t[:, :], lsm[:, 0:1], None, ALU.mult, None, [b_sm], [baf])
                S.dma("pool", aff_s[ta:ta + 128, :], af[:, :], r=[baf], w=[DB("aff", ta)])
        fence(S)
        mem.reset(PBASE)

        if stop == "E":
            raise _Stop()
        mF = mem.mark()
        stg4 = RR(mem, [128, 8, 512], F32, 2)
        cst4 = RR(mem, [128, 8, 512], BF16, 2)
        for ex in range(NEXP):
            for (wsrc, wdst, nm) in ((wg_d, wgb_s, "wg"), (wu_d, wub_s, "wu")):
                st_, bst = stg4.get()
                S.dma("sp", st_[:, :, :], wsrc[ex, :, :].rearrange("(k p) f -> p k f", p=128), w=[bst])
                cb_, bcb_ = cst4.get()
                cp("pool" if nm == "wg" else "dve", cb_[:, :, :], st_[:, :, :], [bst], [bcb_])
                S.dma("pool", wdst[ex, :, :, :], cb_[:, :, :], r=[bcb_], w=[DB(nm, ex)])
            st_, bst = stg4.get()
            S.dma("sp", st_[:, 0:4, :].rearrange("p k (a f) -> p k a f", a=1), wd_d[ex, :, :].rearrange("(k p) (a f) -> p k a f", p=128, a=1)[:, :, :, 0:512], w=[bst])
            S.dma("sp", st_[:, 4:8, :].rearrange("p k (a f) -> p k a f", a=1), wd_d[ex, :, :].rearrange("(k p) (a f) -> p k a f", p=128, a=2)[:, :, 1:2, :], w=[bst])
            cb_, bcb_ = cst4.get()
            cp("pool", cb_[:, :, :], st_[:, :, :], [bst], [bcb_])
            S.dma("pool", wdb_s[ex, :, :, 0:512], cb_[:, 0:4, :], r=[bcb_], w=[DB("wd0", ex)])
            S.dma("pool", wdb_s[ex, :, :, 512:1024], cb_[:, 4:8, :], r=[bcb_], w=[DB("wd1", ex)])
        fence(S)
        mem.reset(mF)
        thr = mem.sb([128, 2, 16]); b_thr = Buf()
        mB = mem.mark()
        for (r, tok0, ntok, cap) in ((0, 0, NLAT, 2 * NLAT // NEXP), (1, NLAT, NCTX, 2 * NCTX // NEXP)):
            per = ntok // 128
            afa = mem.sb([128, per, 16]); b_afa = Buf()
            S.dma("sp", afa[:, :, :], aff_s[tok0:tok0 + ntok, :].rearrange("(p t) e -> p t e", p=128), r=[DB("aff", ta) for ta in range(tok0, tok0 + ntok, 128)], w=[b_afa])
            afv = afa[:, :, :].rearrange("p t e -> p e t")
            cmp_ = mem.sb([128, 16, per]); b_cmp = Buf()
            lo = mem.sb([128, 16]); hi = mem.sb([128, 16]); mid = mem.sb([128, 16]); cnt = mem.sb([128, 16]); prd = mem.sb([128, 16]); dlt = mem.sb([128, 16])
            b_b = Buf()
            S.op("dve", lambda e, lo=lo: e.memset(lo[:, :], 0.0), w=[b_b])
            S.op("dve", lambda e, hi=hi: e.memset(hi[:, :], 1.0), w=[b_b])
            for it in range(30):
                tt("dve", mid[:, :], lo[:, :], hi[:, :], ALU.add, [b_b], [b_b])
                ts("dve", mid[:, :], mid[:, :], 0.5, None, ALU.mult, None, [b_b], [b_b])
                tt("dve", cmp_[:, :, :], afv, mid[:, :].unsqueeze(2).to_broadcast([128, 16, per]), ALU.is_ge, [b_afa, b_b], [b_cmp])
                S.op("dve", lambda e, cnt=cnt, cmp_=cmp_: e.tensor_reduce(out=cnt[:, :], in_=cmp_[:, :, :], axis=AX.X, op=ALU.add), r=[b_cmp], w=[b_b])
                pc, bpc = PE6.get()
                mm(pc[:, 0:16], onesf[:, :], cnt[:, :], True, True, [b_onesf, b_b], [bpc])
                ts("dve", prd[:, :], pc[:, 0:16], float(cap) - 0.5, None, ALU.is_ge, None, [bpc], [b_b])
                tt("dve", dlt[:, :], mid[:, :], lo[:, :], ALU.subtract, [b_b], [b_b])
                tt("dve", dlt[:, :], dlt[:, :], prd[:, :], ALU.mult, [b_b], [b_b])
                tt("dve", lo[:, :], lo[:, :], dlt[:, :], ALU.add, [b_b], [b_b])
                tt("dve", dlt[:, :], hi[:, :], mid[:, :], ALU.subtract, [b_b], [b_b])
                tt("dve", dlt[:, :], dlt[:, :], prd[:, :], ALU.mult, [b_b], [b_b])
                tt("dve", hi[:, :], mid[:, :], dlt[:, :], ALU.add, [b_b], [b_b])
            cp("dve", thr[:, r, :], lo[:, :], [b_b], [b_thr])
            fence(S)
            mem.reset(mB)
        h2l_p = RR(mem, [128, 8, 1024], BF16, 2)
        acc = mem.sb([128, 8, 1024]); b_acc = Buf()
        sgate = mem.sb([128, 8, 16]); afo = mem.sb([128, 8, 16]); b_sg2 = Buf()
        wgl_p = RR(mem, [128, 8, 512], BF16, 2)
        wul_p = RR(mem, [128, 8, 512], BF16, 2)
        wdl_p = RR(mem, [128, 4, 1024], BF16, 2)
        hid_p = RR(mem, [128, 4, 512], BF16, 2)
        sl_t, b_sl = T512()
        xo_p = RR(mem, [128, 1024], F32, 2)
        STILES = [(i * 1024, 1024) for i in range(NLAT // 1024)] + [(NLAT, 256)]
        for (t0, n) in STILES:
            isc = t0 >= NLAT
            r = 1 if isc else 0
            nsub = n // 128
            h2l, bh2l = h2l_p.get()
            S.dma("sp", h2l[:, :, 0:n], h2_s[:, :, t0:t0 + n].rearrange("k p t -> p k t"), r=[DB("h2", ta) for ta in range(t0, t0 + n, 128)], w=[bh2l])
            S.dma("pool", afo[:, 0:nsub, :], aff_s[t0:t0 + n, :].rearrange("(s t) e -> t s e", t=128), r=[DB("aff", ta) for ta in range(t0, t0 + n, 128)], w=[b_sg2])
            tt("dve", sgate[:, 0:nsub, :], afo[:, 0:nsub, :], thr[:, r, :].unsqueeze(1).to_broadcast([128, nsub, 16]), ALU.is_ge, [b_sg2, b_thr], [b_sg2])
            tt("dve", sgate[:, 0:nsub, :], sgate[:, 0:nsub, :], afo[:, 0:nsub, :], ALU.mult, [b_sg2], [b_sg2])
            S.op("pool", lambda e: e.memset(acc[:, :, :], 0.0), w=[b_acc])
            for ex in range(NEXP):
                wgl, bwg = wgl_p.get()
                wul, bwu = wul_p.get()
                wdl, bwd = wdl_p.get()
                S.dma("sp", wgl[:, :, :], wgb_s[ex, :, :, :], r=[DB("wg", ex)], w=[bwg])
                S.dma("pool", wul[:, :, :], wub_s[ex, :, :, :], r=[DB("wu", ex)], w=[bwu])
                S.dma("sp", wdl[:, :, :], wdb_s[ex, :, :, :], r=[DB("wd0", ex), DB("wd1", ex)], w=[bwd])
                for tt0 in range(0, n, 512):
                    nn = min(512, n - tt0)
                    hid, bhid = hid_p.get()
                    for fc in range(4):
                        pg_, bpg_ = PE6.get()
                        for k in range(8):
                            mm(pg_[:, 0:nn], wgl[:, k, fc * 128:(fc + 1) * 128], h2l[:, k, tt0:tt0 + nn], k == 0, k == 7, [bwg, bh2l], [bpg_])
                        pu2, bpu2 = PE6.get()
                        for k in range(8):
                            mm(pu2[:, 0:nn], wul[:, k, fc * 128:(fc + 1) * 128], h2l[:, k, tt0:tt0 + nn], k == 0, k == 7, [bwu, bh2l], [bpu2])
                        act(sl_t[:, 0:nn], pg_[:, 0:nn], AF.Silu, [bpg_], [b_sl])
                        tt("dve", hid[:, fc, 0:nn], sl_t[:, 0:nn], pu2[:, 0:nn], ALU.mult, [b_sl, bpu2], [bhid])
                    for sub in range(nn // 128):
                        sa = (tt0 // 128) + sub
                        for half in range(2):
                            pdn, bpdn = PE6.get()
                            for fc in range(4):
                                mm(pdn[:, :], hid[:, fc, sub * 128:(sub + 1) * 128], wdl[:, fc, half * 512:(half + 1) * 512], fc == 0, fc == 3, [bhid, bwd], [bpdn])
                            stt("dve", acc[:, sa, half * 512:(half + 1) * 512], pdn[:, :], sgate[:, sa, ex:ex + 1], acc[:, sa, half * 512:(half + 1) * 512], ALU.mult, ALU.add, [bpdn, b_sg2, b_acc], [b_acc])
            dst, do = xdst(t0)
            for sub in range(nsub):
                ta = t0 + sub * 128
                xo, bxo = xo_p.get()
                S.dma("sp", xo[:, :], xm_s[ta:ta + 128, :], r=[DB("xm", ta)], w=[bxo])
                tt("pool", acc[:, sub, :], acc[:, sub, :], G2[:, r, :], ALU.mult, [b_acc, b_G], [b_acc])
                tt("pool", xo[:, :], xo[:, :], acc[:, sub, :], ALU.add, [bxo, b_acc], [bxo])
                S.dma("sp", dst[do + sub * 128:do + (sub + 1) * 128, :], xo[:, :], r=[bxo], final=True)

    LBASE = mem.mark()
    try:
        for li, layer in enumerate(layers):
            first, last = li == 0, li == len(layers) - 1
            xin = x_in_d if first else xbuf[(li - 1) % 2]
            cxin = cx_in_d if first else cxbuf[(li - 1) % 2]
            xout = xo_out_d if last else xbuf[li % 2]
            cxout = cxo_out_d if last else cxbuf[li % 2]
            layer_body(layer, xin, cxin, xout, cxout, wmod_a[li], bmodT_a[li], n1T_a[li], n2T_a[li], win_a[li], wout_a[li], wr_a[li],
                       hvec_a[li], cw_a[li], tt_a[li], None if early else wg_a[li], None if early else wu_a[li], None if early else wd_a[li])
            fence(S)
            mem.reset(LBASE)
    except _Stop:
        pass
    S.emit()
    return nc


def _rm_patterns(nlat=16384):
    NROWS = nlat // 64
    pats = []
    for g in range(nlat // 512):
        for p in range(8):
            if not (0 <= 8 * g - 4 + 2 * p < NROWS):
                continue
            t = []
            for a in range(2):
                for i in range(8):
                    kr = 8 * g - 4 + 2 * p + a
                    rq = 8 * g + i
                    rs0 = min(max(rq - 4, 0), NROWS - 8)
                    t.append(rs0 <= kr < rs0 + 8)
            t = tuple(t)
            if t not in pats:
                pats.append(t)
    return pats


def _consts(pats, nlat=16384):
    NLAT = nlat
    NTOK = NLAT + NCTX
    NEG = -30000.0
    idn = np.eye(128, dtype=np.float32)
    blk = np.zeros((128, 128), np.float32)
    blk[:64, :64] = 1
    blk[64:, 64:] = 1
    rot = np.zeros((128, 128), np.float32)
    for hb in (0, 64):
        for m in range(64):
            q = m // 16
            if q in (0, 2):
                rot[hb + m + 16, hb + m] = -1.0
            else:
                rot[hb + m - 16, hb + m] = 1.0
    s = np.arange(64)
    mf = (s[:, None] <= s[None, :]).astype(np.float32)
    mb = (s[:, None] >= s[None, :]).astype(np.float32)
    cst = np.stack([idn, blk, rot, np.tile(mf, (2, 2)), np.tile(mb, (2, 2))]).astype(np.float32)
    rma = np.zeros((len(pats), 128, 128), np.float32)
    for pi, pt in enumerate(pats):
        for a in range(2):
            for i in range(8):
                if not pt[a * 8 + i]:
                    rma[pi, a * 8 + i, a * 64:(a + 1) * 64] = NEG
    rmb = np.zeros((128, 8, 64), np.float32)
    for a in range(2):
        for i in range(8):
            rmb[a * 8 + i, i, :] = 1.0
    rmb = rmb.reshape(128, 512)
    t = np.arange(NLAT)
    row = (t // 64).astype(np.float32)
    col = (t % 64).astype(np.float32)
    inv = (np.float32(10000.0) ** (-np.arange(16, dtype=np.float32) / np.float32(16))).astype(np.float32)
    ar = row[:, None] * inv
    ac = col[:, None] * inv
    ang = np.concatenate([ar, ar, ac, ac], axis=-1)
    cos = np.ones((128, NTOK), np.float32)
    sin = np.zeros((128, NTOK), np.float32)
    cos[:64, :NLAT] = np.cos(ang).T
    cos[64:, :NLAT] = np.cos(ang).T
    sin[:64, :NLAT] = np.sin(ang).T
    sin[64:, :NLAT] = np.sin(ang).T
    return cst, rma, rmb, cos, sin


def _tt_table(rpb):
    NEG = -30000.0
    tt = np.zeros((128, 6, 22, 64), np.float32)
    qc = np.arange(64)
    ws = np.clip(qc - 8, 0, 48)
    kc = np.arange(64)
    inwin = (kc[:, None] >= ws[None, :]) & (kc[:, None] < ws[None, :] + 16)
    rel = np.clip(kc[:, None] - qc[None, :] + 15, 0, 30)
    for a in range(2):
        for u in range(22):
            dr = a + 10 - u
            if abs(dr) <= 7:
                vals = rpb[:, dr + 7, :][:, rel]
                blk = np.where(inwin[None], vals, np.float32(NEG))
            else:
                blk = np.zeros((6, 64, 64), np.float32)
            tt[a * 64:(a + 1) * 64, :, u, :] = blk.transpose(1, 0, 2)
    return np.ascontiguousarray(tt.reshape(128, 6 * 22 * 64))


_NC_CACHE = {}


def _f(a):
    return np.ascontiguousarray(np.asarray(a, dtype=np.float32))


def _fm(v):
    return np.ascontiguousarray(_f(v).reshape(-1, 128).T)


def _common(layers, W, consts):
    cst, rma, rmb, cos, sin = consts
    lbl = np.ascontiguousarray(_f(W["hg_lb_logits"]).reshape(2, 4, 3, 128).transpose(3, 2, 0, 1))
    st = lambda fn: np.ascontiguousarray(np.stack([fn(l) for l in layers], axis=0))
    return {
        "wmod": st(lambda l: _f(W["w_mod"][l])), "bmodT": st(lambda l: _fm(W["b_mod"][l])),
        "n1T": st(lambda l: _fm(W["norm1_w"][l])), "n2T": st(lambda l: _fm(W["norm2_w"][l])),
        "win": st(lambda l: _f(W["w_in"][l])), "wout": st(lambda l: _f(W["w_out"][l])), "wr": st(lambda l: _f(W["w_router"][l])),
        "hvec": st(lambda l: np.stack([np.tile(_f(W["na_q_norm"][l]), 2), np.tile(_f(W["na_k_norm"][l]), 2),
                                       np.tile(_f(W["hg_norm"][l]), 2)], axis=1)),
        "lbl": lbl, "cw": st(lambda l: _f(W["conv_w"][l]).reshape(3, 2, 128).transpose(2, 1, 0)),
        "tt": st(lambda l: _tt_table(_f(W["na_rpb"][l]))), "rma": rma, "rmb": rmb, "cst": cst, "cos": cos, "sin": sin,
        "wg": st(lambda l: _f(W["w_exp_gate"][l])), "wu": st(lambda l: _f(W["w_exp_up"][l])), "wd": st(lambda l: _f(W["w_exp_down"][l])),
    }


def run_layer(nc, common, xs, cxs, c, c_ctx, ncores=2):
    names = set(a.memorylocations[0].name for a in nc.m.functions[0].allocations
                if isinstance(a, mybir.MemoryLocationSet) and a.kind == "ExternalInput")
    in_maps = []
    for core in range(ncores):
        b = core % 2
        c2 = np.stack([c[b], c_ctx], axis=0)
        c2T = np.ascontiguousarray(c2.reshape(2, 8, 128).transpose(2, 1, 0))
        d = dict(common)
        d.update({"x": xs[b], "cx": cxs[b], "c2T": c2T})
        in_maps.append({k: v for k, v in d.items() if k in names})
    return run_bass_kernel_spmd(nc, in_maps, core_ids=list(range(ncores)))


def kernel(x, c, ctx, c_ctx, w_mod, b_mod, norm1_w, w_in, na_q_norm, na_k_norm, na_rpb, hg_lb_logits, hg_norm, conv_w,
           w_out, norm2_w, w_router, w_exp_gate, w_exp_up, w_exp_down):
    W = dict(w_mod=w_mod, b_mod=b_mod, norm1_w=norm1_w, w_in=w_in, na_q_norm=na_q_norm, na_k_norm=na_k_norm, na_rpb=na_rpb,
             hg_lb_logits=hg_lb_logits, hg_norm=hg_norm, conv_w=conv_w, w_out=w_out, norm2_w=norm2_w, w_router=w_router,
             w_exp_gate=w_exp_gate, w_exp_up=w_exp_up, w_exp_down=w_exp_down)
    x = _f(x); ctx = _f(ctx); c = _f(c); c_ctx = _f(c_ctx)
    nlat = x.shape[1]
    pats = _rm_patterns(nlat)
    consts = _consts(pats, nlat)
    xs = [x[0], x[1]]
    cxs = [ctx[0], ctx[1]]
    layers = [0, 1, 2, 3]
    key = (tuple(layers), nlat)
    if key not in _NC_CACHE:
        _NC_CACHE[key] = build_model(layers, pats, nlat)
    res = run_layer(_NC_CACHE[key], _common(layers, W, consts), xs, cxs, c, c_ctx)
    xs = [np.asarray(res.results[b]["xo"], dtype=np.float32) for b in range(2)]
    return np.stack(xs, axis=0).astype(np.float32)
```

```python
import contextlib
import numpy as np
import concourse.bass as bass
import concourse.mybir as mybir
from concourse.bass_utils import run_bass_kernel_spmd

F32 = mybir.dt.float32
BF16 = mybir.dt.bfloat16
ALU = mybir.AluOpType
AF = mybir.ActivationFunctionType
AX = mybir.AxisListType

ENGS = ("sp", "act", "dve", "pool", "pe")
NSLOT = 8


class Buf:
    __slots__ = ("name", "w", "rs")

    def __init__(self, name=""):
        self.name = name
        self.w = None
        self.rs = []


class Ins:
    __slots__ = ("eng", "fn", "deps", "signal", "tick", "dma", "n", "k")


class Sched:
    def __init__(self, nc):
        self.nc = nc
        self.q = {e: [] for e in ENGS}
        self.ndma = {e: 0 for e in ENGS}
        self.stack = contextlib.ExitStack()
        self.finals = []
        self._nm = 0

    def sb(self, shape, dt=F32, name=None):
        self._nm += 1
        return self.stack.enter_context(self.nc.sbuf_tensor(name or f"sb{self._nm}", list(shape), dt))

    def ps(self, shape, dt=F32, name=None):
        self._nm += 1
        return self.stack.enter_context(self.nc.psum_tensor(name or f"ps{self._nm}", list(shape), dt))

    def dram(self, name, shape, dt=F32, kind="Internal"):
        return self.nc.dram_tensor(name, list(shape), dt, kind=kind)

    def op(self, eng, fn, r=(), w=(), dma=False):
        ins = Ins()
        ins.eng = eng
        ins.fn = fn
        ins.dma = dma
        ins.signal = False
        ins.tick = 0
        deps = {}
        for b in r:
            if b.w is not None:
                deps[id(b.w)] = b.w
        for b in w:
            if b.w is not None:
                deps[id(b.w)] = b.w
            for rd in b.rs:
                deps[id(rd)] = rd
        ins.deps = list(deps.values())
        for b in r:
            if not dma:
                b.rs = [x for x in b.rs if x.dma or x.eng != eng]
            b.rs.append(ins)
        for b in w:
            b.w = ins
            b.rs = []
        ins.k = len(self.q[eng])
        if dma:
            ins.n = self.ndma[eng]
            self.ndma[eng] += 1
        self.q[eng].append(ins)
        return ins

    def dma(self, eng, out, in_, r=(), w=(), final=False):
        ins = self.op(eng, lambda e: e.dma_start(out=out, in_=in_), r=r, w=w, dma=True)
        if final:
            self.finals.append(ins)
        return ins

    def emit(self):
        nc = self.nc
        for e in ENGS:
            for ins in self.q[e]:
                for d in ins.deps:
                    if d.dma:
                        continue
                    if d.eng == ins.eng:
                        if d.eng == "pe" and not ins.dma:
                            continue
                        if ins.dma or (ins.k - d.k) <= 8:
                            d.signal = True
                    else:
                        d.signal = True
        for e in ENGS:
            t = 0
            for ins in self.q[e]:
                if ins.signal and not ins.dma:
                    t += 1
                    ins.tick = t
        st = self.stack
        csem = {e: st.enter_context(nc.semaphore(f"c_{e}")) for e in ENGS}
        dsem = {e: [st.enter_context(nc.semaphore(f"d_{e}{i}")) for i in range(NSLOT)] for e in ("sp", "act", "pool")}
        engobj = {}
        finals = self.finals

        def run(e, eng):
            waited = {}

            def wait(sem, val):
                key = id(sem)
                if waited.get(key, 0) >= val:
                    return
                waited[key] = val
                eng.wait_ge(sem, val)

            for ins in self.q[e]:
                for d in ins.deps:
                    if d.dma:
                        wait(dsem[d.eng][d.n % NSLOT], 16 * (d.n // NSLOT + 1))
                    elif d.signal:
                        if d.eng == e and not ins.dma and (e == "pe" or (ins.k - d.k) > 8):
                            continue
                        wait(csem[d.eng], d.tick)
                if ins.dma:
                    if ins.n >= NSLOT:
                        wait(dsem[e][ins.n % NSLOT], 16 * (ins.n // NSLOT))
                    ins.fn(eng).then_inc(dsem[e][ins.n % NSLOT], 16)
                else:
                    bi = ins.fn(eng)
                    if ins.signal:
                        bi.then_inc(csem[e], 1)
            if e == "sp":
                for qe in ("sp", "act", "pool"):
                    n = self.ndma[qe]
                    for s in range(min(n, NSLOT)):
                        last = ((n - 1 - s) // NSLOT) * NSLOT + s
                        wait(dsem[qe][s], 16 * (last // NSLOT + 1))

        with nc.allow_non_contiguous_dma(reason="small strided scratch DMAs"), nc.Block() as block:
            @block.sync
            def _(eng):
                run("sp", eng)

            @block.scalar
            def _(eng):
                run("act", eng)

            @block.vector
            def _(eng):
                run("dve", eng)

            @block.gpsimd
            def _(eng):
                run("pool", eng)

            @block.tensor
            def _(eng):
                run("pe", eng)
        self.stack.close()


NLAT = 16384
NCTX = 256
NTOK = NLAT + NCTX
DM = 1024
NIN = 3840
NEXP = 16
EDIM = 512
TILES = [(i * 512, 512) for i in range(32)] + [(NLAT, 256)]
SB_BASE = 16512
SB_TOP = 229344


class Mem:
    def __init__(self, S):
        self.S = S
        self.off = SB_BASE
        self.n = 0

    def sb(self, shape, dt=F32, name=None):
        sz = int(np.prod(shape[1:])) * (2 if dt == BF16 else 4)
        sz = (sz + 31) // 32 * 32
        self.n += 1
        t = self.S.nc.alloc_sbuf_tensor_at(f"m{self.n}" + (("_" + name) if name else ""), list(shape), dt, offset=self.off)
        self.off += sz
        assert self.off <= SB_TOP, f"SBUF overflow {self.off}"
        return t

    def mark(self):
        return self.off

    def reset(self, m):
        self.off = m


class RR:
    def __init__(self, mem, shape, dt, n):
        self.t = [mem.sb(shape, dt) for _ in range(n)]
        self.b = [Buf() for _ in range(n)]
        self.i = 0

    def get(self):
        i = self.i
        self.i = (i + 1) % len(self.t)
        return self.t[i], self.b[i]


class PRR:
    def __init__(self, tiles):
        self.t = tiles
        self.b = [Buf() for _ in tiles]
        self.i = 0

    def get(self):
        i = self.i
        self.i = (i + 1) % len(self.t)
        return self.t[i], self.b[i]


def fence(S):
    lasts = []
    for e in ENGS:
        q = S.q[e]
        nd = 0
        seen_c = False
        for ins in reversed(q):
            if ins.dma:
                if nd < NSLOT:
                    lasts.append(ins)
                    nd += 1
            elif not seen_c:
                lasts.append(ins)
                seen_c = True
            if nd >= NSLOT and seen_c:
                break
    fb = Buf()
    for e in ENGS:
        ins = S.op(e, lambda eng: eng.nop(), r=(), w=())
        ins.deps = [d for d in lasts if d is not ins]


def build_model(layers, rm_patterns, nlat=16384, debug=False, stop=None):
    NL_ = len(layers)
    NLAT = nlat
    NTOK = NLAT + NCTX
    NROWS = NLAT // 64
    TILES = [(i * 512, 512) for i in range(NLAT // 512)] + [(NLAT, 256)]
    nc = bass.Bass("TRN2", target_bir_lowering=False)
    S = Sched(nc)
    mem = Mem(S)
    EI = "ExternalInput"

    class _Stop(Exception):
        pass

    def chk(name):
        if stop == name:
            raise _Stop()


    def din(name, shape, dt=F32):
        return nc.dram_tensor(name, list(shape), dt, kind=EI)

    x_in_d = din("x", [NLAT, DM])
    cx_in_d = din("cx", [NCTX, DM])
    c2T_d = din("c2T", [128, 8, 2])
    wmod_a = din("wmod", [NL_, DM, 6 * DM])
    bmodT_a = din("bmodT", [NL_, 128, 48])
    n1T_a = din("n1T", [NL_, 128, 8])
    n2T_a = din("n2T", [NL_, 128, 8])
    win_a = din("win", [NL_, DM, NIN])
    wout_a = din("wout", [NL_, DM, DM])
    wr_a = din("wr", [NL_, DM, NEXP])
    hvec_a = din("hvec", [NL_, 128, 3])
    lbl_d = din("lbl", [128, 3, 2, 4])
    cw_a = din("cw", [NL_, 128, 2, 3])
    tt_a = din("tt", [NL_, 128, 6 * 22 * 64])
    rma_d = din("rma", [len(rm_patterns), 128, 128])
    rmb_d = din("rmb", [128, 512])
    cst_d = din("cst", [5, 128, 128])
    cos_d = din("cos", [128, NTOK])
    sin_d = din("sin", [128, NTOK])
    early = stop is not None and (stop.startswith("A") or stop == "pro")
    wg_a = None if early else din("wg", [NL_, NEXP, DM, EDIM])
    wu_a = None if early else din("wu", [NL_, NEXP, DM, EDIM])
    wd_a = None if early else din("wd", [NL_, NEXP, EDIM, DM])
    xo_out_d = nc.dram_tensor("xo", [NLAT, DM], F32, kind="ExternalOutput")
    cxo_out_d = nc.dram_tensor("cxo", [NCTX, DM], F32, kind="ExternalOutput")
    xbuf = [nc.dram_tensor(f"xbuf{i}", [NLAT, DM], F32) for i in range(2)]
    cxbuf = [nc.dram_tensor(f"cxbuf{i}", [NCTX, DM], F32) for i in range(2)]

    def xsrc(t0, n):
        return (x_d, t0) if t0 < NLAT else (cx_d, t0 - NLAT)

    def xdst(t0):
        return (xo_d, t0) if t0 < NLAT else (cxo_d, t0 - NLAT)

    def scr(name, shape, dt=F32):
        if debug and name in ("mix_s", "xm_s", "aff_s", "QT_s", "KT_s", "V_s", "oacc_s", "h2_s"):
            return nc.dram_tensor(name, list(shape), dt, kind="ExternalOutput")
        return nc.dram_tensor(name, list(shape), dt)

    QT_s = scr("QT_s", [3, 2, 128, NTOK], BF16)
    KT_s = scr("KT_s", [3, 128, NTOK], BF16)
    V_s = scr("V_s", [NTOK, 384], BF16)
    mix_s = scr("mix_s", [8, 128, NTOK], BF16)
    oacc_s = scr("oacc_s", [3, 128, NTOK])
    qhat_s = scr("qhat_s", [2, 3, 128, NTOK], BF16)
    U_s = scr("U_s", [2, 3, 128, NTOK // 64, 64])
    D_s = scr("D_s", [2, 3, 128, NTOK // 64])
    gs_s = scr("gs_s", [3, 128, NTOK])
    z_s = scr("z_s", [2, 128, NTOK + 4])
    cb_s = scr("cb_s", [2, 128, NTOK], BF16)
    xm_s = scr("xm_s", [NTOK, DM])
    h2_s = scr("h2_s", [8, 128, NTOK], BF16)
    aff_s = scr("aff_s", [NTOK, NEXP])
    wgb_s = scr("wgb_s", [NEXP, 128, 8, EDIM], BF16)
    wub_s = scr("wub_s", [NEXP, 128, 8, EDIM], BF16)
    wdb_s = scr("wdb_s", [NEXP, 128, 4, DM], BF16)
    dbuf = {}

    def DB(*key):
        if key not in dbuf:
            dbuf[key] = Buf()
        return dbuf[key]

    pfs = [S.ps([128, 512], F32) for _ in range(6)]
    pbs = [S.ps([128, 8, 128], BF16) for _ in range(2)]
    PF = PRR(pfs)
    PB = PRR(pbs)

    def mm(out, lhsT, rhs, st, sp_, r, w):
        return S.op("pe", lambda e: e.matmul(out, lhsT, rhs, start=st, stop=sp_), r=r, w=w)

    def tr(out, in_, idn, r, w):
        return S.op("pe", lambda e: e.transpose(out=out, in_=in_, identity=idn), r=r, w=w)

    def act(out, in_, func, r, w, **kw):
        return S.op("act", lambda e: e.activation(out=out, in_=in_, func=func, **kw), r=r, w=w)

    def tt(eng, out, in0, in1, op, r, w):
        return S.op(eng, lambda e: e.tensor_tensor(out=out, in0=in0, in1=in1, op=op), r=r, w=w)

    def ts(eng, out, in0, s1, s2, op0, op1, r, w):
        if s2 is None:
            return S.op(eng, lambda e: e.tensor_scalar(out=out, in0=in0, scalar1=s1, scalar2=None, op0=op0), r=r, w=w)
        return S.op(eng, lambda e: e.tensor_scalar(out=out, in0=in0, scalar1=s1, scalar2=s2, op0=op0, op1=op1), r=r, w=w)

    def stt(eng, out, in0, sc, in1, op0, op1, r, w):
        return S.op(eng, lambda e: e.scalar_tensor_tensor(out=out, in0=in0, scalar=sc, in1=in1, op0=op0, op1=op1), r=r, w=w)

    def cp(eng, out, in_, r, w):
        return S.op(eng, lambda e: e.tensor_copy(out=out, in_=in_), r=r, w=w)

    def recip(out, in_, r, w):
        return S.op("dve", lambda e: e.reciprocal(out=out, in_=in_), r=r, w=w)

    cstf = mem.sb([128, 5, 128]); b_cstf = Buf()
    S.dma("sp", cstf[:, :, :], cst_d[:, :, :].rearrange("c p n -> p c n"), w=[b_cstf])
    cstb = mem.sb([128, 5, 128], BF16); b_cst = Buf()
    cp("dve", cstb[:, :, :], cstf[:, :, :], [b_cstf], [b_cst])
    idb = cstb[:, 0, :]; blkb = cstb[:, 1, :]; rotb = cstb[:, 2, :]
    idf = cstf[:, 0, :]
    onesf = mem.sb([128, 128]); b_onesf = Buf()
    S.op("pool", lambda e: e.memset(onesf[:, :], 1.0), w=[b_onesf])
    onesb = mem.sb([128, 128], BF16); b_onesb = Buf()
    S.op("pool", lambda e: e.memset(onesb[:, :], 1.0), w=[b_onesb])

    def layer_body(layer, x_d, cx_d, xo_d, cxo_d, wmod_d, bmodT_d, n1T_d, n2T_d, win_d, wout_d, wr_d, hvec_d, cw_d, tt_d, wg_d, wu_d, wd_d):
        def xsrc(t0, n):
            return (x_d, t0) if t0 < NLAT else (cx_d, t0 - NLAT)

        def xdst(t0):
            return (xo_d, t0) if t0 < NLAT else (cxo_d, t0 - NLAT)

        modT = mem.sb([128, 48, 2]); b_mod = Buf()
        c2T = mem.sb([128, 8, 2]); b_c2 = Buf()
        S.dma("sp", c2T[:, :, :], c2T_d[:, :, :], w=[b_c2])
        scT = mem.sb([128, 8, 2]); b_sc = Buf()
        act(scT[:, :, :], c2T[:, :, :], AF.Silu, [b_c2], [b_sc])
        bmodT = mem.sb([128, 48]); b_bm = Buf()
        S.dma("sp", bmodT[:, :], bmodT_d[:, :], w=[b_bm])
        m0 = mem.mark()
        wmp = RR(mem, [128, 8, 1024], F32, 2)
        pm, bpm = PF.get()
        for m in range(6):
            wt, bw = wmp.get()
            for k in range(8):
                S.dma("sp" if k % 2 == 0 else "pool", wt[:, k, :], wmod_d[k * 128:(k + 1) * 128, m * 1024:(m + 1) * 1024], w=[bw])
            for oc in range(8):
                for k in range(8):
                    mm(pm[:, (m * 8 + oc) * 2:(m * 8 + oc) * 2 + 2], wt[:, k, oc * 128:(oc + 1) * 128], scT[:, k, :], k == 0, k == 7, [bw, b_sc], [bpm])
        tt("dve", modT[:, :, :], pm[:, 0:96].rearrange("p (a r) -> p a r", r=2), bmodT[:, :].unsqueeze(2).to_broadcast([128, 48, 2]), ALU.add, [bpm, b_bm], [b_mod])
        mem.reset(m0)
        fence(S)
        n1T = mem.sb([128, 8]); n2T = mem.sb([128, 8]); b_n = Buf()
        S.dma("sp", n1T[:, :], n1T_d[:, :], w=[b_n])
        S.dma("sp", n2T[:, :], n2T_d[:, :], w=[b_n])
        A1 = mem.sb([128, 8, 2]); A2 = mem.sb([128, 8, 2]); b_A = Buf()
        for (A, nT, ms) in ((A1, n1T, 1), (A2, n2T, 4)):
            ts("dve", A[:, :, :], modT[:, ms * 8:(ms + 1) * 8, :], 1.0, None, ALU.add, None, [b_mod], [b_A])
            tt("dve", A[:, :, :], A[:, :, :], nT[:, :].unsqueeze(2).to_broadcast([128, 8, 2]), ALU.mult, [b_A, b_n], [b_A])

        def Bm(ms, k, r):
            return modT[:, ms * 8 + k, r:r + 1]

        G1 = mem.sb([128, 2, 1024]); G2 = mem.sb([128, 2, 1024]); b_G = Buf()
        dg = mem.sb([128, 128]); b_dg = Buf()
        for (G, ms) in ((G1, 2), (G2, 5)):
            for r in range(2):
                for half in range(2):
                    pg, bpg = PF.get()
                    for kk in range(4):
                        k = half * 4 + kk
                        ts("dve", dg[:, :], idf, modT[:, ms * 8 + k, r:r + 1], None, ALU.mult, None, [b_cstf, b_mod], [b_dg])
                        mm(pg[:, kk * 128:(kk + 1) * 128], onesf[:, :], dg[:, :], True, True, [b_onesf, b_dg], [bpg])
                    cp("dve", G[:, r, half * 512:(half + 1) * 512], pg[:, :], [bpg], [b_G])
        hvec = mem.sb([128, 3]); b_hv = Buf()
        S.dma("sp", hvec[:, :], hvec_d[:, :], w=[b_hv])
        qw8 = mem.sb([128, 1])
        ts("dve", qw8[:, :], hvec[:, 0:1], 0.125, None, ALU.mult, None, [b_hv], [b_hv])
        lbl = mem.sb([128, 3, 2, 4]); b_lb = Buf()
        S.dma("sp", lbl[:, :, :, :], lbl_d[:, :, :, :], w=[b_lb])
        lmx = mem.sb([128, 3, 2]); lsum = mem.sb([128, 3, 2]); lbv = mem.sb([128, 3, 2]); omlb = mem.sb([128, 3, 2])
        S.op("dve", lambda e: e.tensor_reduce(out=lmx[:, :, :], in_=lbl[:, :, :, :], axis=AX.X, op=ALU.max), r=[b_lb], w=[b_lb])
        tt("dve", lbl[:, :, :, :], lbl[:, :, :, :], lmx[:, :, :].unsqueeze(3).to_broadcast([128, 3, 2, 4]), ALU.subtract, [b_lb], [b_lb])
        act(lbl[:, :, :, :], lbl[:, :, :, :], AF.Exp, [b_lb], [b_lb])
        S.op("dve", lambda e: e.tensor_reduce(out=lsum[:, :, :], in_=lbl[:, :, :, :], axis=AX.X, op=ALU.add), r=[b_lb], w=[b_lb])
        recip(lsum[:, :, :], lsum[:, :, :], [b_lb], [b_lb])
        S.op("dve", lambda e: e.memset(lbv[:, :, :], 0.0), r=[b_lb], w=[b_lb])
        for jl in range(1, layer + 1):
            tt("dve", lbv[:, :, :], lbv[:, :, :], lbl[:, :, :, jl], ALU.add, [b_lb], [b_lb])
        tt("dve", lbv[:, :, :], lbv[:, :, :], lsum[:, :, :], ALU.mult, [b_lb], [b_lb])
        ts("dve", omlb[:, :, :], lbv[:, :, :], -1.0, 1.0, ALU.mult, ALU.add, [b_lb], [b_lb])
        cw = mem.sb([128, 2, 3]); b_cw = Buf()
        S.dma("sp", cw[:, :, :], cw_d[:, :, :], w=[b_cw])
        PBASE = mem.mark()

        if stop == "pro":
            raise _Stop()
        def norm_tile(src_d, row0, r, A, ms_shift, xt, bx, sm, hT, bhT, col0, fp32_out=None):
            junk, bj, ss, rt, rs_, xn, bxn, bsm = sm
            act(junk[:, :], xt[:, :], AF.Square, [bx], [bj, bsm], accum_out=ss[:, :])
            act(rt[:, :], ss[:, :], AF.Sqrt, [bsm], [bsm], scale=1.0 / DM, bias=1e-6)
            recip(rs_[:, :], rt[:, :], [bsm], [bsm])
            act(xn[:, :], xt[:, :], AF.Copy, [bx, bsm], [bxn], scale=rs_[:, 0:1])
            pt, bpt = PB.get()
            for k in range(8):
                tr(pt[:, k, :], xn[:, k * 128:(k + 1) * 128], idb, [bxn, b_cst], [bpt])
            for k in range(8):
                if k % 2 == 0:
                    ts("dve", hT[:, k, col0:col0 + 128], pt[:, k, :], A[:, k, r:r + 1], Bm(ms_shift, k, r), ALU.mult, ALU.add, [bpt, b_A, b_mod], [bhT])
                else:
                    act(hT[:, k, col0:col0 + 128], pt[:, k, :], AF.Identity, [bpt, b_A, b_mod], [bhT], scale=A[:, k, r:r + 1], bias=Bm(ms_shift, k, r))

        def norm_scratch(mem):
            junk = mem.sb([128, 1024], BF16)
            ss = mem.sb([128, 1]); rt = mem.sb([128, 1]); rs_ = mem.sb([128, 1])
            xn = mem.sb([128, 1024], BF16)
            return (junk, Buf(), ss, rt, rs_, xn, Buf(), Buf())

        Wb = mem.sb([128, 8, NIN], BF16); b_W = Buf()
        stg = RR(mem, [128, 1920], F32, 2)
        for k in range(8):
            for hf in range(2):
                st_, bst = stg.get()
                S.dma("sp" if hf == 0 else "pool", st_[:, :], win_d[k * 128:(k + 1) * 128, hf * 1920:(hf + 1) * 1920], w=[bst])
                cp("pool" if hf == 0 else "dve", Wb[:, k, hf * 1920:(hf + 1) * 1920], st_[:, :], [bst], [b_W])
        xtp = RR(mem, [128, 1024], F32, 2)
        nsm = norm_scratch(mem)
        hTp = RR(mem, [128, 8, 512], BF16, 2)
        cosp = RR(mem, [128, 512], F32, 2)
        sinp = RR(mem, [128, 512], F32, 2)
        zero_t = mem.sb([128, 512], BF16); b_zero = Buf()
        S.op("pool", lambda e: e.memset(zero_t[:, :], 0.0), w=[b_zero])
        zero_f = mem.sb([128, 4]); b_zf = Buf()
        S.op("pool", lambda e: e.memset(zero_f[:, :], 0.0), w=[b_zf])
        ones64 = mem.sb([128, 64]); b_o64 = Buf()
        S.op("pool", lambda e: e.memset(ones64[:, :], 1.0), w=[b_o64])
        for cc in range(2):
            for col in (0, NLAT + 1, NLAT + 2, NTOK + 3):
                S.dma("pool", z_s[cc, :, col:col + 1], zero_f[:, 0:1], r=[b_zf], w=[DB("z", cc, "pad", col)])

        def T512(dt=F32, name=None):
            return mem.sb([128, 512], dt, name), Buf()

        sq_t, b_sq = T512(BF16, name="sq_t")
        rt_t, b_rt = T512(name="rt_t")
        qn_t, b_qn = T512(BF16, name="qn_t")
        t1_t, b_t1 = T512(name="t1_t")
        t2_t, b_t2 = T512(name="t2_t")
        qo_t, b_qo = T512(BF16, name="qo_t")
        vt_p = RR(mem, [128, 384], BF16, 2)
        itm = mem.sb([128, 4, 384], BF16); b_itm = Buf()
        hq_t, b_hq = T512(name="hq_t")
        sg_t, b_sg = T512(name="sg_t")
        lf_t, b_lf = T512(name="lf_t")
        kk_t, b_kk = T512(name="kk_t")
        A_t, b_At = T512(name="A_t")
        a_t, b_a = T512(name="a_t")
        d_t, b_d = T512(name="d_t")
        e_t, b_e = T512(name="e_t")
        e2_t, b_e2 = T512(name="e2_t")
        qtl_t, b_qtl = T512(BF16, name="qtl_t")
        ktl_t, b_ktl = T512(BF16, name="ktl_t")
        qh_t, b_qh = T512(BF16, name="qh_t")
        kh_t, b_kh = T512(BF16, name="kh_t")
        khtm = mem.sb([128, 4, 128], BF16); b_khtm = Buf()
        Tt = mem.sb([128, 8]); rr_t = mem.sb([128, 16]); Dt = mem.sb([128, 8]); bn_t = mem.sb([128, 8]); b_T = Buf(); b_rr = Buf(); b_D = Buf(); b_bn = Buf()
        kz = [mem.sb([128, 512], BF16, "kzf"), mem.sb([128, 512], BF16, "kzb")]
        b_kz = [Buf(), Buf()]
        for i_ in range(2):
            S.op("pool", lambda e, t=kz[i_]: e.memset(t[:, :], 0.0), w=[b_kz[i_]])
        scm = mem.sb([128, 128], BF16); b_scm = Buf()
        Ut = mem.sb([128, 8, 64]); b_U = Buf()
        osb, b_osb = T512()
        gs_t, b_gs = T512()
        cs_t, b_cs = T512()
        cb_t, b_cb = T512(BF16)
        po_ps, b_po = [pfs[2], pfs[3]], [Buf(), Buf()]
        pu_ps, b_pu = [pfs[4], pfs[5]], [Buf(), Buf()]
        PF4 = PRR(pfs[0:2])

        def do_tile(t0, n):
            isc = t0 >= NLAT
            r = 1 if isc else 0
            nsub = n // 128
            nch = n // 64
            c0 = t0 // 64
            hT, bhT = hTp.get()
            src, so = xsrc(t0, n)
            for sub in range(nsub):
                xt, bx = xtp.get()
                S.dma("sp", xt[:, :], src[so + sub * 128: so + (sub + 1) * 128, :], w=[bx])
                norm_tile(src, so, r, A1, 0, xt, bx, nsm, hT, bhT, sub * 128)
            cs, bcs = cosp.get()
            sn, bsn = sinp.get()
            S.dma("pool", cs[:, 0:n], cos_d[:, t0:t0 + n], w=[bcs])
            S.dma("pool", sn[:, 0:n], sin_d[:, t0:t0 + n], w=[bsn])

            def proj(c0_):
                pf, bpf = PF4.get()
                for k in range(8):
                    mm(pf[:, 0:n], Wb[:, k, c0_:c0_ + 128], hT[:, k, 0:n], k == 0, k == 7, [b_W, bhT], [bpf])
                return pf, bpf

            chk("A1")
            for which in range(2):
                for pr in range(3):
                    pf, bpf = proj(which * 384 + pr * 128)
                    act(sq_t[:, 0:n], pf[:, 0:n], AF.Square, [bpf], [b_sq])
                    pss, bpss = PF4.get()
                    mm(pss[:, 0:n], blkb, sq_t[:, 0:n], True, True, [b_cst, b_sq], [bpss])
                    act(rt_t[:, 0:n], pss[:, 0:n], AF.Sqrt, [bpss], [b_rt], scale=1.0 / 64, bias=1e-6)
                    recip(rt_t[:, 0:n], rt_t[:, 0:n], [b_rt], [b_rt])
                    wv = qw8[:, 0:1] if which == 0 else hvec[:, 1:2]
                    stt("dve", qn_t[:, 0:n], pf[:, 0:n], wv, rt_t[:, 0:n], ALU.mult, ALU.mult, [bpf, b_rt, b_hv], [b_qn])
                    prot, bprot = PF4.get()
                    mm(prot[:, 0:n], rotb, qn_t[:, 0:n], True, True, [b_cst, b_qn], [bprot])
                    tt("pool", t1_t[:, 0:n], qn_t[:, 0:n], cs[:, 0:n], ALU.mult, [b_qn, bcs], [b_t1])
                    tt("dve", t2_t[:, 0:n], prot[:, 0:n], sn[:, 0:n], ALU.mult, [bprot, bsn], [b_t2])
                    tt("pool", qo_t[:, 0:n], t1_t[:, 0:n], t2_t[:, 0:n], ALU.add, [b_t1, b_t2], [b_qo])
                    if which == 0:
                        for hh in range(2):
                            oh = 1 - hh
                            S.dma("sp", QT_s[pr, hh, hh * 64:(hh + 1) * 64, t0:t0 + n], qo_t[hh * 64:(hh + 1) * 64, 0:n], r=[b_qo], w=[DB("Q", pr, hh, t0)])
                            S.dma("pool", QT_s[pr, hh, oh * 64:(oh + 1) * 64, t0:t0 + n], zero_t[oh * 64:(oh + 1) * 64, 0:n], r=[b_zero], w=[DB("Qz", pr, hh, t0)])
                    else:
                        S.dma("sp", KT_s[pr, :, t0:t0 + n], qo_t[:, 0:n], r=[b_qo], w=[DB("K", pr, t0)])
            chk("A2")
            for sub in range(nsub):
                pv, bpv = PF4.get()
                for k in range(8):
                    mm(pv[:, 0:384], hT[:, k, sub * 128:(sub + 1) * 128], Wb[:, k, 768:1152], k == 0, k == 7, [bhT, b_W], [bpv])
                vt, bvt = vt_p.get()
                act(vt[:, :], pv[:, 0:384], AF.Copy, [bpv], [bvt])
                S.dma("sp", V_s[t0 + sub * 128:t0 + (sub + 1) * 128, :], vt[:, :], r=[bvt], w=[DB("V", t0, sub)])
                pi, bpi = PF4.get()
                for k in range(8):
                    mm(pi[:, 0:384], hT[:, k, sub * 128:(sub + 1) * 128], Wb[:, k, 2304:2688], k == 0, k == 7, [bhT, b_W], [bpi])
                cp("dve", itm[:, sub, :], pi[:, 0:384], [bpi], [b_itm])
            chk("A3")
            for hp in range(3):
                pf, bpf = proj(1152 + hp * 128)
                act(hq_t[:, 0:n], pf[:, 0:n], AF.Copy, [bpf], [b_hq], scale=0.125)
                pf, bpf = proj(2688 + hp * 128)
                act(gs_t[:, 0:n], pf[:, 0:n], AF.Silu, [bpf], [b_gs])
                S.dma("pool", gs_s[hp, :, t0:t0 + n], gs_t[:, 0:n], r=[b_gs], w=[DB("gs", hp, t0)])
                for dr in range(2):
                    pf, bpf = proj(1536 + dr * 384 + hp * 128)
                    act(sg_t[:, 0:n], pf[:, 0:n], AF.Sigmoid, [bpf], [b_sg])
                    ts("dve", sg_t[:, 0:n], sg_t[:, 0:n], omlb[:, hp, dr:dr + 1], lbv[:, hp, dr:dr + 1], ALU.mult, ALU.add, [b_sg, b_lb], [b_sg])
                    act(lf_t[:, 0:n], sg_t[:, 0:n], AF.Ln, [b_sg], [b_lf])
                    ts("pool", kk_t[:, 0:n], sg_t[:, 0:n], -1.0, 1.0, ALU.mult, ALU.add, [b_sg], [b_kk])
                    for c in range(nch):
                        S.op("dve", lambda e, c=c: e.tensor_tensor_scan(out=A_t[:, c * 64:(c + 1) * 64], data0=ones64[:, :], data1=lf_t[:, c * 64:(c + 1) * 64], initial=0.0, op0=ALU.mult, op1=ALU.add), r=[b_lf, b_o64], w=[b_At])
                    A3 = A_t[:, 0:n].rearrange("p (c s) -> p c s", s=64)
                    cp("dve", Tt[:, 0:nch], A3[:, :, 63], [b_At], [b_T])
                    Tb = Tt[:, 0:nch].unsqueeze(2).to_broadcast([128, nch, 64])
                    a3 = a_t[:, 0:n].rearrange("p (c s) -> p c s", s=64)
                    if dr == 0:
                        cp("pool", a_t[:, 0:n], A_t[:, 0:n], [b_At], [b_a])
                    else:
                        tt("pool", a_t[:, 0:n], lf_t[:, 0:n], A_t[:, 0:n], ALU.subtract, [b_lf, b_At], [b_a])
                        tt("pool", a3, a3, Tb, ALU.add, [b_a, b_T], [b_a])
                    d3 = d_t[:, 0:n].rearrange("p (c s) -> p c s", s=64)
                    cp("dve", bn_t[:, 0:nch], a3[:, :, 31 if dr == 0 else 32], [b_a], [b_bn])
                    tt("pool", d3, a3, bn_t[:, 0:nch].unsqueeze(2).to_broadcast([128, nch, 64]), ALU.subtract, [b_a, b_bn], [b_d])
                    act(e_t[:, 0:n], d_t[:, 0:n], AF.Exp, [b_d], [b_e])
                    tt("dve", qtl_t[:, 0:n], hq_t[:, 0:n], e_t[:, 0:n], ALU.mult, [b_hq, b_e], [b_qtl])
                    act(e2_t[:, 0:n], d_t[:, 0:n], AF.Exp, [b_d], [b_e2], scale=-1.0)
                    tt("pool", ktl_t[:, 0:n], kk_t[:, 0:n], e2_t[:, 0:n], ALU.mult, [b_kk, b_e2], [b_ktl])
                    hsl = slice(0, 32) if dr == 0 else slice(32, 64)
                    cp("pool", kz[dr][:, 0:n].rearrange("p (c s) -> p c s", s=64)[:, :, hsl], ktl_t[:, 0:n].rearrange("p (c s) -> p c s", s=64)[:, :, hsl], [b_ktl], [b_kz[dr]])
                    act(e_t[:, 0:n], a_t[:, 0:n], AF.Exp, [b_a], [b_e])
                    tt("dve", qh_t[:, 0:n], hq_t[:, 0:n], e_t[:, 0:n], ALU.mult, [b_hq, b_e], [b_qh])
                    S.dma("sp", qhat_s[dr, hp, :, t0:t0 + n], qh_t[:, 0:n], r=[b_qh], w=[DB("qh", dr, hp, t0)])
                    tt("pool", d3, Tb, a3, ALU.subtract, [b_a, b_T], [b_d])
                    act(e2_t[:, 0:n], d_t[:, 0:n], AF.Exp, [b_d], [b_e2])
                    tt("pool", kh_t[:, 0:n], kk_t[:, 0:n], e2_t[:, 0:n], ALU.mult, [b_kk, b_e2], [b_kh])
                    act(Dt[:, 0:nch], Tt[:, 0:nch], AF.Exp, [b_T], [b_D])
                    S.dma("pool", D_s[dr, hp, :, c0:c0 + nch], Dt[:, 0:nch], r=[b_D], w=[DB("D", dr, hp, t0)])
                    chk("A4")
                    pt, bpt = PB.get()
                    for sub in range(nsub):
                        tr(pt[:, sub, :], kh_t[:, sub * 128:(sub + 1) * 128], idb, [b_kh, b_cst], [bpt])
                    act(khtm[:, 0:nsub, :], pt[:, 0:nsub, :], AF.Copy, [bpt], [b_khtm])
                    mk = cstb[:, 3 + dr, :]
                    for sub in range(nsub):
                        pscs = [PF4.get(), PF4.get()]
                        for c_ in range(2):
                            tb = sub * 128 + c_ * 64
                            rows = slice(c_ * 64, (c_ + 1) * 64)
                            for hh in range(2):
                                hs = slice(hh * 64, (hh + 1) * 64)
                                psc, bpsc = pscs[hh]
                                tfull, tz = (1, 0) if dr == 0 else (0, 1)
                                mm(psc[rows, tfull * 32:(tfull + 1) * 32], ktl_t[hs, tb:tb + 64], qtl_t[hs, tb + tfull * 32:tb + (tfull + 1) * 32], True, True, [b_ktl, b_qtl], [bpsc])
                                mm(psc[rows, tz * 32:(tz + 1) * 32], kz[dr][hs, tb:tb + 64], qtl_t[hs, tb + tz * 32:tb + (tz + 1) * 32], True, True, [b_kz[dr], b_qtl], [bpsc])
                        for hh in range(2):
                            psc, bpsc = pscs[hh]
                            tt("dve", scm[:, hh * 64:(hh + 1) * 64], psc[:, 0:64], mk[:, 0:64], ALU.mult, [bpsc, b_cst], [b_scm])
                        for c_ in range(2):
                            ps_ = slice(c_ * 64, (c_ + 1) * 64)
                            for hh in range(2):
                                hs = slice(hh * 64, (hh + 1) * 64)
                                vcol = slice((hp * 2 + hh) * 64, (hp * 2 + hh + 1) * 64)
                                mm(pu_ps[c_][hs, sub * 64:(sub + 1) * 64], khtm[ps_, sub, hs], itm[ps_, sub, vcol], True, True, [b_khtm, b_itm], [b_pu[c_]])
                                mm(po_ps[c_][hs, sub * 64:(sub + 1) * 64], itm[ps_, sub, vcol], scm[ps_, hs], True, True, [b_itm, b_scm], [b_po[c_]])
                    for c_ in range(2):
                        cp("dve", Ut[:, 0:nch, :].rearrange("p (s c) d -> p s c d", c=2)[:, :, c_, :], pu_ps[c_][:, 0:nsub * 64].rearrange("p (s d) -> p s d", d=64), [b_pu[c_]], [b_U])
                    S.dma("sp", U_s[dr, hp, :, c0:c0 + nch, :], Ut[:, 0:nch, :], r=[b_U], w=[DB("U", dr, hp, t0)])
                    for c_ in range(2):
                        ov = osb[:, 0:n].rearrange("p (s c t) -> p s c t", c=2, t=64)[:, :, c_, :]
                        pv_ = po_ps[c_][:, 0:nsub * 64].rearrange("p (s t) -> p s t", t=64)
                        if dr == 0:
                            act(ov, pv_, AF.Copy, [b_po[c_]], [b_osb])
                        else:
                            tt("dve", ov, ov, pv_, ALU.add, [b_osb, b_po[c_]], [b_osb])
                S.dma("sp", oacc_s[hp, :, t0:t0 + n], osb[:, 0:n], r=[b_osb], w=[DB("oacc", hp, t0)])
            chk("A5")
            zoff = t0 + 3 if isc else t0 + 1
            for cc in range(2):
                pf, bpf = proj(3328 + cc * 128)
                act(cs_t[:, 0:n], pf[:, 0:n], AF.Copy, [bpf], [b_cs])
                pf, bpf = proj(3584 + cc * 128)
                tt("dve", cs_t[:, 0:n], cs_t[:, 0:n], pf[:, 0:n], ALU.mult, [b_cs, bpf], [b_cs])
                S.dma("pool", z_s[cc, :, zoff:zoff + n], cs_t[:, 0:n], r=[b_cs], w=[DB("z", cc, t0)])
                pf, bpf = proj(3072 + cc * 128)
                act(cb_t[:, 0:n], pf[:, 0:n], AF.Copy, [bpf], [b_cb])
                S.dma("pool", cb_s[cc, :, t0:t0 + n], cb_t[:, 0:n], r=[b_cb], w=[DB("cb", cc, t0)])
        try:
            for (t0_, n_) in (TILES[:1] if (stop or "").startswith("A") and stop != "A" else TILES):
                do_tile(t0_, n_)
        except _Stop:
            raise _Stop()
        fence(S)
        mem.reset(PBASE)

        if stop == "A":
            raise _Stop()
        NP_ = len(rm_patterns)
        ttb = mem.sb([128, 6 * 22 * 64], BF16); b_tt = Buf()
        stg2 = RR(mem, [128, 2112], F32, 2)
        for i4 in range(4):
            st_, bst = stg2.get()
            S.dma("sp", st_[:, :], tt_d[:, i4 * 2112:(i4 + 1) * 2112], w=[bst])
            cp("dve", ttb[:, i4 * 2112:(i4 + 1) * 2112], st_[:, :], [bst], [b_tt])
        rmaf = mem.sb([128, NP_, 128]); rmab = mem.sb([128, NP_, 128], BF16); rmbf = mem.sb([128, 512]); rmbb = mem.sb([128, 512], BF16); b_rm = Buf()
        S.dma("sp", rmaf[:, :, :], rma_d[:, :, :].rearrange("n k m -> k n m"), w=[b_rm])
        S.dma("sp", rmbf[:, :], rmb_d[:, :], w=[b_rm])
        cp("dve", rmab[:, :, :], rmaf[:, :, :], [b_rm], [b_rm])
        cp("dve", rmbb[:, :], rmbf[:, :], [b_rm], [b_rm])
        kcx = mem.sb([128, 3, 256], BF16); vcx = mem.sb([128, 2, 384], BF16); b_cxkv = Buf()
        S.dma("sp", kcx[:, :, :], KT_s[:, :, NLAT:NTOK].rearrange("c p t -> p c t"), r=[DB("K", pr, NLAT) for pr in range(3)], w=[b_cxkv])
        S.dma("sp", vcx[:, :, :], V_s[NLAT:NTOK, :].rearrange("(s t) c -> t s c", t=128), r=[DB("V", NLAT, s_) for s_ in range(2)], w=[b_cxkv])
        qp = RR(mem, [128, 6, 512], BF16, 2)
        kp = RR(mem, [128, 3, 1024], BF16, 2)
        vp = RR(mem, [128, 8, 384], BF16, 2)
        ptp = RR(mem, [128, 512], BF16, 3)
        rin_t, b_rin = T512()
        ob_p = RR(mem, [128, 512], BF16, 2)
        PS2 = PRR(pfs[0:2])
        pos = PRR(pfs[2:4])
        pds = PRR(pfs[4:6])

        def rowpat(g, p):
            out = []
            for a in range(2):
                for i in range(8):
                    kr = 8 * g - 4 + 2 * p + a
                    rq = 8 * g + i
                    rs0 = min(max(rq - 4, 0), NROWS - 8)
                    out.append(rs0 <= kr < rs0 + 8)
            return tuple(out)

        for (t0, n) in TILES:
            isc = t0 >= NLAT
            g = t0 // 512
            qt, bq = qp.get()
            S.dma("sp", qt[:, :, 0:n], QT_s[:, :, :, t0:t0 + n].rearrange("c h p t -> p (c h) t"),
                  r=[DB(nm, pr, hh, t0) for nm in ("Q", "Qz") for pr in range(3) for hh in range(2)], w=[bq])
            chunks = []
            if not isc:
                ps_valid = [p for p in range(8) if 0 <= 8 * g - 4 + 2 * p < NROWS]
                p_lo, p_hi = ps_valid[0], ps_valid[-1]
                k0 = (8 * g - 4) * 64
                kt, bk = kp.get()
                vt_, bv = vp.get()
                tl, th = k0 + p_lo * 128, k0 + (p_hi + 1) * 128
                tiles_touched = sorted(set([(tl // 512) * 512, ((th - 1) // 512) * 512, (((tl + th) // 2) // 512) * 512]))
                S.dma("pool", kt[:, :, p_lo * 128:(p_hi + 1) * 128], KT_s[:, :, tl:th].rearrange("c p t -> p c t"),
                      r=[DB("K", pr, tt0) for pr in range(3) for tt0 in tiles_touched], w=[bk])
                S.dma("pool", vt_[:, p_lo:p_hi + 1, :], V_s[tl:th, :].rearrange("(s t) c -> t s c", t=128),
                      r=[DB("V", tt0, s_) for tt0 in tiles_touched for s_ in range(4)], w=[bv])
                chunks = [("loc", p) for p in ps_valid]
            chunks += [("ctx", 0), ("ctx", 1)]
            for pr in range(3):
                po_, bpo = pos.get()
                pd_, bpd = pds.get()
                for hh in range(2):
                    h = pr * 2 + hh
                    hs = slice(hh * 64, (hh + 1) * 64)
                    for ci, (kind, p) in enumerate(chunks):
                        st_ps, bst_ps = PS2.get()
                        first, last = ci == 0, ci == len(chunks) - 1
                        if kind == "loc":
                            mm(st_ps[:, 0:n], kt[:, pr, p * 128:(p + 1) * 128], qt[:, h, 0:n], True, False, [bk, bq], [bst_ps])
                            u0 = 14 - 2 * p
                            mm(st_ps[:, 0:n], idb, ttb[:, (h * 22 + u0) * 64:(h * 22 + u0 + 8) * 64], False, False, [b_cst, b_tt], [bst_ps])
                            pat = rm_patterns.index(rowpat(g, p))
                            mm(st_ps[:, 0:n], rmab[:, pat, :], rmbb[:, :], False, True, [b_rm], [bst_ps])
                            vl = vt_[:, p, h * 64:(h + 1) * 64]
                            rv = [bv]
                        else:
                            mm(st_ps[:, 0:n], kcx[:, pr, p * 128:(p + 1) * 128], qt[:, h, 0:n], True, True, [b_cxkv, bq], [bst_ps])
                            vl = vcx[:, p, h * 64:(h + 1) * 64]
                            rv = [b_cxkv]
                        pT, bpT = ptp.get()
                        act(pT[:, 0:n], st_ps[:, 0:n], AF.Exp, [bst_ps], [bpT])
                        mm(po_[hs, 0:n], vl, pT[:, 0:n], first, last, rv + [bpT], [bpo])
                        mm(pd_[hs, 0:n], onesb[:, 0:64], pT[:, 0:n], first, last, [b_onesb, bpT], [bpd])
                recip(rin_t[:, 0:n], pd_[:, 0:n], [bpd], [b_rin])
                ob, bob = ob_p.get()
                tt("dve", ob[:, 0:n], po_[:, 0:n], rin_t[:, 0:n], ALU.mult, [bpo, b_rin], [bob])
                S.dma("sp", mix_s[pr, :, t0:t0 + n], ob[:, 0:n], r=[bob], w=[DB("mix", pr, t0)])
        fence(S)
        mem.reset(PBASE)

        if stop == "B":
            raise _Stop()
        S32 = [[mem.sb([128, 64]) for _ in range(3)] for _ in range(2)]
        Sbf = [[mem.sb([128, 64], BF16) for _ in range(3)] for _ in range(2)]
        bS = [[Buf() for _ in range(3)] for _ in range(2)]
        bSb = [[Buf() for _ in range(3)] for _ in range(2)]
        qh_p = RR(mem, [128, 3, 512], BF16, 2)
        U_p = RR(mem, [128, 3, 8, 64], F32, 2)
        D_p = RR(mem, [128, 3, 8], F32, 2)
        oa_p = RR(mem, [128, 3, 512], F32, 2)
        gsl_p = RR(mem, [128, 3, 512], F32, 2)
        sq2, b_sq2 = T512(BF16)
        rt2, b_rt2 = T512()
        y2, b_y2 = T512()
        yb_p = RR(mem, [128, 512], BF16, 2)
        pin = [[pfs[0], pfs[1]], [pfs[2], pfs[3]], [pfs[4], pfs[5]]]
        bpin = [[Buf(), Buf()] for _ in range(3)]
        PSX = PRR(pfs[0:6])
        PSX.b = [bpin[i // 2][i % 2] for i in range(6)]
        for dr in range(2):
            for hp in range(3):
                S.op("pool", lambda e, t=S32[dr][hp]: e.memset(t[:, :], 0.0), w=[bS[dr][hp]])
                S.op("pool", lambda e, t=Sbf[dr][hp]: e.memset(t[:, :], 0.0), w=[bSb[dr][hp]])
            order = [TILES[-1]] + (TILES[:-1] if dr == 0 else TILES[:-1][::-1])
            for (t0, n) in order:
                nch = n // 64
                c0 = t0 // 64
                qh, bqh = qh_p.get()
                Uu, bUu = U_p.get()
                Dd, bDd = D_p.get()
                oa, boa = oa_p.get()
                S.dma("sp", qh[:, :, 0:n], qhat_s[dr, :, :, t0:t0 + n].rearrange("c p t -> p c t"), r=[DB("qh", dr, hp, t0) for hp in range(3)], w=[bqh])
                S.dma("pool", Uu[:, :, 0:nch, :], U_s[dr, :, :, c0:c0 + nch, :].rearrange("c p a b -> p c a b"), r=[DB("U", dr, hp, t0) for hp in range(3)], w=[bUu])
                S.dma("pool", Dd[:, :, 0:nch], D_s[dr, :, :, c0:c0 + nch].rearrange("c p a -> p c a"), r=[DB("D", dr, hp, t0) for hp in range(3)], w=[bDd])
                S.dma("sp", oa[:, :, 0:n], oacc_s[:, :, t0:t0 + n].rearrange("c p t -> p c t"), r=[DB("oacc", hp, t0) for hp in range(3)], w=[boa])
                if dr == 1:
                    gl, bgl = gsl_p.get()
                    S.dma("sp", gl[:, :, 0:n], gs_s[:, :, t0:t0 + n].rearrange("c p t -> p c t"), r=[DB("gs", hp, t0) for hp in range(3)], w=[bgl])
                corder = list(range(nch)) if dr == 0 else list(range(nch))[::-1]
                for c in corder:
                    for hp in range(3):
                        for hh in range(2):
                            hs = slice(hh * 64, (hh + 1) * 64)
                            mm(pin[hp][hh][hs, c * 64:(c + 1) * 64], Sbf[dr][hp][hs, :], qh[hs, hp, c * 64:(c + 1) * 64], True, True, [bSb[dr][hp], bqh], [bpin[hp][hh]])
                        stt("dve", S32[dr][hp][:, :], S32[dr][hp][:, :], Dd[:, hp, c:c + 1], Uu[:, hp, c, :], ALU.mult, ALU.add, [bS[dr][hp], bDd, bUu], [bS[dr][hp]])
                        cp("pool", Sbf[dr][hp][:, :], S32[dr][hp][:, :], [bS[dr][hp]], [bSb[dr][hp]])
                for hp in range(3):
                    for hh in range(2):
                        hs = slice(hh * 64, (hh + 1) * 64)
                        tt("dve", oa[hs, hp, 0:n], oa[hs, hp, 0:n], pin[hp][hh][hs, 0:n], ALU.add, [boa, bpin[hp][hh]], [boa])
                if dr == 0:
                    S.dma("sp", oacc_s[:, :, t0:t0 + n].rearrange("c p t -> p c t"), oa[:, :, 0:n], r=[boa], w=[DB("oacc", hp, t0) for hp in range(3)])
                else:
                    for hp in range(3):
                        act(sq2[:, 0:n], oa[:, hp, 0:n], AF.Square, [boa], [b_sq2])
                        pss, bpss = PSX.get()
                        mm(pss[:, 0:n], blkb, sq2[:, 0:n], True, True, [b_cst, b_sq2], [bpss])
                        act(rt2[:, 0:n], pss[:, 0:n], AF.Sqrt, [bpss], [b_rt2], scale=1.0 / 64, bias=1e-6)
                        recip(rt2[:, 0:n], rt2[:, 0:n], [b_rt2], [b_rt2])
                        stt("dve", y2[:, 0:n], oa[:, hp, 0:n], hvec[:, 2:3], rt2[:, 0:n], ALU.mult, ALU.mult, [boa, b_rt2, b_hv], [b_y2])
                        yb, byb = yb_p.get()
                        tt("pool", yb[:, 0:n], y2[:, 0:n], gl[:, hp, 0:n], ALU.mult, [b_y2, bgl], [byb])
                        S.dma("sp", mix_s[3 + hp, :, t0:t0 + n], yb[:, 0:n], r=[byb], w=[DB("mix", 3 + hp, t0)])
        fence(S)
        mem.reset(PBASE)

        if stop == "C":
            raise _Stop()
        zw_p = RR(mem, [128, 514], F32, 2)
        cbl_p = RR(mem, [128, 512], BF16, 2)
        y3, b_y3 = T512()
        yc_p = RR(mem, [128, 512], BF16, 2)
        for (t0, n) in TILES:
            isc = t0 >= NLAT
            zoff = t0 + 3 if isc else t0 + 1
            for cc in range(2):
                zw, bzw = zw_p.get()
                cbl, bcbl = cbl_p.get()
                rz = [DB("z", cc, tt0) for tt0 in (t0 - 512, t0, t0 + 512) if 0 <= tt0 < NLAT or tt0 == t0] + [DB("z", cc, "pad", col) for col in (0, NLAT + 1, NLAT + 2, NTOK + 3)]
                S.dma("sp", zw[:, 0:n + 2], z_s[cc, :, zoff - 1:zoff + n + 1], r=rz, w=[bzw])
                S.dma("pool", cbl[:, 0:n], cb_s[cc, :, t0:t0 + n], r=[DB("cb", cc, t0)], w=[bcbl])
                ts("dve", y3[:, 0:n], zw[:, 0:n], cw[:, cc, 0:1], None, ALU.mult, None, [bzw, b_cw], [b_y3])
                stt("dve", y3[:, 0:n], zw[:, 1:n + 1], cw[:, cc, 1:2], y3[:, 0:n], ALU.mult, ALU.add, [bzw, b_cw, b_y3], [b_y3])
                stt("dve", y3[:, 0:n], zw[:, 2:n + 2], cw[:, cc, 2:3], y3[:, 0:n], ALU.mult, ALU.add, [bzw, b_cw, b_y3], [b_y3])
                yc, byc = yc_p.get()
                tt("pool", yc[:, 0:n], y3[:, 0:n], cbl[:, 0:n], ALU.mult, [b_y3, bcbl], [byc])
                S.dma("sp", mix_s[6 + cc, :, t0:t0 + n], yc[:, 0:n], r=[byc], w=[DB("mix", 6 + cc, t0)])
        fence(S)
        mem.reset(PBASE)

        if stop == "D":
            raise _Stop()
        Wob = mem.sb([128, 8, 1024], BF16); b_Wo = Buf()
        stg3 = RR(mem, [128, 1024], F32, 2)
        for k in range(8):
            st_, bst = stg3.get()
            S.dma("sp", st_[:, :], wout_d[k * 128:(k + 1) * 128, :], w=[bst])
            cp("pool", Wob[:, k, :], st_[:, :], [bst], [b_Wo])
        wrf = mem.sb([128, 8, 16]); wrb = mem.sb([128, 8, 16], BF16); b_wr = Buf()
        S.dma("sp", wrf[:, :, :], wr_d[:, :].rearrange("(k p) e -> p k e", p=128), w=[b_wr])
        cp("dve", wrb[:, :, :], wrf[:, :, :], [b_wr], [b_wr])
        mx_p = RR(mem, [128, 8, 128], BF16, 2)
        xt_p = RR(mem, [128, 1024], F32, 2)
        xm_p = RR(mem, [128, 1024], F32, 2)
        nsm2 = norm_scratch(mem)
        h2_p = RR(mem, [128, 8, 128], BF16, 2)
        lmax = mem.sb([128, 1]); lsm = mem.sb([128, 1]); ex_t = mem.sb([128, 16]); b_sm = Buf()
        af_p = RR(mem, [128, 16], F32, 2)
        PE6 = PRR(pfs[0:6])
        for (t0, n) in TILES:
            isc = t0 >= NLAT
            r = 1 if isc else 0
            src, so = xsrc(t0, n)
            for sub in range(n // 128):
                ta = t0 + sub * 128
                mx, bmx = mx_p.get()
                S.dma("sp", mx[:, :, :], mix_s[:, :, ta:ta + 128].rearrange("k p t -> p k t"), r=[DB("mix", k, t0) for k in range(8)], w=[bmx])
                xt, bx = xt_p.get()
                S.dma("pool", xt[:, :], src[so + sub * 128:so + (sub + 1) * 128, :], w=[bx])
                xm, bxm = xm_p.get()
                for half in range(2):
                    pso, bpso = PE6.get()
                    for k in range(8):
                        mm(pso[:, :], mx[:, k, :], Wob[:, k, half * 512:(half + 1) * 512], k == 0, k == 7, [bmx, b_Wo], [bpso])
                    tt("dve", xm[:, half * 512:(half + 1) * 512], pso[:, :], G1[:, r, half * 512:(half + 1) * 512], ALU.mult, [bpso, b_G], [bxm])
                tt("pool", xm[:, :], xm[:, :], xt[:, :], ALU.add, [bxm, bx], [bxm])
                S.dma("sp", xm_s[ta:ta + 128, :], xm[:, :], r=[bxm], w=[DB("xm", ta)])
                h2, bh2 = h2_p.get()
                norm_tile(None, 0, r, A2, 3, xm, bxm, nsm2, h2, bh2, 0)
                S.dma("pool", h2_s[:, :, ta:ta + 128].rearrange("k p t -> p k t"), h2[:, :, :], r=[bh2], w=[DB("h2", ta)])
                pl, bpl = PE6.get()
                for k in range(8):
                    mm(pl[:, 0:16], h2[:, k, :], wrb[:, k, :], k == 0, k == 7, [bh2, b_wr], [bpl])
                S.op("dve", lambda e, pl=pl: e.tensor_reduce(out=lmax[:, :], in_=pl[:, 0:16], axis=AX.X, op=ALU.max), r=[bpl], w=[b_sm])
                ts("dve", lmax[:, :], lmax[:, :], -1.0, None, ALU.mult, None, [b_sm], [b_sm])
                act(ex_t[:, :], pl[:, 0:16], AF.Exp, [bpl, b_sm], [b_sm], bias=lmax[:, 0:1], accum_out=lsm[:, :])
                recip(lsm[:, :], lsm[:, :], [b_sm], [b_sm])
                af, baf = af_p.get()
                ts("dve", af[:, :], ex_t[:, :], lsm[:, 0:1], None, ALU.mult, None, [b_sm], [baf])
                S.dma("pool", aff_s[ta:ta + 128, :], af[:, :], r=[baf], w=[DB("aff", ta)])
        fence(S)
        mem.reset(PBASE)

        if stop == "E":
            raise _Stop()
        mF = mem.mark()
        stg4 = RR(mem, [128, 8, 512], F32, 2)
        cst4 = RR(mem, [128, 8, 512], BF16, 2)
        for ex in range(NEXP):
            for (wsrc, wdst, nm) in ((wg_d, wgb_s, "wg"), (wu_d, wub_s, "wu")):
                st_, bst = stg4.get()
                S.dma("sp", st_[:, :, :], wsrc[ex, :, :].rearrange("(k p) f -> p k f", p=128), w=[bst])
                cb_, bcb_ = cst4.get()
                cp("pool" if nm == "wg" else "dve", cb_[:, :, :], st_[:, :, :], [bst], [bcb_])
                S.dma("pool", wdst[ex, :, :, :], cb_[:, :, :], r=[bcb_], w=[DB(nm, ex)])
            st_, bst = stg4.get()
            S.dma("sp", st_[:, 0:4, :].rearrange("p k (a f) -> p k a f", a=1), wd_d[ex, :, :].rearrange("(k p) (a f) -> p k a f", p=128, a=1)[:, :, :, 0:512], w=[bst])
            S.dma("sp", st_[:, 4:8, :].rearrange("p k (a f) -> p k a f", a=1), wd_d[ex, :, :].rearrange("(k p) (a f) -> p k a f", p=128, a=2)[:, :, 1:2, :], w=[bst])
            cb_, bcb_ = cst4.get()
            cp("pool", cb_[:, :, :], st_[:, :, :], [bst], [bcb_])
            S.dma("pool", wdb_s[ex, :, :, 0:512], cb_[:, 0:4, :], r=[bcb_], w=[DB("wd0", ex)])
            S.dma("pool", wdb_s[ex, :, :, 512:1024], cb_[:, 4:8, :], r=[bcb_], w=[DB("wd1", ex)])
        fence(S)
        mem.reset(mF)
        thr = mem.sb([128, 2, 16]); b_thr = Buf()
        mB = mem.mark()
        for (r, tok0, ntok, cap) in ((0, 0, NLAT, 2 * NLAT // NEXP), (1, NLAT, NCTX, 2 * NCTX // NEXP)):
            per = ntok // 128
            afa = mem.sb([128, per, 16]); b_afa = Buf()
            S.dma("sp", afa[:, :, :], aff_s[tok0:tok0 + ntok, :].rearrange("(p t) e -> p t e", p=128), r=[DB("aff", ta) for ta in range(tok0, tok0 + ntok, 128)], w=[b_afa])
            afv = afa[:, :, :].rearrange("p t e -> p e t")
            cmp_ = mem.sb([128, 16, per]); b_cmp = Buf()
            lo = mem.sb([128, 16]); hi = mem.sb([128, 16]); mid = mem.sb([128, 16]); cnt = mem.sb([128, 16]); prd = mem.sb([128, 16]); dlt = mem.sb([128, 16])
            b_b = Buf()
            S.op("dve", lambda e, lo=lo: e.memset(lo[:, :], 0.0), w=[b_b])
            S.op("dve", lambda e, hi=hi: e.memset(hi[:, :], 1.0), w=[b_b])
            for it in range(30):
                tt("dve", mid[:, :], lo[:, :], hi[:, :], ALU.add, [b_b], [b_b])
                ts("dve", mid[:, :], mid[:, :], 0.5, None, ALU.mult, None, [b_b], [b_b])
                tt("dve", cmp_[:, :, :], afv, mid[:, :].unsqueeze(2).to_broadcast([128, 16, per]), ALU.is_ge, [b_afa, b_b], [b_cmp])
                S.op("dve", lambda e, cnt=cnt, cmp_=cmp_: e.tensor_reduce(out=cnt[:, :], in_=cmp_[:, :, :], axis=AX.X, op=ALU.add), r=[b_cmp], w=[b_b])
                pc, bpc = PE6.get()
                mm(pc[:, 0:16], onesf[:, :], cnt[:, :], True, True, [b_onesf, b_b], [bpc])
                ts("dve", prd[:, :], pc[:, 0:16], float(cap) - 0.5, None, ALU.is_ge, None, [bpc], [b_b])
                tt("dve", dlt[:, :], mid[:, :], lo[:, :], ALU.subtract, [b_b], [b_b])
                tt("dve", dlt[:, :], dlt[:, :], prd[:, :], ALU.mult, [b_b], [b_b])
                tt("dve", lo[:, :], lo[:, :], dlt[:, :], ALU.add, [b_b], [b_b])
                tt("dve", dlt[:, :], hi[:, :], mid[:, :], ALU.subtract, [b_b], [b_b])
                tt("dve", dlt[:, :], dlt[:, :], prd[:, :], ALU.mult, [b_b], [b_b])
                tt("dve", hi[:, :], mid[:, :], dlt[:, :], ALU.add, [b_b], [b_b])
            cp("dve", thr[:, r, :], lo[:, :], [b_b], [b_thr])
            fence(S)
            mem.reset(mB)
        h2l_p = RR(mem, [128, 8, 1024], BF16, 2)
        acc = mem.sb([128, 8, 1024]); b_acc = Buf()
        sgate = mem.sb([128, 8, 16]); afo = mem.sb([128, 8, 16]); b_sg2 = Buf()
        wgl_p = RR(mem, [128, 8, 512], BF16, 2)
        wul_p = RR(mem, [128, 8, 512], BF16, 2)
        wdl_p = RR(mem, [128, 4, 1024], BF16, 2)
        hid_p = RR(mem, [128, 4, 512], BF16, 2)
        sl_t, b_sl = T512()
        xo_p = RR(mem, [128, 1024], F32, 2)
        STILES = [(i * 1024, 1024) for i in range(NLAT // 1024)] + [(NLAT, 256)]
        for (t0, n) in STILES:
            isc = t0 >= NLAT
            r = 1 if isc else 0
            nsub = n // 128
            h2l, bh2l = h2l_p.get()
            S.dma("sp", h2l[:, :, 0:n], h2_s[:, :, t0:t0 + n].rearrange("k p t -> p k t"), r=[DB("h2", ta) for ta in range(t0, t0 + n, 128)], w=[bh2l])
            S.dma("pool", afo[:, 0:nsub, :], aff_s[t0:t0 + n, :].rearrange("(s t) e -> t s e", t=128), r=[DB("aff", ta) for ta in range(t0, t0 + n, 128)], w=[b_sg2])
            tt("dve", sgate[:, 0:nsub, :], afo[:, 0:nsub, :], thr[:, r, :].unsqueeze(1).to_broadcast([128, nsub, 16]), ALU.is_ge, [b_sg2, b_thr], [b_sg2])
            tt("dve", sgate[:, 0:nsub, :], sgate[:, 0:nsub, :], afo[:, 0:nsub, :], ALU.mult, [b_sg2], [b_sg2])
            S.op("pool", lambda e: e.memset(acc[:, :, :], 0.0), w=[b_acc])
            for ex in range(NEXP):
                wgl, bwg = wgl_p.get()
                wul, bwu = wul_p.get()
                wdl, bwd = wdl_p.get()
                S.dma("sp", wgl[:, :, :], wgb_s[ex, :, :, :], r=[DB("wg", ex)], w=[bwg])
                S.dma("pool", wul[:, :, :], wub_s[ex, :, :, :], r=[DB("wu", ex)], w=[bwu])
                S.dma("sp", wdl[:, :, :], wdb_s[ex, :, :, :], r=[DB("wd0", ex), DB("wd1", ex)], w=[bwd])
                for tt0 in range(0, n, 512):
                    nn = min(512, n - tt0)
                    hid, bhid = hid_p.get()
                    for fc in range(4):
                        pg_, bpg_ = PE6.get()
                        for k in range(8):
                            mm(pg_[:, 0:nn], wgl[:, k, fc * 128:(fc + 1) * 128], h2l[:, k, tt0:tt0 + nn], k == 0, k == 7, [bwg, bh2l], [bpg_])
                        pu2, bpu2 = PE6.get()
                        for k in range(8):
                            mm(pu2[:, 0:nn], wul[:, k, fc * 128:(fc + 1) * 128], h2l[:, k, tt0:tt0 + nn], k == 0, k == 7, [bwu, bh2l], [bpu2])
                        act(sl_t[:, 0:nn], pg_[:, 0:nn], AF.Silu, [bpg_], [b_sl])
                        tt("dve", hid[:, fc, 0:nn], sl_t[:, 0:nn], pu2[:, 0:nn], ALU.mult, [b_sl, bpu2], [bhid])
                    for sub in range(nn // 128):
                        sa = (tt0 // 128) + sub
                        for half in range(2):
                            pdn, bpdn = PE6.get()
                            for fc in range(4):
                                mm(pdn[:, :], hid[:, fc, sub * 128:(sub + 1) * 128], wdl[:, fc, half * 512:(half + 1) * 512], fc == 0, fc == 3, [bhid, bwd], [bpdn])
                            stt("dve", acc[:, sa, half * 512:(half + 1) * 512], pdn[:, :], sgate[:, sa, ex:ex + 1], acc[:, sa, half * 512:(half + 1) * 512], ALU.mult, ALU.add, [bpdn, b_sg2, b_acc], [b_acc])
            dst, do = xdst(t0)
            for sub in range(nsub):
                ta = t0 + sub * 128
                xo, bxo = xo_p.get()
                S.dma("sp", xo[:, :], xm_s[ta:ta + 128, :], r=[DB("xm", ta)], w=[bxo])
                tt("pool", acc[:, sub, :], acc[:, sub, :], G2[:, r, :], ALU.mult, [b_acc, b_G], [b_acc])
                tt("pool", xo[:, :], xo[:, :], acc[:, sub, :], ALU.add, [bxo, b_acc], [bxo])
                S.dma("sp", dst[do + sub * 128:do + (sub + 1) * 128, :], xo[:, :], r=[bxo], final=True)

    LBASE = mem.mark()
    try:
        for li, layer in enumerate(layers):
            first, last = li == 0, li == len(layers) - 1
            xin = x_in_d if first else xbuf[(li - 1) % 2]
            cxin = cx_in_d if first else cxbuf[(li - 1) % 2]
            xout = xo_out_d if last else xbuf[li % 2]
            cxout = cxo_out_d if last else cxbuf[li % 2]
            layer_body(layer, xin, cxin, xout, cxout, wmod_a[li], bmodT_a[li], n1T_a[li], n2T_a[li], win_a[li], wout_a[li], wr_a[li],
                       hvec_a[li], cw_a[li], tt_a[li], None if early else wg_a[li], None if early else wu_a[li], None if early else wd_a[li])
            fence(S)
            mem.reset(LBASE)
    except _Stop:
        pass
    S.emit()
    return nc


def _rm_patterns(nlat=16384):
    NROWS = nlat // 64
    pats = []
    for g in range(nlat // 512):
        for p in range(8):
            if not (0 <= 8 * g - 4 + 2 * p < NROWS):
                continue
            t = []
            for a in range(2):
                for i in range(8):
                    kr = 8 * g - 4 + 2 * p + a
                    rq = 8 * g + i
                    rs0 = min(max(rq - 4, 0), NROWS - 8)
                    t.append(rs0 <= kr < rs0 + 8)
            t = tuple(t)
            if t not in pats:
                pats.append(t)
    return pats


def _consts(pats, nlat=16384):
    NLAT = nlat
    NTOK = NLAT + NCTX
    NEG = -30000.0
    idn = np.eye(128, dtype=np.float32)
    blk = np.zeros((128, 128), np.float32)
    blk[:64, :64] = 1
    blk[64:, 64:] = 1
    rot = np.zeros((128, 128), np.float32)
    for hb in (0, 64):
        for m in range(64):
            q = m // 16
            if q in (0, 2):
                rot[hb + m + 16, hb + m] = -1.0
            else:
                rot[hb + m - 16, hb + m] = 1.0
    s = np.arange(64)
    mf = (s[:, None] <= s[None, :]).astype(np.float32)
    mb = (s[:, None] >= s[None, :]).astype(np.float32)
    cst = np.stack([idn, blk, rot, np.tile(mf, (2, 2)), np.tile(mb, (2, 2))]).astype(np.float32)
    rma = np.zeros((len(pats), 128, 128), np.float32)
    for pi, pt in enumerate(pats):
        for a in range(2):
            for i in range(8):
                if not pt[a * 8 + i]:
                    rma[pi, a * 8 + i, a * 64:(a + 1) * 64] = NEG
    rmb = np.zeros((128, 8, 64), np.float32)
    for a in range(2):
        for i in range(8):
            rmb[a * 8 + i, i, :] = 1.0
    rmb = rmb.reshape(128, 512)
    t = np.arange(NLAT)
    row = (t // 64).astype(np.float32)
    col = (t % 64).astype(np.float32)
    inv = (np.float32(10000.0) ** (-np.arange(16, dtype=np.float32) / np.float32(16))).astype(np.float32)
    ar = row[:, None] * inv
    ac = col[:, None] * inv
    ang = np.concatenate([ar, ar, ac, ac], axis=-1)
    cos = np.ones((128, NTOK), np.float32)
    sin = np.zeros((128, NTOK), np.float32)
    cos[:64, :NLAT] = np.cos(ang).T
    cos[64:, :NLAT] = np.cos(ang).T
    sin[:64, :NLAT] = np.sin(ang).T
    sin[64:, :NLAT] = np.sin(ang).T
    return cst, rma, rmb, cos, sin


def _tt_table(rpb):
    NEG = -30000.0
    tt = np.zeros((128, 6, 22, 64), np.float32)
    qc = np.arange(64)
    ws = np.clip(qc - 8, 0, 48)
    kc = np.arange(64)
    inwin = (kc[:, None] >= ws[None, :]) & (kc[:, None] < ws[None, :] + 16)
    rel = np.clip(kc[:, None] - qc[None, :] + 15, 0, 30)
    for a in range(2):
        for u in range(22):
            dr = a + 10 - u
            if abs(dr) <= 7:
                vals = rpb[:, dr + 7, :][:, rel]
                blk = np.where(inwin[None], vals, np.float32(NEG))
            else:
                blk = np.zeros((6, 64, 64), np.float32)
            tt[a * 64:(a + 1) * 64, :, u, :] = blk.transpose(1, 0, 2)
    return np.ascontiguousarray(tt.reshape(128, 6 * 22 * 64))


_NC_CACHE = {}


def _f(a):
    return np.ascontiguousarray(np.asarray(a, dtype=np.float32))


def _fm(v):
    return np.ascontiguousarray(_f(v).reshape(-1, 128).T)


def _common(layers, W, consts):
    cst, rma, rmb, cos, sin = consts
    lbl = np.ascontiguousarray(_f(W["hg_lb_logits"]).reshape(2, 4, 3, 128).transpose(3, 2, 0, 1))
    st = lambda fn: np.ascontiguousarray(np.stack([fn(l) for l in layers], axis=0))
    return {
        "wmod": st(lambda l: _f(W["w_mod"][l])), "bmodT": st(lambda l: _fm(W["b_mod"][l])),
        "n1T": st(lambda l: _fm(W["norm1_w"][l])), "n2T": st(lambda l: _fm(W["norm2_w"][l])),
        "win": st(lambda l: _f(W["w_in"][l])), "wout": st(lambda l: _f(W["w_out"][l])), "wr": st(lambda l: _f(W["w_router"][l])),
        "hvec": st(lambda l: np.stack([np.tile(_f(W["na_q_norm"][l]), 2), np.tile(_f(W["na_k_norm"][l]), 2),
                                       np.tile(_f(W["hg_norm"][l]), 2)], axis=1)),
        "lbl": lbl, "cw": st(lambda l: _f(W["conv_w"][l]).reshape(3, 2, 128).transpose(2, 1, 0)),
        "tt": st(lambda l: _tt_table(_f(W["na_rpb"][l]))), "rma": rma, "rmb": rmb, "cst": cst, "cos": cos, "sin": sin,
        "wg": st(lambda l: _f(W["w_exp_gate"][l])), "wu": st(lambda l: _f(W["w_exp_up"][l])), "wd": st(lambda l: _f(W["w_exp_down"][l])),
    }


def run_layer(nc, common, xs, cxs, c, c_ctx, ncores=2):
    names = set(a.memorylocations[0].name for a in nc.m.functions[0].allocations
                if isinstance(a, mybir.MemoryLocationSet) and a.kind == "ExternalInput")
    in_maps = []
    for core in range(ncores):
        b = core % 2
        c2 = np.stack([c[b], c_ctx], axis=0)
        c2T = np.ascontiguousarray(c2.reshape(2, 8, 128).transpose(2, 1, 0))
        d = dict(common)
        d.update({"x": xs[b], "cx": cxs[b], "c2T": c2T})
        in_maps.append({k: v for k, v in d.items() if k in names})
    return run_bass_kernel_spmd(nc, in_maps, core_ids=list(range(ncores)))


def kernel(x, c, ctx, c_ctx, w_mod, b_mod, norm1_w, w_in, na_q_norm, na_k_norm, na_rpb, hg_lb_logits, hg_norm, conv_w,
           w_out, norm2_w, w_router, w_exp_gate, w_exp_up, w_exp_down):
    W = dict(w_mod=w_mod, b_mod=b_mod, norm1_w=norm1_w, w_in=w_in, na_q_norm=na_q_norm, na_k_norm=na_k_norm, na_rpb=na_rpb,
             hg_lb_logits=hg_lb_logits, hg_norm=hg_norm, conv_w=conv_w, w_out=w_out, norm2_w=norm2_w, w_router=w_router,
             w_exp_gate=w_exp_gate, w_exp_up=w_exp_up, w_exp_down=w_exp_down)
    x = _f(x); ctx = _f(ctx); c = _f(c); c_ctx = _f(c_ctx)
    nlat = x.shape[1]
    pats = _rm_patterns(nlat)
    consts = _consts(pats, nlat)
    xs = [x[0], x[1]]
    cxs = [ctx[0], ctx[1]]
    layers = [0, 1, 2, 3]
    key = (tuple(layers), nlat)
    if key not in _NC_CACHE:
        _NC_CACHE[key] = build_model(layers, pats, nlat)
    res = run_layer(_NC_CACHE[key], _common(layers, W, consts), xs, cxs, c, c_ctx)
    xs = [np.asarray(res.results[b]["xo"], dtype=np.float32) for b in range(2)]
    return np.stack(xs, axis=0).astype(np.float32)
```

```python
import contextlib
import numpy as np
import concourse.bass as bass
import concourse.mybir as mybir
from concourse.bass_utils import run_bass_kernel_spmd

F32 = mybir.dt.float32
BF16 = mybir.dt.bfloat16
ALU = mybir.AluOpType
AF = mybir.ActivationFunctionType
AX = mybir.AxisListType

ENGS = ("sp", "act", "dve", "pool", "pe")
NSLOT = 8


class Buf:
    __slots__ = ("name", "w", "rs")

    def __init__(self, name=""):
        self.name = name
        self.w = None
        self.rs = []


class Ins:
    __slots__ = ("eng", "fn", "deps", "signal", "tick", "dma", "n", "k")


class Sched:
    def __init__(self, nc):
        self.nc = nc
        self.q = {e: [] for e in ENGS}
        self.ndma = {e: 0 for e in ENGS}
        self.stack = contextlib.ExitStack()
        self.finals = []
        self._nm = 0

    def sb(self, shape, dt=F32, name=None):
        self._nm += 1
        return self.stack.enter_context(self.nc.sbuf_tensor(name or f"sb{self._nm}", list(shape), dt))

    def ps(self, shape, dt=F32, name=None):
        self._nm += 1
        return self.stack.enter_context(self.nc.psum_tensor(name or f"ps{self._nm}", list(shape), dt))

    def dram(self, name, shape, dt=F32, kind="Internal"):
        return self.nc.dram_tensor(name, list(shape), dt, kind=kind)

    def op(self, eng, fn, r=(), w=(), dma=False):
        ins = Ins()
        ins.eng = eng
        ins.fn = fn
        ins.dma = dma
        ins.signal = False
        ins.tick = 0
        deps = {}
        for b in r:
            if b.w is not None:
                deps[id(b.w)] = b.w
        for b in w:
            if b.w is not None:
                deps[id(b.w)] = b.w
            for rd in b.rs:
                deps[id(rd)] = rd
        ins.deps = list(deps.values())
        for b in r:
            if not dma:
                b.rs = [x for x in b.rs if x.dma or x.eng != eng]
            b.rs.append(ins)
        for b in w:
            b.w = ins
            b.rs = []
        ins.k = len(self.q[eng])
        if dma:
            ins.n = self.ndma[eng]
            self.ndma[eng] += 1
        self.q[eng].append(ins)
        return ins

    def dma(self, eng, out, in_, r=(), w=(), final=False):
        ins = self.op(eng, lambda e: e.dma_start(out=out, in_=in_), r=r, w=w, dma=True)
        if final:
            self.finals.append(ins)
        return ins

    def emit(self):
        nc = self.nc
        for e in ENGS:
            for ins in self.q[e]:
                for d in ins.deps:
                    if d.dma:
                        continue
                    if d.eng == ins.eng:
                        if d.eng == "pe" and not ins.dma:
                            continue
                        if ins.dma or (ins.k - d.k) <= 8:
                            d.signal = True
                    else:
                        d.signal = True
        for e in ENGS:
            t = 0
            for ins in self.q[e]:
                if ins.signal and not ins.dma:
                    t += 1
                    ins.tick = t
        st = self.stack
        csem = {e: st.enter_context(nc.semaphore(f"c_{e}")) for e in ENGS}
        dsem = {e: [st.enter_context(nc.semaphore(f"d_{e}{i}")) for i in range(NSLOT)] for e in ("sp", "act", "pool")}
        engobj = {}
        finals = self.finals

        def run(e, eng):
            waited = {}

            def wait(sem, val):
                key = id(sem)
                if waited.get(key, 0) >= val:
                    return
                waited[key] = val
                eng.wait_ge(sem, val)

            for ins in self.q[e]:
                for d in ins.deps:
                    if d.dma:
                        wait(dsem[d.eng][d.n % NSLOT], 16 * (d.n // NSLOT + 1))
                    elif d.signal:
                        if d.eng == e and not ins.dma and (e == "pe" or (ins.k - d.k) > 8):
                            continue
                        wait(csem[d.eng], d.tick)
                if ins.dma:
                    if ins.n >= NSLOT:
                        wait(dsem[e][ins.n % NSLOT], 16 * (ins.n // NSLOT))
                    ins.fn(eng).then_inc(dsem[e][ins.n % NSLOT], 16)
                else:
                    bi = ins.fn(eng)
                    if ins.signal:
                        bi.then_inc(csem[e], 1)
            if e == "sp":
                for qe in ("sp", "act", "pool"):
                    n = self.ndma[qe]
                    for s in range(min(n, NSLOT)):
                        last = ((n - 1 - s) // NSLOT) * NSLOT + s
                        wait(dsem[qe][s], 16 * (last // NSLOT + 1))

        with nc.allow_non_contiguous_dma(reason="small strided scratch DMAs"), nc.Block() as block:
            @block.sync
            def _(eng):
                run("sp", eng)

            @block.scalar
            def _(eng):
                run("act", eng)

            @block.vector
            def _(eng):
                run("dve", eng)

            @block.gpsimd
            def _(eng):
                run("pool", eng)

            @block.tensor
            def _(eng):
                run("pe", eng)
        self.stack.close()


NLAT = 16384
NCTX = 256
NTOK = NLAT + NCTX
DM = 1024
NIN = 3840
NEXP = 16
EDIM = 512
TILES = [(i * 512, 512) for i in range(32)] + [(NLAT, 256)]
SB_BASE = 16512
SB_TOP = 229344


class Mem:
    def __init__(self, S):
        self.S = S
        self.off = SB_BASE
        self.n = 0

    def sb(self, shape, dt=F32, name=None):
        sz = int(np.prod(shape[1:])) * (2 if dt == BF16 else 4)
        sz = (sz + 31) // 32 * 32
        self.n += 1
        t = self.S.nc.alloc_sbuf_tensor_at(f"m{self.n}" + (("_" + name) if name else ""), list(shape), dt, offset=self.off)
        self.off += sz
        assert self.off <= SB_TOP, f"SBUF overflow {self.off}"
        return t

    def mark(self):
        return self.off

    def reset(self, m):
        self.off = m


class RR:
    def __init__(self, mem, shape, dt, n):
        self.t = [mem.sb(shape, dt) for _ in range(n)]
        self.b = [Buf() for _ in range(n)]
        self.i = 0

    def get(self):
        i = self.i
        self.i = (i + 1) % len(self.t)
        return self.t[i], self.b[i]


class PRR:
    def __init__(self, tiles):
        self.t = tiles
        self.b = [Buf() for _ in tiles]
        self.i = 0

    def get(self):
        i = self.i
        self.i = (i + 1) % len(self.t)
        return self.t[i], self.b[i]


def fence(S):
    lasts = []
    for e in ENGS:
        q = S.q[e]
        nd = 0
        seen_c = False
        for ins in reversed(q):
            if ins.dma:
                if nd < NSLOT:
                    lasts.append(ins)
                    nd += 1
            elif not seen_c:
                lasts.append(ins)
                seen_c = True
            if nd >= NSLOT and seen_c:
                break
    fb = Buf()
    for e in ENGS:
        ins = S.op(e, lambda eng: eng.nop(), r=(), w=())
        ins.deps = [d for d in lasts if d is not ins]


def build_model(layers, rm_patterns, nlat=16384, debug=False, stop=None):
    NL_ = len(layers)
    NLAT = nlat
    NTOK = NLAT + NCTX
    NROWS = NLAT // 64
    TILES = [(i * 512, 512) for i in range(NLAT // 512)] + [(NLAT, 256)]
    nc = bass.Bass("TRN2", target_bir_lowering=False)
    S = Sched(nc)
    mem = Mem(S)
    EI = "ExternalInput"

    class _Stop(Exception):
        pass

    def chk(name):
        if stop == name:
            raise _Stop()


    def din(name, shape, dt=F32):
        return nc.dram_tensor(name, list(shape), dt, kind=EI)

    x_in_d = din("x", [NLAT, DM])
    cx_in_d = din("cx", [NCTX, DM])
    c2T_d = din("c2T", [128, 8, 2])
    wmod_a = din("wmod", [NL_, DM, 6 * DM])
    bmodT_a = din("bmodT", [NL_, 128, 48])
    n1T_a = din("n1T", [NL_, 128, 8])
    n2T_a = din("n2T", [NL_, 128, 8])
    win_a = din("win", [NL_, DM, NIN])
    wout_a = din("wout", [NL_, DM, DM])
    wr_a = din("wr", [NL_, DM, NEXP])
    hvec_a = din("hvec", [NL_, 128, 3])
    lbl_d = din("lbl", [128, 3, 2, 4])
    cw_a = din("cw", [NL_, 128, 2, 3])
    tt_a = din("tt", [NL_, 128, 6 * 22 * 64])
    rma_d = din("rma", [len(rm_patterns), 128, 128])
    rmb_d = din("rmb", [128, 512])
    cst_d = din("cst", [5, 128, 128])
    cos_d = din("cos", [128, NTOK])
    sin_d = din("sin", [128, NTOK])
    early = stop is not None and (stop.startswith("A") or stop == "pro")
    wg_a = None if early else din("wg", [NL_, NEXP, DM, EDIM])
    wu_a = None if early else din("wu", [NL_, NEXP, DM, EDIM])
    wd_a = None if early else din("wd", [NL_, NEXP, EDIM, DM])
    xo_out_d = nc.dram_tensor("xo", [NLAT, DM], F32, kind="ExternalOutput")
    cxo_out_d = nc.dram_tensor("cxo", [NCTX, DM], F32, kind="ExternalOutput")
    xbuf = [nc.dram_tensor(f"xbuf{i}", [NLAT, DM], F32) for i in range(2)]
    cxbuf = [nc.dram_tensor(f"cxbuf{i}", [NCTX, DM], F32) for i in range(2)]

    def xsrc(t0, n):
        return (x_d, t0) if t0 < NLAT else (cx_d, t0 - NLAT)

    def xdst(t0):
        return (xo_d, t0) if t0 < NLAT else (cxo_d, t0 - NLAT)

    def scr(name, shape, dt=F32):
        if debug and name in ("mix_s", "xm_s", "aff_s", "QT_s", "KT_s", "V_s", "oacc_s", "h2_s"):
            return nc.dram_tensor(name, list(shape), dt, kind="ExternalOutput")
        return nc.dram_tensor(name, list(shape), dt)

    QT_s = scr("QT_s", [3, 2, 128, NTOK], BF16)
    KT_s = scr("KT_s", [3, 128, NTOK], BF16)
    V_s = scr("V_s", [NTOK, 384], BF16)
    mix_s = scr("mix_s", [8, 128, NTOK], BF16)
    oacc_s = scr("oacc_s", [3, 128, NTOK])
    qhat_s = scr("qhat_s", [2, 3, 128, NTOK], BF16)
    U_s = scr("U_s", [2, 3, 128, NTOK // 64, 64])
    D_s = scr("D_s", [2, 3, 128, NTOK // 64])
    gs_s = scr("gs_s", [3, 128, NTOK])
    z_s = scr("z_s", [2, 128, NTOK + 4])
    cb_s = scr("cb_s", [2, 128, NTOK], BF16)
    xm_s = scr("xm_s", [NTOK, DM])
    h2_s = scr("h2_s", [8, 128, NTOK], BF16)
    aff_s = scr("aff_s", [NTOK, NEXP])
    wgb_s = scr("wgb_s", [NEXP, 128, 8, EDIM], BF16)
    wub_s = scr("wub_s", [NEXP, 128, 8, EDIM], BF16)
    wdb_s = scr("wdb_s", [NEXP, 128, 4, DM], BF16)
    dbuf = {}

    def DB(*key):
        if key not in dbuf:
            dbuf[key] = Buf()
        return dbuf[key]

    pfs = [S.ps([128, 512], F32) for _ in range(6)]
    pbs = [S.ps([128, 8, 128], BF16) for _ in range(2)]
    PF = PRR(pfs)
    PB = PRR(pbs)

    def mm(out, lhsT, rhs, st, sp_, r, w):
        return S.op("pe", lambda e: e.matmul(out, lhsT, rhs, start=st, stop=sp_), r=r, w=w)

    def tr(out, in_, idn, r, w):
        return S.op("pe", lambda e: e.transpose(out=out, in_=in_, identity=idn), r=r, w=w)

    def act(out, in_, func, r, w, **kw):
        return S.op("act", lambda e: e.activation(out=out, in_=in_, func=func, **kw), r=r, w=w)

    def tt(eng, out, in0, in1, op, r, w):
        return S.op(eng, lambda e: e.tensor_tensor(out=out, in0=in0, in1=in1, op=op), r=r, w=w)

    def ts(eng, out, in0, s1, s2, op0, op1, r, w):
        if s2 is None:
            return S.op(eng, lambda e: e.tensor_scalar(out=out, in0=in0, scalar1=s1, scalar2=None, op0=op0), r=r, w=w)
        return S.op(eng, lambda e: e.tensor_scalar(out=out, in0=in0, scalar1=s1, scalar2=s2, op0=op0, op1=op1), r=r, w=w)

    def stt(eng, out, in0, sc, in1, op0, op1, r, w):
        return S.op(eng, lambda e: e.scalar_tensor_tensor(out=out, in0=in0, scalar=sc, in1=in1, op0=op0, op1=op1), r=r, w=w)

    def cp(eng, out, in_, r, w):
        return S.op(eng, lambda e: e.tensor_copy(out=out, in_=in_), r=r, w=w)

    def recip(out, in_, r, w):
        return S.op("dve", lambda e: e.reciprocal(out=out, in_=in_), r=r, w=w)

    cstf = mem.sb([128, 5, 128]); b_cstf = Buf()
    S.dma("sp", cstf[:, :, :], cst_d[:, :, :].rearrange("c p n -> p c n"), w=[b_cstf])
    cstb = mem.sb([128, 5, 128], BF16); b_cst = Buf()
    cp("dve", cstb[:, :, :], cstf[:, :, :], [b_cstf], [b_cst])
    idb = cstb[:, 0, :]; blkb = cstb[:, 1, :]; rotb = cstb[:, 2, :]
    idf = cstf[:, 0, :]
    onesf = mem.sb([128, 128]); b_onesf = Buf()
    S.op("pool", lambda e: e.memset(onesf[:, :], 1.0), w=[b_onesf])
    onesb = mem.sb([128, 128], BF16); b_onesb = Buf()
    S.op("pool", lambda e: e.memset(onesb[:, :], 1.0), w=[b_onesb])

    def layer_body(layer, x_d, cx_d, xo_d, cxo_d, wmod_d, bmodT_d, n1T_d, n2T_d, win_d, wout_d, wr_d, hvec_d, cw_d, tt_d, wg_d, wu_d, wd_d):
        def xsrc(t0, n):
            return (x_d, t0) if t0 < NLAT else (cx_d, t0 - NLAT)

        def xdst(t0):
            return (xo_d, t0) if t0 < NLAT else (cxo_d, t0 - NLAT)

        modT = mem.sb([128, 48, 2]); b_mod = Buf()
        c2T = mem.sb([128, 8, 2]); b_c2 = Buf()
        S.dma("sp", c2T[:, :, :], c2T_d[:, :, :], w=[b_c2])
        scT = mem.sb([128, 8, 2]); b_sc = Buf()
        act(scT[:, :, :], c2T[:, :, :], AF.Silu, [b_c2], [b_sc])
        bmodT = mem.sb([128, 48]); b_bm = Buf()
        S.dma("sp", bmodT[:, :], bmodT_d[:, :], w=[b_bm])
        m0 = mem.mark()
        wmp = RR(mem, [128, 8, 1024], F32, 2)
        pm, bpm = PF.get()
        for m in range(6):
            wt, bw = wmp.get()
            for k in range(8):
                S.dma("sp" if k % 2 == 0 else "pool", wt[:, k, :], wmod_d[k * 128:(k + 1) * 128, m * 1024:(m + 1) * 1024], w=[bw])
            for oc in range(8):
                for k in range(8):
                    mm(pm[:, (m * 8 + oc) * 2:(m * 8 + oc) * 2 + 2], wt[:, k, oc * 128:(oc + 1) * 128], scT[:, k, :], k == 0, k == 7, [bw, b_sc], [bpm])
        tt("dve", modT[:, :, :], pm[:, 0:96].rearrange("p (a r) -> p a r", r=2), bmodT[:, :].unsqueeze(2).to_broadcast([128, 48, 2]), ALU.add, [bpm, b_bm], [b_mod])
        mem.reset(m0)
        fence(S)
        n1T = mem.sb([128, 8]); n2T = mem.sb([128, 8]); b_n = Buf()
        S.dma("sp", n1T[:, :], n1T_d[:, :], w=[b_n])
        S.dma("sp", n2T[:, :], n2T_d[:, :], w=[b_n])
        A1 = mem.sb([128, 8, 2]); A2 = mem.sb([128, 8, 2]); b_A = Buf()
        for (A, nT, ms) in ((A1, n1T, 1), (A2, n2T, 4)):
            ts("dve", A[:, :, :], modT[:, ms * 8:(ms + 1) * 8, :], 1.0, None, ALU.add, None, [b_mod], [b_A])
            tt("dve", A[:, :, :], A[:, :, :], nT[:, :].unsqueeze(2).to_broadcast([128, 8, 2]), ALU.mult, [b_A, b_n], [b_A])

        def Bm(ms, k, r):
            return modT[:, ms * 8 + k, r:r + 1]

        G1 = mem.sb([128, 2, 1024]); G2 = mem.sb([128, 2, 1024]); b_G = Buf()
        dg = mem.sb([128, 128]); b_dg = Buf()
        for (G, ms) in ((G1, 2), (G2, 5)):
            for r in range(2):
                for half in range(2):
                    pg, bpg = PF.get()
                    for kk in range(4):
                        k = half * 4 + kk
                        ts("dve", dg[:, :], idf, modT[:, ms * 8 + k, r:r + 1], None, ALU.mult, None, [b_cstf, b_mod], [b_dg])
                        mm(pg[:, kk * 128:(kk + 1) * 128], onesf[:, :], dg[:, :], True, True, [b_onesf, b_dg], [bpg])
                    cp("dve", G[:, r, half * 512:(half + 1) * 512], pg[:, :], [bpg], [b_G])
        hvec = mem.sb([128, 3]); b_hv = Buf()
        S.dma("sp", hvec[:, :], hvec_d[:, :], w=[b_hv])
        qw8 = mem.sb([128, 1])
        ts("dve", qw8[:, :], hvec[:, 0:1], 0.125, None, ALU.mult, None, [b_hv], [b_hv])
        lbl = mem.sb([128, 3, 2, 4]); b_lb = Buf()
        S.dma("sp", lbl[:, :, :, :], lbl_d[:, :, :, :], w=[b_lb])
        lmx = mem.sb([128, 3, 2]); lsum = mem.sb([128, 3, 2]); lbv = mem.sb([128, 3, 2]); omlb = mem.sb([128, 3, 2])
        S.op("dve", lambda e: e.tensor_reduce(out=lmx[:, :, :], in_=lbl[:, :, :, :], axis=AX.X, op=ALU.max), r=[b_lb], w=[b_lb])
        tt("dve", lbl[:, :, :, :], lbl[:, :, :, :], lmx[:, :, :].unsqueeze(3).to_broadcast([128, 3, 2, 4]), ALU.subtract, [b_lb], [b_lb])
        act(lbl[:, :, :, :], lbl[:, :, :, :], AF.Exp, [b_lb], [b_lb])
        S.op("dve", lambda e: e.tensor_reduce(out=lsum[:, :, :], in_=lbl[:, :, :, :], axis=AX.X, op=ALU.add), r=[b_lb], w=[b_lb])
        recip(lsum[:, :, :], lsum[:, :, :], [b_lb], [b_lb])
        S.op("dve", lambda e: e.memset(lbv[:, :, :], 0.0), r=[b_lb], w=[b_lb])
        for jl in range(1, layer + 1):
            tt("dve", lbv[:, :, :], lbv[:, :, :], lbl[:, :, :, jl], ALU.add, [b_lb], [b_lb])
        tt("dve", lbv[:, :, :], lbv[:, :, :], lsum[:, :, :], ALU.mult, [b_lb], [b_lb])
        ts("dve", omlb[:, :, :], lbv[:, :, :], -1.0, 1.0, ALU.mult, ALU.add, [b_lb], [b_lb])
        cw = mem.sb([128, 2, 3]); b_cw = Buf()
        S.dma("sp", cw[:, :, :], cw_d[:, :, :], w=[b_cw])
        PBASE = mem.mark()

        if stop == "pro":
            raise _Stop()
        def norm_tile(src_d, row0, r, A, ms_shift, xt, bx, sm, hT, bhT, col0, fp32_out=None):
            junk, bj, ss, rt, rs_, xn, bxn, bsm = sm
            act(junk[:, :], xt[:, :], AF.Square, [bx], [bj, bsm], accum_out=ss[:, :])
            act(rt[:, :], ss[:, :], AF.Sqrt, [bsm], [bsm], scale=1.0 / DM, bias=1e-6)
            recip(rs_[:, :], rt[:, :], [bsm], [bsm])
            act(xn[:, :], xt[:, :], AF.Copy, [bx, bsm], [bxn], scale=rs_[:, 0:1])
            pt, bpt = PB.get()
            for k in range(8):
                tr(pt[:, k, :], xn[:, k * 128:(k + 1) * 128], idb, [bxn, b_cst], [bpt])
            for k in range(8):
                if k % 2 == 0:
                    ts("dve", hT[:, k, col0:col0 + 128], pt[:, k, :], A[:, k, r:r + 1], Bm(ms_shift, k, r), ALU.mult, ALU.add, [bpt, b_A, b_mod], [bhT])
                else:
                    act(hT[:, k, col0:col0 + 128], pt[:, k, :], AF.Identity, [bpt, b_A, b_mod], [bhT], scale=A[:, k, r:r + 1], bias=Bm(ms_shift, k, r))

        def norm_scratch(mem):
            junk = mem.sb([128, 1024], BF16)
            ss = mem.sb([128, 1]); rt = mem.sb([128, 1]); rs_ = mem.sb([128, 1])
            xn = mem.sb([128, 1024], BF16)
            return (junk, Buf(), ss, rt, rs_, xn, Buf(), Buf())

        Wb = mem.sb([128, 8, NIN], BF16); b_W = Buf()
        stg = RR(mem, [128, 1920], F32, 2)
        for k in range(8):
            for hf in range(2):
                st_, bst = stg.get()
                S.dma("sp" if hf == 0 else "pool", st_[:, :], win_d[k * 128:(k + 1) * 128, hf * 1920:(hf + 1) * 1920], w=[bst])
                cp("pool" if hf == 0 else "dve", Wb[:, k, hf * 1920:(hf + 1) * 1920], st_[:, :], [bst], [b_W])
        xtp = RR(mem, [128, 1024], F32, 2)
        nsm = norm_scratch(mem)
        hTp = RR(mem, [128, 8, 512], BF16, 2)
        cosp = RR(mem, [128, 512], F32, 2)
        sinp = RR(mem, [128, 512], F32, 2)
        zero_t = mem.sb([128, 512], BF16); b_zero = Buf()
        S.op("pool", lambda e: e.memset(zero_t[:, :], 0.0), w=[b_zero])
        zero_f = mem.sb([128, 4]); b_zf = Buf()
        S.op("pool", lambda e: e.memset(zero_f[:, :], 0.0), w=[b_zf])
        ones64 = mem.sb([128, 64]); b_o64 = Buf()
        S.op("pool", lambda e: e.memset(ones64[:, :], 1.0), w=[b_o64])
        for cc in range(2):
            for col in (0, NLAT + 1, NLAT + 2, NTOK + 3):
                S.dma("pool", z_s[cc, :, col:col + 1], zero_f[:, 0:1], r=[b_zf], w=[DB("z", cc, "pad", col)])

        def T512(dt=F32, name=None):
            return mem.sb([128, 512], dt, name), Buf()

        sq_t, b_sq = T512(BF16, name="sq_t")
        rt_t, b_rt = T512(name="rt_t")
        qn_t, b_qn = T512(BF16, name="qn_t")
        t1_t, b_t1 = T512(name="t1_t")
        t2_t, b_t2 = T512(name="t2_t")
        qo_t, b_qo = T512(BF16, name="qo_t")
        vt_p = RR(mem, [128, 384], BF16, 2)
        itm = mem.sb([128, 4, 384], BF16); b_itm = Buf()
        hq_t, b_hq = T512(name="hq_t")
        sg_t, b_sg = T512(name="sg_t")
        lf_t, b_lf = T512(name="lf_t")
        kk_t, b_kk = T512(name="kk_t")
        A_t, b_At = T512(name="A_t")
        a_t, b_a = T512(name="a_t")
        d_t, b_d = T512(name="d_t")
        e_t, b_e = T512(name="e_t")
        e2_t, b_e2 = T512(name="e2_t")
        qtl_t, b_qtl = T512(BF16, name="qtl_t")
        ktl_t, b_ktl = T512(BF16, name="ktl_t")
        qh_t, b_qh = T512(BF16, name="qh_t")
        kh_t, b_kh = T512(BF16, name="kh_t")
        khtm = mem.sb([128, 4, 128], BF16); b_khtm = Buf()
        Tt = mem.sb([128, 8]); rr_t = mem.sb([128, 16]); Dt = mem.sb([128, 8]); bn_t = mem.sb([128, 8]); b_T = Buf(); b_rr = Buf(); b_D = Buf(); b_bn = Buf()
        kz = [mem.sb([128, 512], BF16, "kzf"), mem.sb([128, 512], BF16, "kzb")]
        b_kz = [Buf(), Buf()]
        for i_ in range(2):
            S.op("pool", lambda e, t=kz[i_]: e.memset(t[:, :], 0.0), w=[b_kz[i_]])
        scm = mem.sb([128, 128], BF16); b_scm = Buf()
        Ut = mem.sb([128, 8, 64]); b_U = Buf()
        osb, b_osb = T512()
        gs_t, b_gs = T512()
        cs_t, b_cs = T512()
        cb_t, b_cb = T512(BF16)
        po_ps, b_po = [pfs[2], pfs[3]], [Buf(), Buf()]
        pu_ps, b_pu = [pfs[4], pfs[5]], [Buf(), Buf()]
        PF4 = PRR(pfs[0:2])

        HSETS = [dict(sg=(sg_t, b_sg), lf=(lf_t, b_lf), kk=(kk_t, b_kk), A=(A_t, b_At), a=(a_t, b_a), d=(d_t, b_d), e=(e_t, b_e), e2=(e2_t, b_e2),
                      qtl=(qtl_t, b_qtl), ktl=(ktl_t, b_ktl), qh=(qh_t, b_qh), kh=(kh_t, b_kh), T=(Tt, b_T), D=(Dt, b_D), bn=(bn_t, b_bn)),
                 dict(sg=T512(), lf=T512(), kk=T512(), A=T512(), a=T512(), d=T512(), e=T512(), e2=T512(),
                      qtl=T512(BF16), ktl=T512(BF16), qh=T512(BF16), kh=T512(BF16),
                      T=(mem.sb([128, 8]), Buf()), D=(mem.sb([128, 8]), Buf()), bn=(mem.sb([128, 8]), Buf()))]

        def do_tile(t0, n):
            isc = t0 >= NLAT
            r = 1 if isc else 0
            nsub = n // 128
            nch = n // 64
            c0 = t0 // 64
            hT, bhT = hTp.get()
            src, so = xsrc(t0, n)
            for sub in range(nsub):
                xt, bx = xtp.get()
                S.dma("sp", xt[:, :], src[so + sub * 128: so + (sub + 1) * 128, :], w=[bx])
                norm_tile(src, so, r, A1, 0, xt, bx, nsm, hT, bhT, sub * 128)
            cs, bcs = cosp.get()
            sn, bsn = sinp.get()
            S.dma("pool", cs[:, 0:n], cos_d[:, t0:t0 + n], w=[bcs])
            S.dma("pool", sn[:, 0:n], sin_d[:, t0:t0 + n], w=[bsn])

            def proj(c0_):
                pf, bpf = PF4.get()
                for k in range(8):
                    mm(pf[:, 0:n], Wb[:, k, c0_:c0_ + 128], hT[:, k, 0:n], k == 0, k == 7, [b_W, bhT], [bpf])
                return pf, bpf

            chk("A1")
            for which in range(2):
                for pr in range(3):
                    pf, bpf = proj(which * 384 + pr * 128)
                    act(sq_t[:, 0:n], pf[:, 0:n], AF.Square, [bpf], [b_sq])
                    pss, bpss = PF4.get()
                    mm(pss[:, 0:n], blkb, sq_t[:, 0:n], True, True, [b_cst, b_sq], [bpss])
                    act(rt_t[:, 0:n], pss[:, 0:n], AF.Sqrt, [bpss], [b_rt], scale=1.0 / 64, bias=1e-6)
                    recip(rt_t[:, 0:n], rt_t[:, 0:n], [b_rt], [b_rt])
                    wv = qw8[:, 0:1] if which == 0 else hvec[:, 1:2]
                    stt("dve", qn_t[:, 0:n], pf[:, 0:n], wv, rt_t[:, 0:n], ALU.mult, ALU.mult, [bpf, b_rt, b_hv], [b_qn])
                    prot, bprot = PF4.get()
                    mm(prot[:, 0:n], rotb, qn_t[:, 0:n], True, True, [b_cst, b_qn], [bprot])
                    tt("pool", t1_t[:, 0:n], qn_t[:, 0:n], cs[:, 0:n], ALU.mult, [b_qn, bcs], [b_t1])
                    tt("dve", t2_t[:, 0:n], prot[:, 0:n], sn[:, 0:n], ALU.mult, [bprot, bsn], [b_t2])
                    tt("pool", qo_t[:, 0:n], t1_t[:, 0:n], t2_t[:, 0:n], ALU.add, [b_t1, b_t2], [b_qo])
                    if which == 0:
                        for hh in range(2):
                            oh = 1 - hh
                            S.dma("sp", QT_s[pr, hh, hh * 64:(hh + 1) * 64, t0:t0 + n], qo_t[hh * 64:(hh + 1) * 64, 0:n], r=[b_qo], w=[DB("Q", pr, hh, t0)])
                            S.dma("pool", QT_s[pr, hh, oh * 64:(oh + 1) * 64, t0:t0 + n], zero_t[oh * 64:(oh + 1) * 64, 0:n], r=[b_zero], w=[DB("Qz", pr, hh, t0)])
                    else:
                        S.dma("sp", KT_s[pr, :, t0:t0 + n], qo_t[:, 0:n], r=[b_qo], w=[DB("K", pr, t0)])
            chk("A2")
            for sub in range(nsub):
                pv, bpv = PF4.get()
                for k in range(8):
                    mm(pv[:, 0:384], hT[:, k, sub * 128:(sub + 1) * 128], Wb[:, k, 768:1152], k == 0, k == 7, [bhT, b_W], [bpv])
                vt, bvt = vt_p.get()
                act(vt[:, :], pv[:, 0:384], AF.Copy, [bpv], [bvt])
                S.dma("sp", V_s[t0 + sub * 128:t0 + (sub + 1) * 128, :], vt[:, :], r=[bvt], w=[DB("V", t0, sub)])
                pi, bpi = PF4.get()
                for k in range(8):
                    mm(pi[:, 0:384], hT[:, k, sub * 128:(sub + 1) * 128], Wb[:, k, 2304:2688], k == 0, k == 7, [bhT, b_W], [bpi])
                cp("dve", itm[:, sub, :], pi[:, 0:384], [bpi], [b_itm])
            chk("A3")
            for hp in range(3):
                pf, bpf = proj(1152 + hp * 128)
                act(hq_t[:, 0:n], pf[:, 0:n], AF.Copy, [bpf], [b_hq], scale=0.125)
                pf, bpf = proj(2688 + hp * 128)
                act(gs_t[:, 0:n], pf[:, 0:n], AF.Silu, [bpf], [b_gs])
                S.dma("pool", gs_s[hp, :, t0:t0 + n], gs_t[:, 0:n], r=[b_gs], w=[DB("gs", hp, t0)])
                for dr in range(2):
                    hs_ = HSETS[dr]
                    sg_t, b_sg = hs_["sg"]; lf_t, b_lf = hs_["lf"]; kk_t, b_kk = hs_["kk"]; A_t, b_At = hs_["A"]; a_t, b_a = hs_["a"]
                    d_t, b_d = hs_["d"]; e_t, b_e = hs_["e"]; e2_t, b_e2 = hs_["e2"]; qtl_t, b_qtl = hs_["qtl"]; ktl_t, b_ktl = hs_["ktl"]
                    qh_t, b_qh = hs_["qh"]; kh_t, b_kh = hs_["kh"]; Tt, b_T = hs_["T"]; Dt, b_D = hs_["D"]; bn_t, b_bn = hs_["bn"]
                    pf, bpf = proj(1536 + dr * 384 + hp * 128)
                    act(sg_t[:, 0:n], pf[:, 0:n], AF.Sigmoid, [bpf], [b_sg])
                    ts("dve", sg_t[:, 0:n], sg_t[:, 0:n], omlb[:, hp, dr:dr + 1], lbv[:, hp, dr:dr + 1], ALU.mult, ALU.add, [b_sg, b_lb], [b_sg])
                    act(lf_t[:, 0:n], sg_t[:, 0:n], AF.Ln, [b_sg], [b_lf])
                    ts("pool", kk_t[:, 0:n], sg_t[:, 0:n], -1.0, 1.0, ALU.mult, ALU.add, [b_sg], [b_kk])
                    for c in range(nch):
                        S.op("dve", lambda e, c=c, A_t=A_t, lf_t=lf_t: e.tensor_tensor_scan(out=A_t[:, c * 64:(c + 1) * 64], data0=ones64[:, :], data1=lf_t[:, c * 64:(c + 1) * 64], initial=0.0, op0=ALU.mult, op1=ALU.add), r=[b_lf, b_o64], w=[b_At])
                    A3 = A_t[:, 0:n].rearrange("p (c s) -> p c s", s=64)
                    cp("dve", Tt[:, 0:nch], A3[:, :, 63], [b_At], [b_T])
                    Tb = Tt[:, 0:nch].unsqueeze(2).to_broadcast([128, nch, 64])
                    a3 = a_t[:, 0:n].rearrange("p (c s) -> p c s", s=64)
                    if dr == 0:
                        cp("pool", a_t[:, 0:n], A_t[:, 0:n], [b_At], [b_a])
                    else:
                        tt("pool", a_t[:, 0:n], lf_t[:, 0:n], A_t[:, 0:n], ALU.subtract, [b_lf, b_At], [b_a])
                        tt("pool", a3, a3, Tb, ALU.add, [b_a, b_T], [b_a])
                    d3 = d_t[:, 0:n].rearrange("p (c s) -> p c s", s=64)
                    cp("dve", bn_t[:, 0:nch], a3[:, :, 31 if dr == 0 else 32], [b_a], [b_bn])
                    tt("pool", d3, a3, bn_t[:, 0:nch].unsqueeze(2).to_broadcast([128, nch, 64]), ALU.subtract, [b_a, b_bn], [b_d])
                    act(e_t[:, 0:n], d_t[:, 0:n], AF.Exp, [b_d], [b_e])
                    tt("dve", qtl_t[:, 0:n], hq_t[:, 0:n], e_t[:, 0:n], ALU.mult, [b_hq, b_e], [b_qtl])
                    act(e2_t[:, 0:n], d_t[:, 0:n], AF.Exp, [b_d], [b_e2], scale=-1.0)
                    tt("pool", ktl_t[:, 0:n], kk_t[:, 0:n], e2_t[:, 0:n], ALU.mult, [b_kk, b_e2], [b_ktl])
                    hsl = slice(0, 32) if dr == 0 else slice(32, 64)
                    cp("pool", kz[dr][:, 0:n].rearrange("p (c s) -> p c s", s=64)[:, :, hsl], ktl_t[:, 0:n].rearrange("p (c s) -> p c s", s=64)[:, :, hsl], [b_ktl], [b_kz[dr]])
                    act(e_t[:, 0:n], a_t[:, 0:n], AF.Exp, [b_a], [b_e])
                    tt("dve", qh_t[:, 0:n], hq_t[:, 0:n], e_t[:, 0:n], ALU.mult, [b_hq, b_e], [b_qh])
                    S.dma("sp", qhat_s[dr, hp, :, t0:t0 + n], qh_t[:, 0:n], r=[b_qh], w=[DB("qh", dr, hp, t0)])
                    tt("pool", d3, Tb, a3, ALU.subtract, [b_a, b_T], [b_d])
                    act(e2_t[:, 0:n], d_t[:, 0:n], AF.Exp, [b_d], [b_e2])
                    tt("pool", kh_t[:, 0:n], kk_t[:, 0:n], e2_t[:, 0:n], ALU.mult, [b_kk, b_e2], [b_kh])
                    act(Dt[:, 0:nch], Tt[:, 0:nch], AF.Exp, [b_T], [b_D])
                    S.dma("pool", D_s[dr, hp, :, c0:c0 + nch], Dt[:, 0:nch], r=[b_D], w=[DB("D", dr, hp, t0)])
                    chk("A4")
                    pt, bpt = PB.get()
                    for sub in range(nsub):
                        tr(pt[:, sub, :], kh_t[:, sub * 128:(sub + 1) * 128], idb, [b_kh, b_cst], [bpt])
                    act(khtm[:, 0:nsub, :], pt[:, 0:nsub, :], AF.Copy, [bpt], [b_khtm])
                    mk = cstb[:, 3 + dr, :]
                    for sub in range(nsub):
                        pscs = [PF4.get(), PF4.get()]
                        for c_ in range(2):
                            tb = sub * 128 + c_ * 64
                            rows = slice(c_ * 64, (c_ + 1) * 64)
                            for hh in range(2):
                                hs = slice(hh * 64, (hh + 1) * 64)
                                psc, bpsc = pscs[hh]
                                tfull, tz = (1, 0) if dr == 0 else (0, 1)
                                mm(psc[rows, tfull * 32:(tfull + 1) * 32], ktl_t[hs, tb:tb + 64], qtl_t[hs, tb + tfull * 32:tb + (tfull + 1) * 32], True, True, [b_ktl, b_qtl], [bpsc])
                                mm(psc[rows, tz * 32:(tz + 1) * 32], kz[dr][hs, tb:tb + 64], qtl_t[hs, tb + tz * 32:tb + (tz + 1) * 32], True, True, [b_kz[dr], b_qtl], [bpsc])
                        for hh in range(2):
                            psc, bpsc = pscs[hh]
                            tt("dve", scm[:, hh * 64:(hh + 1) * 64], psc[:, 0:64], mk[:, 0:64], ALU.mult, [bpsc, b_cst], [b_scm])
                        for c_ in range(2):
                            ps_ = slice(c_ * 64, (c_ + 1) * 64)
                            for hh in range(2):
                                hs = slice(hh * 64, (hh + 1) * 64)
                                vcol = slice((hp * 2 + hh) * 64, (hp * 2 + hh + 1) * 64)
                                mm(pu_ps[c_][hs, sub * 64:(sub + 1) * 64], khtm[ps_, sub, hs], itm[ps_, sub, vcol], True, True, [b_khtm, b_itm], [b_pu[c_]])
                                mm(po_ps[c_][hs, sub * 64:(sub + 1) * 64], itm[ps_, sub, vcol], scm[ps_, hs], True, True, [b_itm, b_scm], [b_po[c_]])
                    for c_ in range(2):
                        cp("dve", Ut[:, 0:nch, :].rearrange("p (s c) d -> p s c d", c=2)[:, :, c_, :], pu_ps[c_][:, 0:nsub * 64].rearrange("p (s d) -> p s d", d=64), [b_pu[c_]], [b_U])
                    S.dma("sp", U_s[dr, hp, :, c0:c0 + nch, :], Ut[:, 0:nch, :], r=[b_U], w=[DB("U", dr, hp, t0)])
                    for c_ in range(2):
                        ov = osb[:, 0:n].rearrange("p (s c t) -> p s c t", c=2, t=64)[:, :, c_, :]
                        pv_ = po_ps[c_][:, 0:nsub * 64].rearrange("p (s t) -> p s t", t=64)
                        if dr == 0:
                            act(ov, pv_, AF.Copy, [b_po[c_]], [b_osb])
                        else:
                            tt("dve", ov, ov, pv_, ALU.add, [b_osb, b_po[c_]], [b_osb])
                S.dma("sp", oacc_s[hp, :, t0:t0 + n], osb[:, 0:n], r=[b_osb], w=[DB("oacc", hp, t0)])
            chk("A5")
            zoff = t0 + 3 if isc else t0 + 1
            for cc in range(2):
                pf, bpf = proj(3328 + cc * 128)
                act(cs_t[:, 0:n], pf[:, 0:n], AF.Copy, [bpf], [b_cs])
                pf, bpf = proj(3584 + cc * 128)
                tt("dve", cs_t[:, 0:n], cs_t[:, 0:n], pf[:, 0:n], ALU.mult, [b_cs, bpf], [b_cs])
                S.dma("pool", z_s[cc, :, zoff:zoff + n], cs_t[:, 0:n], r=[b_cs], w=[DB("z", cc, t0)])
                pf, bpf = proj(3072 + cc * 128)
                act(cb_t[:, 0:n], pf[:, 0:n], AF.Copy, [bpf], [b_cb])
                S.dma("pool", cb_s[cc, :, t0:t0 + n], cb_t[:, 0:n], r=[b_cb], w=[DB("cb", cc, t0)])
        try:
            for (t0_, n_) in (TILES[:1] if (stop or "").startswith("A") and stop != "A" else TILES):
                do_tile(t0_, n_)
        except _Stop:
            raise _Stop()
        fence(S)
        mem.reset(PBASE)

        if stop == "A":
            raise _Stop()
        NP_ = len(rm_patterns)
        ttb = mem.sb([128, 6 * 22 * 64], BF16); b_tt = Buf()
        stg2 = RR(mem, [128, 2112], F32, 2)
        for i4 in range(4):
            st_, bst = stg2.get()
            S.dma("sp", st_[:, :], tt_d[:, i4 * 2112:(i4 + 1) * 2112], w=[bst])
            cp("dve", ttb[:, i4 * 2112:(i4 + 1) * 2112], st_[:, :], [bst], [b_tt])
        rmaf = mem.sb([128, NP_, 128]); rmab = mem.sb([128, NP_, 128], BF16); rmbf = mem.sb([128, 512]); rmbb = mem.sb([128, 512], BF16); b_rm = Buf()
        S.dma("sp", rmaf[:, :, :], rma_d[:, :, :].rearrange("n k m -> k n m"), w=[b_rm])
        S.dma("sp", rmbf[:, :], rmb_d[:, :], w=[b_rm])
        cp("dve", rmab[:, :, :], rmaf[:, :, :], [b_rm], [b_rm])
        cp("dve", rmbb[:, :], rmbf[:, :], [b_rm], [b_rm])
        kcx = mem.sb([128, 3, 256], BF16); vcx = mem.sb([128, 2, 384], BF16); b_cxkv = Buf()
        S.dma("sp", kcx[:, :, :], KT_s[:, :, NLAT:NTOK].rearrange("c p t -> p c t"), r=[DB("K", pr, NLAT) for pr in range(3)], w=[b_cxkv])
        S.dma("sp", vcx[:, :, :], V_s[NLAT:NTOK, :].rearrange("(s t) c -> t s c", t=128), r=[DB("V", NLAT, s_) for s_ in range(2)], w=[b_cxkv])
        qp = RR(mem, [128, 6, 512], BF16, 2)
        kp = RR(mem, [128, 3, 1024], BF16, 2)
        vp = RR(mem, [128, 8, 384], BF16, 2)
        ptp = RR(mem, [128, 512], BF16, 3)
        rin_t, b_rin = T512()
        ob_p = RR(mem, [128, 512], BF16, 2)
        PS2 = PRR(pfs[0:2])
        pos = PRR(pfs[2:4])
        pds = PRR(pfs[4:6])

        def rowpat(g, p):
            out = []
            for a in range(2):
                for i in range(8):
                    kr = 8 * g - 4 + 2 * p + a
                    rq = 8 * g + i
                    rs0 = min(max(rq - 4, 0), NROWS - 8)
                    out.append(rs0 <= kr < rs0 + 8)
            return tuple(out)

        for (t0, n) in TILES:
            isc = t0 >= NLAT
            g = t0 // 512
            qt, bq = qp.get()
            S.dma("sp", qt[:, :, 0:n], QT_s[:, :, :, t0:t0 + n].rearrange("c h p t -> p (c h) t"),
                  r=[DB(nm, pr, hh, t0) for nm in ("Q", "Qz") for pr in range(3) for hh in range(2)], w=[bq])
            chunks = []
            if not isc:
                ps_valid = [p for p in range(8) if 0 <= 8 * g - 4 + 2 * p < NROWS]
                p_lo, p_hi = ps_valid[0], ps_valid[-1]
                k0 = (8 * g - 4) * 64
                kt, bk = kp.get()
                vt_, bv = vp.get()
                tl, th = k0 + p_lo * 128, k0 + (p_hi + 1) * 128
                tiles_touched = sorted(set([(tl // 512) * 512, ((th - 1) // 512) * 512, (((tl + th) // 2) // 512) * 512]))
                S.dma("pool", kt[:, :, p_lo * 128:(p_hi + 1) * 128], KT_s[:, :, tl:th].rearrange("c p t -> p c t"),
                      r=[DB("K", pr, tt0) for pr in range(3) for tt0 in tiles_touched], w=[bk])
                S.dma("pool", vt_[:, p_lo:p_hi + 1, :], V_s[tl:th, :].rearrange("(s t) c -> t s c", t=128),
                      r=[DB("V", tt0, s_) for tt0 in tiles_touched for s_ in range(4)], w=[bv])
                chunks = [("loc", p) for p in ps_valid]
            chunks += [("ctx", 0), ("ctx", 1)]
            for pr in range(3):
                po_, bpo = pos.get()
                pd_, bpd = pds.get()
                for hh in range(2):
                    h = pr * 2 + hh
                    hs = slice(hh * 64, (hh + 1) * 64)
                    for ci, (kind, p) in enumerate(chunks):
                        st_ps, bst_ps = PS2.get()
                        first, last = ci == 0, ci == len(chunks) - 1
                        if kind == "loc":
                            mm(st_ps[:, 0:n], kt[:, pr, p * 128:(p + 1) * 128], qt[:, h, 0:n], True, False, [bk, bq], [bst_ps])
                            u0 = 14 - 2 * p
                            mm(st_ps[:, 0:n], idb, ttb[:, (h * 22 + u0) * 64:(h * 22 + u0 + 8) * 64], False, False, [b_cst, b_tt], [bst_ps])
                            pat = rm_patterns.index(rowpat(g, p))
                            mm(st_ps[:, 0:n], rmab[:, pat, :], rmbb[:, :], False, True, [b_rm], [bst_ps])
                            vl = vt_[:, p, h * 64:(h + 1) * 64]
                            rv = [bv]
                        else:
                            mm(st_ps[:, 0:n], kcx[:, pr, p * 128:(p + 1) * 128], qt[:, h, 0:n], True, True, [b_cxkv, bq], [bst_ps])
                            vl = vcx[:, p, h * 64:(h + 1) * 64]
                            rv = [b_cxkv]
                        pT, bpT = ptp.get()
                        act(pT[:, 0:n], st_ps[:, 0:n], AF.Exp, [bst_ps], [bpT])
                        mm(po_[hs, 0:n], vl, pT[:, 0:n], first, last, rv + [bpT], [bpo])
                        mm(pd_[hs, 0:n], onesb[:, 0:64], pT[:, 0:n], first, last, [b_onesb, bpT], [bpd])
                recip(rin_t[:, 0:n], pd_[:, 0:n], [bpd], [b_rin])
                ob, bob = ob_p.get()
                tt("dve", ob[:, 0:n], po_[:, 0:n], rin_t[:, 0:n], ALU.mult, [bpo, b_rin], [bob])
                S.dma("sp", mix_s[pr, :, t0:t0 + n], ob[:, 0:n], r=[bob], w=[DB("mix", pr, t0)])
        fence(S)
        mem.reset(PBASE)

        if stop == "B":
            raise _Stop()
        S32 = [[mem.sb([128, 64]) for _ in range(3)] for _ in range(2)]
        Sbf = [[mem.sb([128, 64], BF16) for _ in range(3)] for _ in range(2)]
        bS = [[Buf() for _ in range(3)] for _ in range(2)]
        bSb = [[Buf() for _ in range(3)] for _ in range(2)]
        qh_p = RR(mem, [128, 3, 512], BF16, 2)
        U_p = RR(mem, [128, 3, 8, 64], F32, 2)
        D_p = RR(mem, [128, 3, 8], F32, 2)
        oa_p = RR(mem, [128, 3, 512], F32, 2)
        gsl_p = RR(mem, [128, 3, 512], F32, 2)
        sq2, b_sq2 = T512(BF16)
        rt2, b_rt2 = T512()
        y2, b_y2 = T512()
        yb_p = RR(mem, [128, 512], BF16, 2)
        pin = [[pfs[0], pfs[1]], [pfs[2], pfs[3]], [pfs[4], pfs[5]]]
        bpin = [[Buf(), Buf()] for _ in range(3)]
        PSX = PRR(pfs[0:6])
        PSX.b = [bpin[i // 2][i % 2] for i in range(6)]
        for dr in range(2):
            for hp in range(3):
                S.op("pool", lambda e, t=S32[dr][hp]: e.memset(t[:, :], 0.0), w=[bS[dr][hp]])
                S.op("pool", lambda e, t=Sbf[dr][hp]: e.memset(t[:, :], 0.0), w=[bSb[dr][hp]])
            order = [TILES[-1]] + (TILES[:-1] if dr == 0 else TILES[:-1][::-1])
            for (t0, n) in order:
                nch = n // 64
                c0 = t0 // 64
                qh, bqh = qh_p.get()
                Uu, bUu = U_p.get()
                Dd, bDd = D_p.get()
                oa, boa = oa_p.get()
                S.dma("sp", qh[:, :, 0:n], qhat_s[dr, :, :, t0:t0 + n].rearrange("c p t -> p c t"), r=[DB("qh", dr, hp, t0) for hp in range(3)], w=[bqh])
                S.dma("pool", Uu[:, :, 0:nch, :], U_s[dr, :, :, c0:c0 + nch, :].rearrange("c p a b -> p c a b"), r=[DB("U", dr, hp, t0) for hp in range(3)], w=[bUu])
                S.dma("pool", Dd[:, :, 0:nch], D_s[dr, :, :, c0:c0 + nch].rearrange("c p a -> p c a"), r=[DB("D", dr, hp, t0) for hp in range(3)], w=[bDd])
                S.dma("sp", oa[:, :, 0:n], oacc_s[:, :, t0:t0 + n].rearrange("c p t -> p c t"), r=[DB("oacc", hp, t0) for hp in range(3)], w=[boa])
                if dr == 1:
                    gl, bgl = gsl_p.get()
                    S.dma("sp", gl[:, :, 0:n], gs_s[:, :, t0:t0 + n].rearrange("c p t -> p c t"), r=[DB("gs", hp, t0) for hp in range(3)], w=[bgl])
                corder = list(range(nch)) if dr == 0 else list(range(nch))[::-1]
                for c in corder:
                    for hp in range(3):
                        for hh in range(2):
                            hs = slice(hh * 64, (hh + 1) * 64)
                            mm(pin[hp][hh][hs, c * 64:(c + 1) * 64], Sbf[dr][hp][hs, :], qh[hs, hp, c * 64:(c + 1) * 64], True, True, [bSb[dr][hp], bqh], [bpin[hp][hh]])
                        stt("dve", S32[dr][hp][:, :], S32[dr][hp][:, :], Dd[:, hp, c:c + 1], Uu[:, hp, c, :], ALU.mult, ALU.add, [bS[dr][hp], bDd, bUu], [bS[dr][hp]])
                        cp("pool", Sbf[dr][hp][:, :], S32[dr][hp][:, :], [bS[dr][hp]], [bSb[dr][hp]])
                for hp in range(3):
                    for hh in range(2):
                        hs = slice(hh * 64, (hh + 1) * 64)
                        tt("dve", oa[hs, hp, 0:n], oa[hs, hp, 0:n], pin[hp][hh][hs, 0:n], ALU.add, [boa, bpin[hp][hh]], [boa])
                if dr == 0:
                    S.dma("sp", oacc_s[:, :, t0:t0 + n].rearrange("c p t -> p c t"), oa[:, :, 0:n], r=[boa], w=[DB("oacc", hp, t0) for hp in range(3)])
                else:
                    for hp in range(3):
                        act(sq2[:, 0:n], oa[:, hp, 0:n], AF.Square, [boa], [b_sq2])
                        pss, bpss = PSX.get()
                        mm(pss[:, 0:n], blkb, sq2[:, 0:n], True, True, [b_cst, b_sq2], [bpss])
                        act(rt2[:, 0:n], pss[:, 0:n], AF.Sqrt, [bpss], [b_rt2], scale=1.0 / 64, bias=1e-6)
                        recip(rt2[:, 0:n], rt2[:, 0:n], [b_rt2], [b_rt2])
                        stt("dve", y2[:, 0:n], oa[:, hp, 0:n], hvec[:, 2:3], rt2[:, 0:n], ALU.mult, ALU.mult, [boa, b_rt2, b_hv], [b_y2])
                        yb, byb = yb_p.get()
                        tt("pool", yb[:, 0:n], y2[:, 0:n], gl[:, hp, 0:n], ALU.mult, [b_y2, bgl], [byb])
                        S.dma("sp", mix_s[3 + hp, :, t0:t0 + n], yb[:, 0:n], r=[byb], w=[DB("mix", 3 + hp, t0)])
        fence(S)
        mem.reset(PBASE)

        if stop == "C":
            raise _Stop()
        zw_p = RR(mem, [128, 514], F32, 2)
        cbl_p = RR(mem, [128, 512], BF16, 2)
        y3, b_y3 = T512()
        yc_p = RR(mem, [128, 512], BF16, 2)
        for (t0, n) in TILES:
            isc = t0 >= NLAT
            zoff = t0 + 3 if isc else t0 + 1
            for cc in range(2):
                zw, bzw = zw_p.get()
                cbl, bcbl = cbl_p.get()
                rz = [DB("z", cc, tt0) for tt0 in (t0 - 512, t0, t0 + 512) if 0 <= tt0 < NLAT or tt0 == t0] + [DB("z", cc, "pad", col) for col in (0, NLAT + 1, NLAT + 2, NTOK + 3)]
                S.dma("sp", zw[:, 0:n + 2], z_s[cc, :, zoff - 1:zoff + n + 1], r=rz, w=[bzw])
                S.dma("pool", cbl[:, 0:n], cb_s[cc, :, t0:t0 + n], r=[DB("cb", cc, t0)], w=[bcbl])
                ts("dve", y3[:, 0:n], zw[:, 0:n], cw[:, cc, 0:1], None, ALU.mult, None, [bzw, b_cw], [b_y3])
                stt("dve", y3[:, 0:n], zw[:, 1:n + 1], cw[:, cc, 1:2], y3[:, 0:n], ALU.mult, ALU.add, [bzw, b_cw, b_y3], [b_y3])
                stt("dve", y3[:, 0:n], zw[:, 2:n + 2], cw[:, cc, 2:3], y3[:, 0:n], ALU.mult, ALU.add, [bzw, b_cw, b_y3], [b_y3])
                yc, byc = yc_p.get()
                tt("pool", yc[:, 0:n], y3[:, 0:n], cbl[:, 0:n], ALU.mult, [b_y3, bcbl], [byc])
                S.dma("sp", mix_s[6 + cc, :, t0:t0 + n], yc[:, 0:n], r=[byc], w=[DB("mix", 6 + cc, t0)])
        fence(S)
        mem.reset(PBASE)

        if stop == "D":
            raise _Stop()
        Wob = mem.sb([128, 8, 1024], BF16); b_Wo = Buf()
        stg3 = RR(mem, [128, 1024], F32, 2)
        for k in range(8):
            st_, bst = stg3.get()
            S.dma("sp", st_[:, :], wout_d[k * 128:(k + 1) * 128, :], w=[bst])
            cp("pool", Wob[:, k, :], st_[:, :], [bst], [b_Wo])
        wrf = mem.sb([128, 8, 16]); wrb = mem.sb([128, 8, 16], BF16); b_wr = Buf()
        S.dma("sp", wrf[:, :, :], wr_d[:, :].rearrange("(k p) e -> p k e", p=128), w=[b_wr])
        cp("dve", wrb[:, :, :], wrf[:, :, :], [b_wr], [b_wr])
        mx_p = RR(mem, [128, 8, 128], BF16, 2)
        xt_p = RR(mem, [128, 1024], F32, 2)
        xm_p = RR(mem, [128, 1024], F32, 2)
        nsm2 = norm_scratch(mem)
        h2_p = RR(mem, [128, 8, 128], BF16, 2)
        lmax = mem.sb([128, 1]); lsm = mem.sb([128, 1]); ex_t = mem.sb([128, 16]); b_sm = Buf()
        af_p = RR(mem, [128, 16], F32, 2)
        PE6 = PRR(pfs[0:6])
        for (t0, n) in TILES:
            isc = t0 >= NLAT
            r = 1 if isc else 0
            src, so = xsrc(t0, n)
            for sub in range(n // 128):
                ta = t0 + sub * 128
                mx, bmx = mx_p.get()
                S.dma("sp", mx[:, :, :], mix_s[:, :, ta:ta + 128].rearrange("k p t -> p k t"), r=[DB("mix", k, t0) for k in range(8)], w=[bmx])
                xt, bx = xt_p.get()
                S.dma("pool", xt[:, :], src[so + sub * 128:so + (sub + 1) * 128, :], w=[bx])
                xm, bxm = xm_p.get()
                for half in range(2):
                    pso, bpso = PE6.get()
                    for k in range(8):
                        mm(pso[:, :], mx[:, k, :], Wob[:, k, half * 512:(half + 1) * 512], k == 0, k == 7, [bmx, b_Wo], [bpso])
                    tt("dve", xm[:, half * 512:(half + 1) * 512], pso[:, :], G1[:, r, half * 512:(half + 1) * 512], ALU.mult, [bpso, b_G], [bxm])
                tt("pool", xm[:, :], xm[:, :], xt[:, :], ALU.add, [bxm, bx], [bxm])
                S.dma("sp", xm_s[ta:ta + 128, :], xm[:, :], r=[bxm], w=[DB("xm", ta)])
                h2, bh2 = h2_p.get()
                norm_tile(None, 0, r, A2, 3, xm, bxm, nsm2, h2, bh2, 0)
                S.dma("pool", h2_s[:, :, ta:ta + 128].rearrange("k p t -> p k t"), h2[:, :, :], r=[bh2], w=[DB("h2", ta)])
                pl, bpl = PE6.get()
                for k in range(8):
                    mm(pl[:, 0:16], h2[:, k, :], wrb[:, k, :], k == 0, k == 7, [bh2, b_wr], [bpl])
                S.op("dve", lambda e, pl=pl: e.tensor_reduce(out=lmax[:, :], in_=pl[:, 0:16], axis=AX.X, op=ALU.max), r=[bpl], w=[b_sm])
                ts("dve", lmax[:, :], lmax[:, :], -1.0, None, ALU.mult, None, [b_sm], [b_sm])
                act(ex_t[:, :], pl[:, 0:16], AF.Exp, [bpl, b_sm], [b_sm], bias=lmax[:, 0:1], accum_out=lsm[:, :])
                recip(lsm[:, :], lsm[:, :], [b_sm], [b_sm])
                af, baf = af_p.get()
                ts("dve", af[:, :], ex_t[:, :], lsm[:, 0:1], None, ALU.mult, None, [b_sm], [baf])
                S.dma("pool", aff_s[ta:ta + 128, :], af[:, :], r=[baf], w=[DB("aff", ta)])
        fence(S)
        mem.reset(PBASE)

        if stop == "E":
            raise _Stop()
        mF = mem.mark()
        stg4 = RR(mem, [128, 8, 512], F32, 2)
        cst4 = RR(mem, [128, 8, 512], BF16, 2)
        for ex in range(NEXP):
            for (wsrc, wdst, nm) in ((wg_d, wgb_s, "wg"), (wu_d, wub_s, "wu")):
                st_, bst = stg4.get()
                S.dma("sp", st_[:, :, :], wsrc[ex, :, :].rearrange("(k p) f -> p k f", p=128), w=[bst])
                cb_, bcb_ = cst4.get()
                cp("pool" if nm == "wg" else "dve", cb_[:, :, :], st_[:, :, :], [bst], [bcb_])
                S.dma("pool", wdst[ex, :, :, :], cb_[:, :, :], r=[bcb_], w=[DB(nm, ex)])
            st_, bst = stg4.get()
            S.dma("sp", st_[:, 0:4, :].rearrange("p k (a f) -> p k a f", a=1), wd_d[ex, :, :].rearrange("(k p) (a f) -> p k a f", p=128, a=1)[:, :, :, 0:512], w=[bst])
            S.dma("sp", st_[:, 4:8, :].rearrange("p k (a f) -> p k a f", a=1), wd_d[ex, :, :].rearrange("(k p) (a f) -> p k a f", p=128, a=2)[:, :, 1:2, :], w=[bst])
            cb_, bcb_ = cst4.get()
            cp("pool", cb_[:, :, :], st_[:, :, :], [bst], [bcb_])
            S.dma("pool", wdb_s[ex, :, :, 0:512], cb_[:, 0:4, :], r=[bcb_], w=[DB("wd0", ex)])
            S.dma("pool", wdb_s[ex, :, :, 512:1024], cb_[:, 4:8, :], r=[bcb_], w=[DB("wd1", ex)])
        fence(S)
        mem.reset(mF)
        thr = mem.sb([128, 2, 16]); b_thr = Buf()
        mB = mem.mark()
        for (r, tok0, ntok, cap) in ((0, 0, NLAT, 2 * NLAT // NEXP), (1, NLAT, NCTX, 2 * NCTX // NEXP)):
            per = ntok // 128
            afa = mem.sb([128, per, 16]); b_afa = Buf()
            S.dma("sp", afa[:, :, :], aff_s[tok0:tok0 + ntok, :].rearrange("(p t) e -> p t e", p=128), r=[DB("aff", ta) for ta in range(tok0, tok0 + ntok, 128)], w=[b_afa])
            afv = afa[:, :, :].rearrange("p t e -> p e t")
            cmp_ = mem.sb([128, 16, per]); b_cmp = Buf()
            lo = mem.sb([128, 16]); hi = mem.sb([128, 16]); mid = mem.sb([128, 16]); cnt = mem.sb([128, 16]); prd = mem.sb([128, 16]); dlt = mem.sb([128, 16])
            b_b = Buf()
            S.op("dve", lambda e, lo=lo: e.memset(lo[:, :], 0.0), w=[b_b])
            S.op("dve", lambda e, hi=hi: e.memset(hi[:, :], 1.0), w=[b_b])
            for it in range(30):
                tt("dve", mid[:, :], lo[:, :], hi[:, :], ALU.add, [b_b], [b_b])
                ts("dve", mid[:, :], mid[:, :], 0.5, None, ALU.mult, None, [b_b], [b_b])
                tt("dve", cmp_[:, :, :], afv, mid[:, :].unsqueeze(2).to_broadcast([128, 16, per]), ALU.is_ge, [b_afa, b_b], [b_cmp])
                S.op("dve", lambda e, cnt=cnt, cmp_=cmp_: e.tensor_reduce(out=cnt[:, :], in_=cmp_[:, :, :], axis=AX.X, op=ALU.add), r=[b_cmp], w=[b_b])
                pc, bpc = PE6.get()
                mm(pc[:, 0:16], onesf[:, :], cnt[:, :], True, True, [b_onesf, b_b], [bpc])
                ts("dve", prd[:, :], pc[:, 0:16], float(cap) - 0.5, None, ALU.is_ge, None, [bpc], [b_b])
                tt("dve", dlt[:, :], mid[:, :], lo[:, :], ALU.subtract, [b_b], [b_b])
                tt("dve", dlt[:, :], dlt[:, :], prd[:, :], ALU.mult, [b_b], [b_b])
                tt("dve", lo[:, :], lo[:, :], dlt[:, :], ALU.add, [b_b], [b_b])
                tt("dve", dlt[:, :], hi[:, :], mid[:, :], ALU.subtract, [b_b], [b_b])
                tt("dve", dlt[:, :], dlt[:, :], prd[:, :], ALU.mult, [b_b], [b_b])
                tt("dve", hi[:, :], mid[:, :], dlt[:, :], ALU.add, [b_b], [b_b])
            cp("dve", thr[:, r, :], lo[:, :], [b_b], [b_thr])
            fence(S)
            mem.reset(mB)
        h2l_p = RR(mem, [128, 8, 1024], BF16, 2)
        acc = mem.sb([128, 8, 1024]); b_acc = Buf()
        sgate = mem.sb([128, 8, 16]); afo = mem.sb([128, 8, 16]); b_sg2 = Buf()
        wgl_p = RR(mem, [128, 8, 512], BF16, 2)
        wul_p = RR(mem, [128, 8, 512], BF16, 2)
        wdl_p = RR(mem, [128, 4, 1024], BF16, 2)
        hid_p = RR(mem, [128, 4, 512], BF16, 2)
        sl_t, b_sl = T512()
        xo_p = RR(mem, [128, 1024], F32, 2)
        STILES = [(i * 1024, 1024) for i in range(NLAT // 1024)] + [(NLAT, 256)]
        for (t0, n) in STILES:
            isc = t0 >= NLAT
            r = 1 if isc else 0
            nsub = n // 128
            h2l, bh2l = h2l_p.get()
            S.dma("sp", h2l[:, :, 0:n], h2_s[:, :, t0:t0 + n].rearrange("k p t -> p k t"), r=[DB("h2", ta) for ta in range(t0, t0 + n, 128)], w=[bh2l])
            S.dma("pool", afo[:, 0:nsub, :], aff_s[t0:t0 + n, :].rearrange("(s t) e -> t s e", t=128), r=[DB("aff", ta) for ta in range(t0, t0 + n, 128)], w=[b_sg2])
            tt("dve", sgate[:, 0:nsub, :], afo[:, 0:nsub, :], thr[:, r, :].unsqueeze(1).to_broadcast([128, nsub, 16]), ALU.is_ge, [b_sg2, b_thr], [b_sg2])
            tt("dve", sgate[:, 0:nsub, :], sgate[:, 0:nsub, :], afo[:, 0:nsub, :], ALU.mult, [b_sg2], [b_sg2])
            S.op("pool", lambda e: e.memset(acc[:, :, :], 0.0), w=[b_acc])
            for ex in range(NEXP):
                wgl, bwg = wgl_p.get()
                wul, bwu = wul_p.get()
                wdl, bwd = wdl_p.get()
                S.dma("sp", wgl[:, :, :], wgb_s[ex, :, :, :], r=[DB("wg", ex)], w=[bwg])
                S.dma("pool", wul[:, :, :], wub_s[ex, :, :, :], r=[DB("wu", ex)], w=[bwu])
                S.dma("sp", wdl[:, :, :], wdb_s[ex, :, :, :], r=[DB("wd0", ex), DB("wd1", ex)], w=[bwd])
                for tt0 in range(0, n, 512):
                    nn = min(512, n - tt0)
                    hid, bhid = hid_p.get()
                    for fc in range(4):
                        pg_, bpg_ = PE6.get()
                        for k in range(8):
                            mm(pg_[:, 0:nn], wgl[:, k, fc * 128:(fc + 1) * 128], h2l[:, k, tt0:tt0 + nn], k == 0, k == 7, [bwg, bh2l], [bpg_])
                        pu2, bpu2 = PE6.get()
                        for k in range(8):
                            mm(pu2[:, 0:nn], wul[:, k, fc * 128:(fc + 1) * 128], h2l[:, k, tt0:tt0 + nn], k == 0, k == 7, [bwu, bh2l], [bpu2])
                        act(sl_t[:, 0:nn], pg_[:, 0:nn], AF.Silu, [bpg_], [b_sl])
                        tt("dve", hid[:, fc, 0:nn], sl_t[:, 0:nn], pu2[:, 0:nn], ALU.mult, [b_sl, bpu2], [bhid])
                    for sub in range(nn // 128):
                        sa = (tt0 // 128) + sub
                        for half in range(2):
                            pdn, bpdn = PE6.get()
                            for fc in range(4):
                                mm(pdn[:, :], hid[:, fc, sub * 128:(sub + 1) * 128], wdl[:, fc, half * 512:(half + 1) * 512], fc == 0, fc == 3, [bhid, bwd], [bpdn])
                            stt("dve", acc[:, sa, half * 512:(half + 1) * 512], pdn[:, :], sgate[:, sa, ex:ex + 1], acc[:, sa, half * 512:(half + 1) * 512], ALU.mult, ALU.add, [bpdn, b_sg2, b_acc], [b_acc])
            dst, do = xdst(t0)
            for sub in range(nsub):
                ta = t0 + sub * 128
                xo, bxo = xo_p.get()
                S.dma("sp", xo[:, :], xm_s[ta:ta + 128, :], r=[DB("xm", ta)], w=[bxo])
                tt("pool", acc[:, sub, :], acc[:, sub, :], G2[:, r, :], ALU.mult, [b_acc, b_G], [b_acc])
                tt("pool", xo[:, :], xo[:, :], acc[:, sub, :], ALU.add, [bxo, b_acc], [bxo])
                S.dma("sp", dst[do + sub * 128:do + (sub + 1) * 128, :], xo[:, :], r=[bxo], final=True)

    LBASE = mem.mark()
    try:
        for li, layer in enumerate(layers):
            first, last = li == 0, li == len(layers) - 1
            xin = x_in_d if first else xbuf[(li - 1) % 2]
            cxin = cx_in_d if first else cxbuf[(li - 1) % 2]
            xout = xo_out_d if last else xbuf[li % 2]
            cxout = cxo_out_d if last else cxbuf[li % 2]
            layer_body(layer, xin, cxin, xout, cxout, wmod_a[li], bmodT_a[li], n1T_a[li], n2T_a[li], win_a[li], wout_a[li], wr_a[li],
                       hvec_a[li], cw_a[li], tt_a[li], None if early else wg_a[li], None if early else wu_a[li], None if early else wd_a[li])
            fence(S)
            mem.reset(LBASE)
    except _Stop:
        pass
    S.emit()
    return nc


def _rm_patterns(nlat=16384):
    NROWS = nlat // 64
    pats = []
    for g in range(nlat // 512):
        for p in range(8):
            if not (0 <= 8 * g - 4 + 2 * p < NROWS):
                continue
            t = []
            for a in range(2):
                for i in range(8):
                    kr = 8 * g - 4 + 2 * p + a
                    rq = 8 * g + i
                    rs0 = min(max(rq - 4, 0), NROWS - 8)
                    t.append(rs0 <= kr < rs0 + 8)
            t = tuple(t)
            if t not in pats:
                pats.append(t)
    return pats


def _consts(pats, nlat=16384):
    NLAT = nlat
    NTOK = NLAT + NCTX
    NEG = -30000.0
    idn = np.eye(128, dtype=np.float32)
    blk = np.zeros((128, 128), np.float32)
    blk[:64, :64] = 1
    blk[64:, 64:] = 1
    rot = np.zeros((128, 128), np.float32)
    for hb in (0, 64):
        for m in range(64):
            q = m // 16
            if q in (0, 2):
                rot[hb + m + 16, hb + m] = -1.0
            else:
                rot[hb + m - 16, hb + m] = 1.0
    s = np.arange(64)
    mf = (s[:, None] <= s[None, :]).astype(np.float32)
    mb = (s[:, None] >= s[None, :]).astype(np.float32)
    cst = np.stack([idn, blk, rot, np.tile(mf, (2, 2)), np.tile(mb, (2, 2))]).astype(np.float32)
    rma = np.zeros((len(pats), 128, 128), np.float32)
    for pi, pt in enumerate(pats):
        for a in range(2):
            for i in range(8):
                if not pt[a * 8 + i]:
                    rma[pi, a * 8 + i, a * 64:(a + 1) * 64] = NEG
    rmb = np.zeros((128, 8, 64), np.float32)
    for a in range(2):
        for i in range(8):
            rmb[a * 8 + i, i, :] = 1.0
    rmb = rmb.reshape(128, 512)
    t = np.arange(NLAT)
    row = (t // 64).astype(np.float32)
    col = (t % 64).astype(np.float32)
    inv = (np.float32(10000.0) ** (-np.arange(16, dtype=np.float32) / np.float32(16))).astype(np.float32)
    ar = row[:, None] * inv
    ac = col[:, None] * inv
    ang = np.concatenate([ar, ar, ac, ac], axis=-1)
    cos = np.ones((128, NTOK), np.float32)
    sin = np.zeros((128, NTOK), np.float32)
    cos[:64, :NLAT] = np.cos(ang).T
    cos[64:, :NLAT] = np.cos(ang).T
    sin[:64, :NLAT] = np.sin(ang).T
    sin[64:, :NLAT] = np.sin(ang).T
    return cst, rma, rmb, cos, sin


def _tt_table(rpb):
    NEG = -30000.0
    tt = np.zeros((128, 6, 22, 64), np.float32)
    qc = np.arange(64)
    ws = np.clip(qc - 8, 0, 48)
    kc = np.arange(64)
    inwin = (kc[:, None] >= ws[None, :]) & (kc[:, None] < ws[None, :] + 16)
    rel = np.clip(kc[:, None] - qc[None, :] + 15, 0, 30)
    for a in range(2):
        for u in range(22):
            dr = a + 10 - u
            if abs(dr) <= 7:
                vals = rpb[:, dr + 7, :][:, rel]
                blk = np.where(inwin[None], vals, np.float32(NEG))
            else:
                blk = np.zeros((6, 64, 64), np.float32)
            tt[a * 64:(a + 1) * 64, :, u, :] = blk.transpose(1, 0, 2)
    return np.ascontiguousarray(tt.reshape(128, 6 * 22 * 64))


_NC_CACHE = {}


def _f(a):
    return np.ascontiguousarray(np.asarray(a, dtype=np.float32))


def _fm(v):
    return np.ascontiguousarray(_f(v).reshape(-1, 128).T)


def _common(layers, W, consts):
    cst, rma, rmb, cos, sin = consts
    lbl = np.ascontiguousarray(_f(W["hg_lb_logits"]).reshape(2, 4, 3, 128).transpose(3, 2, 0, 1))
    st = lambda fn: np.ascontiguousarray(np.stack([fn(l) for l in layers], axis=0))
    return {
        "wmod": st(lambda l: _f(W["w_mod"][l])), "bmodT": st(lambda l: _fm(W["b_mod"][l])),
        "n1T": st(lambda l: _fm(W["norm1_w"][l])), "n2T": st(lambda l: _fm(W["norm2_w"][l])),
        "win": st(lambda l: _f(W["w_in"][l])), "wout": st(lambda l: _f(W["w_out"][l])), "wr": st(lambda l: _f(W["w_router"][l])),
        "hvec": st(lambda l: np.stack([np.tile(_f(W["na_q_norm"][l]), 2), np.tile(_f(W["na_k_norm"][l]), 2),
                                       np.tile(_f(W["hg_norm"][l]), 2)], axis=1)),
        "lbl": lbl, "cw": st(lambda l: _f(W["conv_w"][l]).reshape(3, 2, 128).transpose(2, 1, 0)),
        "tt": st(lambda l: _tt_table(_f(W["na_rpb"][l]))), "rma": rma, "rmb": rmb, "cst": cst, "cos": cos, "sin": sin,
        "wg": st(lambda l: _f(W["w_exp_gate"][l])), "wu": st(lambda l: _f(W["w_exp_up"][l])), "wd": st(lambda l: _f(W["w_exp_down"][l])),
    }


def run_layer(nc, common, xs, cxs, c, c_ctx, ncores=2):
    names = set(a.memorylocations[0].name for a in nc.m.functions[0].allocations
                if isinstance(a, mybir.MemoryLocationSet) and a.kind == "ExternalInput")
    in_maps = []
    for core in range(ncores):
        b = core % 2
        c2 = np.stack([c[b], c_ctx], axis=0)
        c2T = np.ascontiguousarray(c2.reshape(2, 8, 128).transpose(2, 1, 0))
        d = dict(common)
        d.update({"x": xs[b], "cx": cxs[b], "c2T": c2T})
        in_maps.append({k: v for k, v in d.items() if k in names})
    return run_bass_kernel_spmd(nc, in_maps, core_ids=list(range(ncores)))


def kernel(x, c, ctx, c_ctx, w_mod, b_mod, norm1_w, w_in, na_q_norm, na_k_norm, na_rpb, hg_lb_logits, hg_norm, conv_w,
           w_out, norm2_w, w_router, w_exp_gate, w_exp_up, w_exp_down):
    W = dict(w_mod=w_mod, b_mod=b_mod, norm1_w=norm1_w, w_in=w_in, na_q_norm=na_q_norm, na_k_norm=na_k_norm, na_rpb=na_rpb,
             hg_lb_logits=hg_lb_logits, hg_norm=hg_norm, conv_w=conv_w, w_out=w_out, norm2_w=norm2_w, w_router=w_router,
             w_exp_gate=w_exp_gate, w_exp_up=w_exp_up, w_exp_down=w_exp_down)
    x = _f(x); ctx = _f(ctx); c = _f(c); c_ctx = _f(c_ctx)
    nlat = x.shape[1]
    pats = _rm_patterns(nlat)
    consts = _consts(pats, nlat)
    xs = [x[0], x[1]]
    cxs = [ctx[0], ctx[1]]
    layers = [0, 1, 2, 3]
    key = (tuple(layers), nlat)
    if key not in _NC_CACHE:
        _NC_CACHE[key] = build_model(layers, pats, nlat)
    res = run_layer(_NC_CACHE[key], _common(layers, W, consts), xs, cxs, c, c_ctx)
    xs = [np.asarray(res.results[b]["xo"], dtype=np.float32) for b in range(2)]
    return np.stack(xs, axis=0).astype(np.float32)
```
